# Optimizing a Trainium2 kernel written in Bass

```python
import jax, jax.numpy as jnp
from jax import lax
import numpy as np

D_MODEL = 1024
BATCH = 4
SEQ = 8192
DEPTH = 1

HEAD = 64
RW_HEADS = 8
RW_DIM = RW_HEADS * HEAD
FOX_HEADS = 8
FOX_DIM = FOX_HEADS * HEAD
MIX_DIM = RW_DIM + FOX_DIM
DECAY_LORA = 64
AAA_LORA = 64
GATE_LORA = 128
RW_SPLITS = (RW_DIM, DECAY_LORA, RW_DIM, RW_DIM, AAA_LORA, GATE_LORA)
RW_COLS = sum(RW_SPLITS)
FOX_SPLITS = (FOX_DIM, FOX_DIM, FOX_DIM, FOX_HEADS)
FOX_COLS = sum(FOX_SPLITS)
IN_COLS = RW_COLS + FOX_COLS
Q_BLOCK = 128
N_GROUPS = 8
EXPERTS_PER_GROUP = 8
N_EXPERTS = N_GROUPS * EXPERTS_PER_GROUP
TOP_K = 2
D_EXPERT = D_MODEL // 2
ROW_BLOCK = 128
NORM_EPS = 1e-6
GN_EPS = 64e-5

kernel_name = "hymba_rwkv7_fox_hmoe"


def rms_norm(x, w, eps=NORM_EPS):
    xf = x.astype(jnp.float32)
    y = xf * lax.rsqrt(jnp.mean(xf * xf, axis=-1, keepdims=True) + eps)
    return (y * w.astype(jnp.float32)).astype(x.dtype)


def split_cols(p, sizes):
    idx = np.cumsum(sizes)[:-1].tolist()
    return jnp.split(p, idx, axis=-1)


def token_shift(p, mu):
    prev = jnp.pad(p, ((0, 0), (1, 0), (0, 0)))[:, :-1]
    return p + (prev - p) * mu


def rwkv7_scan(r, w, k, v, a, b):
    B_, T_, H_, N_ = r.shape

    def step(S, inp):
        r_t, w_t, k_t, v_t, a_t, b_t = inp
        sa = jnp.einsum('bhvk,bhk->bhv', S, a_t)
        S = S * w_t[:, :, None, :] + sa[..., None] * b_t[:, :, None, :] + v_t[..., None] * k_t[:, :, None, :]
        return S, jnp.einsum('bhvk,bhk->bhv', S, r_t)

    xs = tuple(jnp.moveaxis(z, 1, 0) for z in (r, w, k, v, a, b))
    S0 = jnp.zeros((B_, H_, N_, N_), jnp.float32)
    _, y = lax.scan(step, S0, xs)
    return jnp.moveaxis(y, 0, 1)


def rwkv7_mixer(p_rw, mu, w0, w2, a0, a2, g2, k_k, k_a, r_k, ln_w, ln_b):
    B_, T_, _ = p_rw.shape
    ps = token_shift(p_rw.astype(jnp.float32), mu.astype(jnp.float32))
    r, pw, k, v, pa, pg = split_cols(ps, RW_SPLITS)
    w = -jax.nn.softplus(-(w0 + jnp.tanh(pw) @ w2)) - 0.5
    decay = jnp.exp(-jnp.exp(w))
    a = jax.nn.sigmoid(a0 + pa @ a2)
    g = jax.nn.sigmoid(pg) @ g2
    heads = lambda z: z.reshape(B_, T_, RW_HEADS, HEAD)
    kk = heads(k * k_k)
    kk = kk / jnp.maximum(jnp.sqrt(jnp.sum(kk * kk, axis=-1, keepdims=True)), 1e-12)
    k = k * (1.0 + (a - 1.0) * k_a)
    r_h, k_h, v_h, a_h, w_h = heads(r), heads(k), heads(v), heads(a), heads(decay)
    y = rwkv7_scan(r_h, w_h, k_h, v_h, -kk, kk * a_h)
    mean = jnp.mean(y, axis=-1, keepdims=True)
    var = jnp.mean(jnp.square(y - mean), axis=-1, keepdims=True)
    y = ((y - mean) * lax.rsqrt(var + GN_EPS)).reshape(B_, T_, RW_DIM) * ln_w + ln_b
    bonus = jnp.sum(r_h * k_h * r_k, axis=-1, keepdims=True) * v_h
    return (y + bonus.reshape(B_, T_, RW_DIM)) * g


def fox_attention(q, k, v, log_f):
    T_ = q.shape[1]
    c = jnp.cumsum(log_f, axis=1).transpose(0, 2, 1)
    scale = HEAD ** -0.5
    outs = []
    for blk in range(T_ // Q_BLOCK):
        q0, q1 = blk * Q_BLOCK, (blk + 1) * Q_BLOCK
        s = jnp.einsum('bqhd,bkhd->bhqk', q[:, q0:q1], k[:, :q1]).astype(jnp.float32) * scale
        s = s + c[:, :, q0:q1, None] - c[:, :, None, :q1]
        causal = jnp.arange(q1)[None, :] <= jnp.arange(q0, q1)[:, None]
        s = jnp.where(causal, s, -jnp.inf)
        p = jax.nn.softmax(s, axis=-1)
        outs.append(jnp.einsum('bhqk,bkhd->bqhd', p.astype(v.dtype), v[:, :q1]))
    return jnp.concatenate(outs, axis=1)


def fox_mixer(p_fox, b_f, qn_w, kn_w):
    B_, T_, _ = p_fox.shape
    q, k, v, f = split_cols(p_fox, FOX_SPLITS)
    heads = lambda z: z.reshape(B_, T_, FOX_HEADS, HEAD)
    q = rms_norm(heads(q), qn_w)
    k = rms_norm(heads(k), kn_w)
    log_f = jax.nn.log_sigmoid((f + b_f).astype(jnp.float32))
    return fox_attention(q, k, heads(v), log_f).reshape(B_, T_, FOX_DIM)


def hierarchical_moe(h, rg_w, rg_b, re_w, re_b, w_gate, w_up, w_down):
    B_, T_, D_ = h.shape
    xt = h.reshape(-1, D_)
    M = xt.shape[0]
    g_logits = (xt @ rg_w + rg_b).astype(jnp.float32)
    g_prob = jax.nn.softmax(g_logits, axis=-1)
    g_sel = jnp.argmax(g_logits, axis=-1)
    g_gate = jnp.take_along_axis(g_prob, g_sel[:, None], axis=-1)
    e_logits = (xt @ re_w + re_b).astype(jnp.float32).reshape(M, N_GROUPS, EXPERTS_PER_GROUP)
    e_logits = jnp.take_along_axis(e_logits, g_sel[:, None, None], axis=1)[:, 0]
    top_p, top_i = lax.top_k(jax.nn.softmax(e_logits, axis=-1), TOP_K)
    gates = g_gate * top_p / jnp.sum(top_p, axis=-1, keepdims=True)
    expert_id = (g_sel[:, None] * EXPERTS_PER_GROUP + top_i).astype(jnp.int32)
    A = M * TOP_K
    flat_id = expert_id.reshape(A)
    flat_tok = jnp.repeat(jnp.arange(M, dtype=jnp.int32), TOP_K)
    order = jnp.argsort(flat_id)
    sid = flat_id[order]
    counts = jnp.bincount(flat_id, length=N_EXPERTS)
    start = jnp.cumsum(counts) - counts
    padded = (counts + ROW_BLOCK - 1) // ROW_BLOCK * ROW_BLOCK
    pad_end = jnp.cumsum(padded)
    pad_start = pad_end - padded
    dest_sorted = (pad_start[sid] + jnp.arange(A, dtype=jnp.int32) - start[sid]).astype(jnp.int32)
    n_blocks = (A + N_EXPERTS * (ROW_BLOCK - 1) + ROW_BLOCK - 1) // ROW_BLOCK
    buf_tok = jnp.zeros((n_blocks * ROW_BLOCK,), jnp.int32).at[dest_sorted].set(flat_tok[order])
    block_expert = jnp.minimum(
        jnp.searchsorted(pad_end, jnp.arange(n_blocks, dtype=jnp.int32) * ROW_BLOCK, side='right'),
        N_EXPERTS - 1)
    xb = xt[buf_tok].reshape(n_blocks, ROW_BLOCK, D_)

    def expert_block(args):
        xblk, e = args
        hid = jax.nn.silu(xblk @ w_gate[e]) * (xblk @ w_up[e])
        return hid @ w_down[e]

    yb = lax.map(expert_block, (xb, block_expert)).reshape(-1, D_)
    dest = jnp.zeros((A,), jnp.int32).at[order].set(dest_sorted)
    y = jnp.sum(yb[dest].reshape(M, TOP_K, D_) * gates[..., None].astype(yb.dtype), axis=1)
    return y.reshape(B_, T_, D_)


def setup_inputs(seed: int = 0) -> dict:
    key = jax.random.key(seed)
    ks = jax.random.split(key, 32)
    n = lambda i, shape, s: jax.random.normal(ks[i], shape, jnp.float32) * s
    L = DEPTH
    return {
        "x": n(0, (BATCH, SEQ, D_MODEL), 1.0),
        "norm_mix_w": 1.0 + n(1, (L, D_MODEL), 0.05),
        "w_in": n(2, (L, D_MODEL, IN_COLS), D_MODEL ** -0.5),
        "mu_shift": jax.random.uniform(ks[3], (L, RW_COLS), jnp.float32),
        "rw_w0": jax.random.uniform(ks[4], (L, RW_DIM), jnp.float32, minval=-4.0, maxval=1.0),
        "rw_w2": n(5, (L, DECAY_LORA, RW_DIM), 0.1 * DECAY_LORA ** -0.5),
        "rw_a0": n(6, (L, RW_DIM), 0.1),
        "rw_a2": n(7, (L, AAA_LORA, RW_DIM), AAA_LORA ** -0.5),
        "rw_g2": n(8, (L, GATE_LORA, RW_DIM), GATE_LORA ** -0.5),
        "rw_k_k": 0.85 + n(9, (L, RW_DIM), 0.05),
        "rw_k_a": 1.0 + n(10, (L, RW_DIM), 0.05),
        "rw_r_k": n(11, (L, RW_HEADS, HEAD), 0.1),
        "rw_ln_w": 1.0 + n(12, (L, RW_DIM), 0.05),
        "rw_ln_b": n(13, (L, RW_DIM), 0.02),
        "fox_b_f": jax.random.uniform(ks[14], (L, FOX_HEADS), jnp.float32, minval=1.0, maxval=4.0),
        "fox_q_norm_w": 1.0 + n(15, (L, HEAD), 0.05),
        "fox_k_norm_w": 1.0 + n(16, (L, HEAD), 0.05),
        "w_out": n(17, (L, MIX_DIM, D_MODEL), MIX_DIM ** -0.5),
        "norm_ffn_w": 1.0 + n(18, (L, D_MODEL), 0.05),
        "router_group_w": n(19, (L, D_MODEL, N_GROUPS), D_MODEL ** -0.5),
        "router_group_b": n(20, (L, N_GROUPS), 0.01),
        "router_expert_w": n(21, (L, D_MODEL, N_EXPERTS), D_MODEL ** -0.5),
        "router_expert_b": n(22, (L, N_EXPERTS), 0.01),
        "exp_w_gate": n(23, (L, N_EXPERTS, D_MODEL, D_EXPERT), D_MODEL ** -0.5),
        "exp_w_up": n(24, (L, N_EXPERTS, D_MODEL, D_EXPERT), D_MODEL ** -0.5),
        "exp_w_down": n(25, (L, N_EXPERTS, D_EXPERT, D_MODEL), D_EXPERT ** -0.5),
    }


def reference(x, norm_mix_w, w_in, mu_shift, rw_w0, rw_w2, rw_a0, rw_a2, rw_g2, rw_k_k, rw_k_a,
              rw_r_k, rw_ln_w, rw_ln_b, fox_b_f, fox_q_norm_w, fox_k_norm_w, w_out, norm_ffn_w,
              router_group_w, router_group_b, router_expert_w, router_expert_b,
              exp_w_gate, exp_w_up, exp_w_down):
    for l in range(DEPTH):
        h = rms_norm(x, norm_mix_w[l])
        p = h @ w_in[l]
        y_rw = rwkv7_mixer(p[..., :RW_COLS], mu_shift[l], rw_w0[l], rw_w2[l], rw_a0[l], rw_a2[l],
                           rw_g2[l], rw_k_k[l], rw_k_a[l], rw_r_k[l], rw_ln_w[l], rw_ln_b[l])
        y_fox = fox_mixer(p[..., RW_COLS:], fox_b_f[l], fox_q_norm_w[l], fox_k_norm_w[l])
        y_mix = jnp.concatenate([y_rw.astype(x.dtype), y_fox.astype(x.dtype)], axis=-1)
        x = x + y_mix @ w_out[l]
        h = rms_norm(x, norm_ffn_w[l])
        x = x + hierarchical_moe(h, router_group_w[l], router_group_b[l], router_expert_w[l],
                                 router_expert_b[l], exp_w_gate[l], exp_w_up[l], exp_w_down[l]).astype(x.dtype)
    return x
```

```python
import contextlib
import numpy as np
import ml_dtypes
import concourse.bass as bass
import concourse.mybir as mybir
from concourse.bass_utils import run_bass_kernel_spmd

F32 = mybir.dt.float32
BF16 = mybir.dt.bfloat16
AF = mybir.ActivationFunctionType
ALU = mybir.AluOpType

ENGS = ("pe", "act", "dve", "pool", "sp")
EPOCH = 1400
D = 1024
NCOL = 3336
NE = 64
DECAY_C = 0.6065306597126334


class Prog:
    def __init__(self, nc):
        self.nc = nc
        self.ops = {e: [] for e in ENGS}
        self.cnt = {}
        self.seen = {e: {} for e in ENGS}
        self.lastw = {}
        self.readers = {}
        self.eng_n = {e: 0 for e in ENGS}
        self.semkeys = []
        self.dma_n = {}
        self.groups = {}

    def _semkey(self, k):
        if k not in self.cnt:
            self.cnt[k] = 0
            self.semkeys.append(k)
        return k

    def _deps(self, eng, reads, writes):
        deps = {}

        def add(tok):
            if tok is None:
                return
            k, v = tok
            if deps.get(k, 0) < v:
                deps[k] = v
        for b in reads:
            add(self.lastw.get(b))
        for b in writes:
            add(self.lastw.get(b))
            for t in self.readers.get(b, ()):
                add(t)
        waits = []
        for k, v in deps.items():
            if eng == "pe" and k[0] == "pe":
                continue
            if self.seen[eng].get(k, 0) >= v:
                continue
            self.seen[eng][k] = v
            waits.append((k, v))
        return waits

    def _commit(self, tok, reads, writes):
        for b in writes:
            self.lastw[b] = tok
            self.readers[b] = []
        for b in reads:
            self.readers.setdefault(b, []).append(tok)

    def op(self, eng, fn, reads=(), writes=(), serial=False, inc=True):
        waits = self._deps(eng, reads, writes)
        k = self._semkey((eng, self.eng_n[eng] // EPOCH))
        if serial and self.cnt[k] > 0 and self.seen[eng].get(k, 0) < self.cnt[k]:
            self.seen[eng][k] = self.cnt[k]
            waits.append((k, self.cnt[k]))
        if inc:
            self.cnt[k] += 1
            tok = (k, self.cnt[k])
            self.ops[eng].append((waits, fn, (k, 1)))
            if self.cnt[k] >= EPOCH:
                self.eng_n[eng] += EPOCH
        else:
            tok = (k, self.cnt[k] + 1)
            self.ops[eng].append((waits, fn, None))
        self._commit(tok, reads, writes)
        return tok

    def dma(self, eng, slot, fn, reads=(), writes=(), group=None):
        waits = self._deps(eng, reads, writes)
        n = self.dma_n.get(slot, 0)
        self.dma_n[slot] = n + 1
        k = self._semkey(("dma", slot, n // 96))
        self.cnt[k] += 16
        tok = (k, self.cnt[k])
        self.ops[eng].append((waits, fn, (k, 16)))
        self._commit(tok, reads, writes)
        if group is not None:
            self.groups.setdefault(group, []).extend(writes)
        return tok

    def group_end(self, group, slot):
        n = self.dma_n[slot]
        k = ("dma", slot, (n - 1) // 96)
        assert (n - 1) // 96 == 0
        for b in self.groups.pop(group, []):
            self.lastw[b] = (k, self.cnt[k])

    def barrier(self, engs=ENGS):
        for e in engs:
            waits = []
            for k in self.semkeys:
                v = self.cnt[k]
                if v == 0 or self.seen[e].get(k, 0) >= v:
                    continue
                if e == "pe" and k[0] == "pe":
                    continue
                self.seen[e][k] = v
                waits.append((k, v))
            if waits:
                self.ops[e].append((waits, None, None))

    def emit(self):
        nc = self.nc
        with contextlib.ExitStack() as st:
            st.enter_context(nc.allow_non_contiguous_dma(reason="small strided setup / layout DMAs"))
            sems = {}
            print("n_sems", len(self.semkeys), {e: len(v) for e, v in self.ops.items()})
            for i, k in enumerate(self.semkeys):
                sems[k] = st.enter_context(nc.semaphore("s%d" % i))
            block = st.enter_context(nc.Block())
            engmap = {"pe": block.tensor, "act": block.scalar, "dve": block.vector,
                      "pool": block.gpsimd, "sp": block.sync}

            def mk(e):
                def body(engine):
                    for waits, fn, inc in self.ops[e]:
                        for (k, v) in waits:
                            engine.wait_ge(sems[k], v)
                        if fn is not None:
                            ins = fn(engine)
                            if inc is not None:
                                ins.then_inc(sems[inc[0]], inc[1])
                return body
            for e in ENGS:
                if self.ops[e]:
                    engmap[e](mk(e))


def host_consts():
    c = {}
    s = np.arange(64)[:, None]
    t = np.arange(64)[None, :]
    strict = (s < t).astype(np.float32)
    incl = (s <= t).astype(np.float32)
    m4 = np.concatenate([strict, incl, strict, incl], axis=1)
    c["mask4"] = np.ascontiguousarray(np.stack([m4, m4], axis=1))
    ml = (t < s).astype(np.float32)
    c["maskL"] = np.ascontiguousarray(np.stack([ml, ml], axis=1))
    bd = np.zeros((128, 128), np.float32)
    bd[:64, :64] = 1
    bd[64:, 64:] = 1
    c["bdmask"] = bd
    c["ident"] = np.eye(128, dtype=np.float32)
    rm = np.ones((128, 512), np.float32)
    rm[:, ::64] = 0
    c["rmask"] = rm
    sel = np.zeros((128, 128), np.float32)
    sel[127, :] = 1
    c["sel127"] = sel
    ss = np.arange(128)[:, None]
    qq = np.arange(512)[None, :]
    c["nmask"] = np.ascontiguousarray(np.stack([np.where((128 * o + ss) <= qq, 0.0, -240000.0) for o in range(4)], axis=1).astype(np.float32))
    return c


PV_N = 62


def host_pv(inp):
    pv = np.zeros((128, PV_N), np.float32)
    mu = inp["mu_shift"][0]

    def put(col, vec):
        pv[:len(vec), col] = vec
    for j in range(4):
        put(j, mu[j * 128:(j + 1) * 128])
        put(5 + j, mu[576 + j * 128:576 + (j + 1) * 128])
        put(9 + j, mu[1088 + j * 128:1088 + (j + 1) * 128])
        put(15 + j, inp["rw_w0"][0][j * 128:(j + 1) * 128])
        put(19 + j, inp["rw_a0"][0][j * 128:(j + 1) * 128])
        put(23 + j, inp["rw_k_k"][0][j * 128:(j + 1) * 128])
        put(27 + j, inp["rw_k_a"][0][j * 128:(j + 1) * 128])
        put(31 + j, inp["rw_r_k"][0].reshape(-1)[j * 128:(j + 1) * 128])
        put(35 + j, inp["rw_ln_w"][0][j * 128:(j + 1) * 128])
        put(39 + j, inp["rw_ln_b"][0][j * 128:(j + 1) * 128])
    put(4, mu[512:576])
    put(13, mu[1600:1664])
    put(14, mu[1664:1792])
    put(43, np.tile(inp["fox_q_norm_w"][0], 2))
    put(44, np.tile(inp["fox_k_norm_w"][0], 2))
    put(45, inp["fox_b_f"][0])
    for kt in range(8):
        put(46 + kt, inp["norm_mix_w"][0][kt * 128:(kt + 1) * 128])
        put(54 + kt, inp["norm_ffn_w"][0][kt * 128:(kt + 1) * 128])
    return pv


def build(TP, TO, n_exp=NE, dbg=False, stop=None):
    TT = TP + TO
    NB = TT // 512
    NBP = TP // 512
    NBO = TO // 512
    NTL = TT // 128
    nc = bass.Bass("TRN2", target_bir_lowering=False)

    def din(name, shape, dt=F32):
        return nc.dram_tensor(name, shape, dt, kind="ExternalInput").ap()
    xp = din("xp", [TP, D])
    xo = din("xo", [TO, D])
    w_in = din("w_in", [D, NCOL])
    pvd = din("pv", [128, PV_N])
    w2d = din("rw_w2", [64, 512])
    a2d = din("rw_a2", [64, 512])
    g2d = din("rw_g2", [128, 512])
    w_out = din("w_out", [D, D])
    rgw = din("rgw", [D, 8])
    rew = din("rew", [D, 64])
    rbd = din("rb", [1, 72])
    if stop is None:
        wgd = din("wg", [NE, D, 512])
        wud = din("wu", [NE, D, 512])
        wdd = din("wd", [NE, 512, D])
    kbd = din("kb", [128, NTL])
    mask4d = din("mask4", [64, 2, 256])
    maskLd = din("maskL", [64, 2, 64])
    bdd = din("bdmask", [128, 128])
    identd = din("ident", [128, 128])
    rmaskd = din("rmask", [128, 512])
    sel127d = din("sel127", [128, 128])
    nmaskd = din("nmask", [128, 4, 512])
    out = nc.dram_tensor("out", [TO, D], F32, kind="ExternalOutput").ap()
    PT = nc.dram_tensor("PTo", [NCOL, TT + 1], F32, kind="ExternalOutput" if dbg else "Internal").ap()
    YT = nc.dram_tensor("YTo", [D, TO], BF16, kind="ExternalOutput" if dbg else "Internal").ap()

    P = Prog(nc)

    pe_hist = {}
    pe_idx = [0]

    def pe_serial(bank, base):
        pe_idx[0] += 1
        import os as _os2
        if _os2.environ.get("PESER") == "1":
            return True
        ser = False
        h = pe_hist.get(bank)
        if h is not None and h[0] != base and pe_idx[0] - h[1] <= 80:
            ser = True
        pe_hist[bank] = (base, pe_idx[0])
        return ser

    def mm(o, lhsT, rhs, start, stop, reads, writes):
        ser = pe_serial(writes[0], lhsT.base_partition())
        return P.op("pe", lambda e: e.matmul(o, lhsT=lhsT, rhs=rhs, start=start, stop=stop), reads, writes, serial=ser, inc=bool(stop))

    def tr(o, i, idn, reads, writes, last=True):
        ser = pe_serial(writes[0], i.base_partition())
        return P.op("pe", lambda e: e.transpose(o, i, idn), reads, writes, serial=ser, inc=last)

    def act(o, i, func, reads, writes, bias=None, scale=None, accum_out=None):
        kw = {}
        if bias is not None:
            kw["bias"] = bias
        if scale is not None:
            kw["scale"] = scale
        if accum_out is not None:
            kw["accum_out"] = accum_out
        return P.op("act", lambda e: e.activation(out=o, in_=i, func=func, **kw), reads, writes)

    def ts(eng, o, i, s1, s2, op0, op1, reads, writes):
        if s2 is None:
            return P.op(eng, lambda e: e.tensor_scalar(out=o, in0=i, scalar1=s1, scalar2=None, op0=op0), reads, writes)
        return P.op(eng, lambda e: e.tensor_scalar(out=o, in0=i, scalar1=s1, scalar2=s2, op0=op0, op1=op1), reads, writes)

    def tt(eng, o, a, b, op, reads, writes):
        return P.op(eng, lambda e: e.tensor_tensor(out=o, in0=a, in1=b, op=op), reads, writes)

    def stt(o, a, s, b, op0, op1, reads, writes):
        return P.op("dve", lambda e: e.scalar_tensor_tensor(out=o, in0=a, scalar=s, in1=b, op0=op0, op1=op1), reads, writes)

    def cp(eng, o, i, reads, writes):
        if eng == "act":
            return P.op("act", lambda e: e.activation(out=o, in_=i, func=AF.Copy), reads, writes)
        return P.op(eng, lambda e: e.tensor_copy(out=o, in_=i), reads, writes)

    def dma(eng, slot, o, i, reads, writes, group=None):
        return P.dma(eng, slot, lambda e: e.dma_start(out=o, in_=i), reads, writes, group=group)

    def memset(eng, o, val, writes):
        return P.op(eng, lambda e: e.memset(o, val), (), writes)

    with contextlib.ExitStack() as top:
        def sbt(st, name, shape, dt=F32):
            return st.enter_context(nc.sbuf_tensor("s_" + name, shape, dt))
        psb = [top.enter_context(nc.psum_tensor("ps%d" % i, [128, 512], F32)) for i in range(8)]
        ps_i = [0]

        def nps():
            i = ps_i[0] % 8
            ps_i[0] += 1
            return psb[i], "ps%d" % i

        pv = sbt(top, "pv", [128, PV_N])
        pv1 = sbt(top, "pv1", [128, PV_N])
        ident = sbt(top, "ident", [128, 128])
        bdm = sbt(top, "bdm", [128, 128])
        dma("sp", "g_top", pv[:], pvd, (), ["pv"], group="top")
        dma("sp", "g_top", ident[:], identd, (), ["ident"], group="top")
        dma("sp", "g_top", bdm[:], bdd, (), ["bdm"], group="top")
        P.group_end("top", "g_top")
        ts("dve", pv1[:], pv[:], -1.0, 1.0, ALU.mult, ALU.add, ["pv"], ["pv1"])
        import os as _os4
        for _ in range(int(_os4.environ.get("PENOPS", "0"))):
            pt_, pk_ = nps()
            mm(pt_[:, 0:128], ident[:], bdm[:], True, True, ["ident", "bdm"], [pk_])
        for _ in range(int(_os4.environ.get("NOPS", "0"))):
            ts("dve", pv1[:], pv[:], -1.0, 1.0, ALU.mult, ALU.add, ["pv"], ["pv1"])
            P.op("act", lambda e: e.activation(out=pv1[:], in_=pv[:], func=AF.Copy), ["pv"], ["pv1"])
            ts("dve", pv1[:], pv[:], -1.0, 1.0, ALU.mult, ALU.add, ["pv"], ["pv1"])
            ts("dve", pv1[:], pv[:], -1.0, 1.0, ALU.mult, ALU.add, ["pv"], ["pv1"])

        def pvc(col, n=128):
            return pv[0:n, col:col + 1]

        def pv1c(col, n=128):
            return pv1[0:n, col:col + 1]

        chunks = []
        c0 = 0
        while c0 < NCOL:
            n = min(128, NCOL - c0)
            for bnd in (512, 576, 1600, 1664):
                if c0 < bnd < c0 + n:
                    n = bnd - c0
            chunks.append((c0, n))
            c0 += n
        with contextlib.ExitStack() as st:
            win = sbt(st, "win", [128, 8, NCOL], BF16)
            for kt in range(8):
                for pc in range(3):
                    dma("pool", "g_win", win[:, kt, pc * 1112:(pc + 1) * 1112], w_in[kt * 128:(kt + 1) * 128, pc * 1112:(pc + 1) * 1112], (), ["win%d_%d" % (kt, pc)], group="win")
            P.group_end("win", "g_win")
            for kt in range(8):
                P.lastw["win%d" % kt] = P.lastw["win0_0"]
            xtm = [sbt(st, "xtm%d" % i, [128, 4, D]) for i in range(2)]
            xs = sbt(st, "xs", [128, 4, D])
            junk = sbt(st, "junk", [128, D], BF16)
            ssq = sbt(st, "ssq", [128, 4])
            hT = [sbt(st, "hT%d" % i, [128, 8, 512], BF16) for i in range(2)]
            stg = [sbt(st, "stg%d" % i, [128, 512]) for i in range(4)]
            zc = sbt(st, "zc", [128, 1])
            memset("dve", zc[:], 0.0, ["zc"])
            for r0 in range(0, NCOL, 128):
                n = min(128, NCOL - r0)
                dma("sp", "zc", PT[r0:r0 + n, 0:1], zc[0:n, :], ["zc"], [])
            sgi = 0
            for b in range(NB):
                q = b % 2
                src = xp[b * 512:(b + 1) * 512, :] if b < NBP else xo[(b - NBP) * 512:(b - NBP + 1) * 512, :]
                dma("sp", "xtm%d" % q, xtm[q][:], src.rearrange("(s p) d -> p s d", p=128), (), ["xtm%d" % q])
                for s in range(4):
                    act(junk[:], xtm[q][:, s, :], AF.Square, ["xtm%d" % q], ["junk", "ssq"], accum_out=ssq[:, s:s + 1])
                act(ssq[:], ssq[:], AF.Ln, ["ssq"], ["ssq"], bias=1e-6, scale=1.0 / D)
                act(ssq[:], ssq[:], AF.Exp, ["ssq"], ["ssq"], scale=-0.5)
                for s in range(4):
                    ts("dve", xs[:, s, :], xtm[q][:, s, :], ssq[:, s:s + 1], None, ALU.mult, None, ["xtm%d" % q, "ssq"], ["xs"])
                for kt in range(8):
                    pt, pk = nps()
                    for s in range(4):
                        tr(pt[:, s * 128:(s + 1) * 128], xs[:, s, kt * 128:(kt + 1) * 128], ident[:], ["xs", "ident"], [pk], last=(s == 3))
                    if kt % 2 == 0:
                        P.op("act", lambda e, o=hT[q][:, kt, :], i=pt[:], sc=pvc(46 + kt): e.activation(out=o, in_=i, func=AF.Copy, scale=sc),
                             [pk, "pv"], ["hT%d" % q])
                    else:
                        ts("dve", hT[q][:, kt, :], pt[:], pvc(46 + kt), None, ALU.mult, None, [pk, "pv"], ["hT%d" % q])
                for (c0, n) in chunks:
                    if b < NBP and 1792 <= c0 < 2304:
                        continue
                    pt, pk = nps()
                    for kt in range(8):
                        mm(pt[0:n, :], win[:, kt, c0:c0 + n], hT[q][:, kt, :], kt == 0, kt == 7, ["win%d" % kt, "hT%d" % q], [pk])
                    si = sgi % 4
                    sgi += 1
                    cp("act" if si % 2 == 0 else "dve", stg[si][0:n, :], pt[0:n, :], [pk], ["stg%d" % si])
                    dma("sp", "stg%d" % si, PT[c0:c0 + n, 1 + b * 512:1 + (b + 1) * 512], stg[si][0:n, :], ["stg%d" % si], [])
            P.barrier()
        if dbg:
            pass
        if stop == 'A':
            P.barrier()
            P.emit()
            return nc

        with contextlib.ExitStack() as st:
            w2s = sbt(st, "w2s", [64, 512])
            a2s = sbt(st, "a2s", [64, 512])
            g2s = sbt(st, "g2s", [128, 512])
            mask4 = sbt(st, "mask4", [64, 2, 256])
            maskL = sbt(st, "maskL", [64, 2, 64])
            rmask = sbt(st, "rmask", [128, 512])
            for nm, t_, d_ in (("w2s", w2s, w2d), ("a2s", a2s, a2d), ("g2s", g2s, g2d), ("mask4", mask4, mask4d),
                               ("maskL", maskL, maskLd), ("rmask", rmask, rmaskd)):
                dma("sp", "g_B", t_[:], d_, (), [nm], group="B")
            P.group_end("B", "g_B")
            raw = [sbt(st, "raw%d" % i, [128, 513]) for i in range(3)]
            tmp = [sbt(st, "tmp%d" % i, [128, 512]) for i in range(2)]
            names = ["pw_s", "pa_s", "pg_s", "th", "sg", "k_s", "kk", "sq", "rn", "kkn", "a_t", "ka", "sig", "cums", "dcs",
                     "E1", "E2", "E3", "t2"]
            T = {nm: sbt(st, nm, [128, 512]) for nm in names}
            SET = []
            for q in range(2):
                S = {}
                S["AR"] = sbt(st, "AR%d" % q, [128, 8, 2, 64])
                for nm in ("Bt", "Kt", "r_s", "k2", "v_s", "g", "yT"):
                    S[nm] = sbt(st, "%s%d" % (nm, q), [128, 512])
                for nm in ("Btok", "Ktok", "Vtok"):
                    S[nm] = sbt(st, "%s%d" % (nm, q), [64, 8, 128])
                S["Vpad"] = sbt(st, "Vpad%d" % q, [64, 8, 2, 128])
                S["gC"] = sbt(st, "gC%d" % q, [128, 8])
                memset("dve", S["Vpad"][:], 0.0, ["Vpad%d" % q])
                SET.append(S)
            CH = []
            for cq in range(2):
                Cq = {}
                Cq["G1"] = sbt(st, "G1%d" % cq, [64, 2, 256])
                Cq["Lm"] = sbt(st, "Lm%d" % cq, [64, 2, 64])
                Cq["NP"] = [sbt(st, "NP%d_%d" % (cq, i), [64, 2, 128]) for i in range(2)]
                Cq["LL"] = [sbt(st, "LL%d_%d" % (cq, i), [64, 2, 64]) for i in range(2)]
                Cq["TT"] = sbt(st, "TTm%d" % cq, [64, 2, 64])
                Cq["Xs"] = sbt(st, "Xs%d" % cq, [64, 128])
                Cq["Us"] = sbt(st, "Us%d" % cq, [64, 128])
                Cq["Upad"] = sbt(st, "Upad%d" % cq, [64, 2, 128])
                memset("dve", Cq["Upad"][:], 0.0, ["Upad%d" % cq])
                CH.append(Cq)
            STbd = sbt(st, "STbd", [128, 128])
            post = {nm: sbt(st, "po_" + nm, [128, 512]) for nm in ("yc", "sq", "rs", "yn", "rk", "bon")}
            outb = sbt(st, "outb", [128, 512], BF16)
            rawi = [0]

            def shift_tile(row0, n, mucol, b, dst, dkey):
                ri = rawi[0] % 3
                rawi[0] += 1
                rk_ = "raw%d" % ri
                dma("sp", rk_, raw[ri][0:n, :], PT[row0:row0 + n, b * 512:b * 512 + 513], (), [rk_])
                tq = ri % 2
                P.op("act", lambda e, o=tmp[tq][0:n, :], i=raw[ri][0:n, 0:512], sc=pvc(mucol, n): e.activation(out=o, in_=i, func=AF.Copy, scale=sc),
                     [rk_, "pv"], ["tmp%d" % tq])
                stt(dst[0:n, :], raw[ri][0:n, 1:513], pv1c(mucol, n), tmp[tq][0:n, :], ALU.mult, ALU.add,
                    [rk_, "pv1", "tmp%d" % tq], [dkey])

            import os as _os3
            for _ in range(int(_os3.environ.get("PSSHIFT", "0"))):
                nps()
            for j in range(int(_os3.environ.get("JSTART", "0")), 4):
                memset("dve", STbd[:], 0.0, ["STbd"])
                for b in range(NB):
                    q = b % 2
                    S = SET[q]
                    sk = lambda nm: "%s%d" % (nm, q)
                    own = b >= NBP
                    shift_tile(512, 64, 4, b, T["pw_s"], "pw_s")
                    shift_tile(1600, 64, 13, b, T["pa_s"], "pa_s")
                    shift_tile(1664, 128, 14, b, T["pg_s"], "pg_s")
                    shift_tile(j * 128, 128, j, b, S["r_s"], sk("r_s"))
                    shift_tile(576 + j * 128, 128, 5 + j, b, T["k_s"], "k_s")
                    shift_tile(1088 + j * 128, 128, 9 + j, b, S["v_s"], sk("v_s"))
                    act(T["th"][0:64, :], T["pw_s"][0:64, :], AF.Tanh, ["pw_s"], ["th"])
                    pt, pk = nps()
                    mm(pt[:], w2s[0:64, j * 128:(j + 1) * 128], T["th"][0:64, :], True, True, ["w2s", "th"], [pk])
                    act(T["sig"][:], pt[:], AF.Sigmoid, [pk, "pv"], ["sig"], bias=pvc(15 + j))
                    pt, pk = nps()
                    mm(pt[:], a2s[0:64, j * 128:(j + 1) * 128], T["pa_s"][0:64, :], True, True, ["a2s", "pa_s"], [pk])
                    act(T["a_t"][:], pt[:], AF.Sigmoid, [pk, "pv"], ["a_t"], bias=pvc(19 + j))
                    act(T["sg"][:], T["pg_s"][:], AF.Sigmoid, ["pg_s"], ["sg"])
                    pt, pk = nps()
                    mm(pt[:], g2s[:, j * 128:(j + 1) * 128], T["sg"][:], True, True, ["g2s", "sg"], [pk])
                    cp("dve", S["g"][:], pt[:], [pk], [sk("g")])
                    ts("dve", T["kk"][:], T["k_s"][:], pvc(23 + j), None, ALU.mult, None, ["k_s", "pv"], ["kk"])
                    tt("dve", T["sq"][:], T["kk"][:], T["kk"][:], ALU.mult, ["kk"], ["sq"])
                    pt, pk = nps()
                    mm(pt[:], bdm[:], T["sq"][:], True, True, ["bdm", "sq"], [pk])
                    act(T["rn"][:], pt[:], AF.Ln, [pk], ["rn"], bias=1e-16)
                    act(T["rn"][:], T["rn"][:], AF.Exp, ["rn"], ["rn"], scale=-0.5)
                    tt("dve", T["kkn"][:], T["kk"][:], T["rn"][:], ALU.mult, ["kk", "rn"], ["kkn"])
                    ts("dve", T["ka"][:], T["a_t"][:], pvc(27 + j), pv1c(27 + j), ALU.mult, ALU.add, ["a_t", "pv", "pv1"], ["ka"])
                    tt("dve", S["k2"][:], T["k_s"][:], T["ka"][:], ALU.mult, ["k_s", "ka"], [sk("k2")])
                    P.op("dve", lambda e, o=T["cums"][:], m=rmask[:], d1=T["sig"][:]: e.tensor_tensor_scan(out=o, data0=m, data1=d1, initial=0.0, op0=ALU.mult, op1=ALU.add),
                         ["rmask", "sig"], ["cums"])
                    tt("dve", T["dcs"][:], T["cums"][:], T["sig"][:], ALU.subtract, ["cums", "sig"], ["dcs"])
                    act(T["E1"][:], T["cums"][:], AF.Exp, ["cums"], ["E1"], scale=DECAY_C)
                    act(T["E2"][:], T["cums"][:], AF.Exp, ["cums"], ["E2"], scale=-DECAY_C)
                    act(T["E3"][:], T["dcs"][:], AF.Exp, ["dcs"], ["E3"], scale=-DECAY_C)
                    stt(S["AR"][:, :, 0, :], T["kkn"][:].rearrange("p (c t) -> p c t", t=64), -1.0,
                        T["E3"][:].rearrange("p (c t) -> p c t", t=64), ALU.mult, ALU.mult, ["kkn", "E3"], [sk("AR")])
                    tt("dve", S["AR"][:, :, 1, :], S["r_s"][:].rearrange("p (c t) -> p c t", t=64),
                       T["E2"][:].rearrange("p (c t) -> p c t", t=64), ALU.mult, [sk("r_s"), "E2"], [sk("AR")])
                    tt("dve", T["t2"][:], T["kkn"][:], T["a_t"][:], ALU.mult, ["kkn", "a_t"], ["t2"])
                    tt("dve", S["Bt"][:], T["t2"][:], T["E1"][:], ALU.mult, ["t2", "E1"], [sk("Bt")])
                    tt("dve", S["Kt"][:], S["k2"][:], T["E1"][:], ALU.mult, [sk("k2"), "E1"], [sk("Kt")])
                    cp("dve", S["gC"][:], T["E2"][:].rearrange("p (c t) -> p c t", t=64)[:, :, 63], ["E2"], [sk("gC")])
                    for nm_src, nm_dst in (("Bt", "Btok"), ("Kt", "Ktok"), ("v_s", "Vtok")):
                        for half in range(2):
                            pt, pk = nps()
                            for c4 in range(4):
                                c = half * 4 + c4
                                tr(pt[0:64, c4 * 128:(c4 + 1) * 128], S[nm_src][:, c * 64:(c + 1) * 64], ident[:], [sk(nm_src), "ident"], [pk], last=(c4 == 3))
                            cp("dve" if half == 0 else "dve", S[nm_dst][:, half * 4:(half + 1) * 4, :],
                               pt[0:64, :].rearrange("p (c f) -> p c f", f=128), [pk], [sk(nm_dst)])
                    for h in range(2):
                        cp("dve", S["Vpad"][:, :, h, h * 64:(h + 1) * 64], S["Vtok"][:, :, h * 64:(h + 1) * 64], [sk("Vtok")], [sk("Vpad")])
                    if stop is not None and stop.startswith("Bk") and [int(v) for v in stop[2:].split("_")] == [j, b, -1]:
                        P.barrier()
                        P.emit()
                        return nc
                    if stop == 'B1':
                        P.barrier()
                        P.emit()
                        return nc
                    for c in range(8):
                        cq = c % 2
                        Cq = CH[cq]
                        ck = lambda nm: "%s%d" % (nm, cq)
                        csl = slice(c * 64, (c + 1) * 64)
                        pg_, pgk = nps()
                        pl_, plk = nps()
                        for h in range(2):
                            hs = slice(h * 64, (h + 1) * 64)
                            mm(pg_[0:64, h * 256:h * 256 + 128], S["Bt"][hs, csl], S["AR"][hs, c, :, :], True, True, [sk("Bt"), sk("AR")], [pgk])
                            mm(pg_[0:64, h * 256 + 128:h * 256 + 256], S["Kt"][hs, csl], S["AR"][hs, c, :, :], True, True, [sk("Kt"), sk("AR")], [pgk])
                            mm(pl_[0:64, h * 64:(h + 1) * 64], S["AR"][hs, c, 0, :], S["Bt"][hs, csl], True, True, [sk("AR"), sk("Bt")], [plk])
                        tt("dve", Cq["G1"][:], pg_[0:64, :].rearrange("p (h f) -> p h f", h=2), mask4[:], ALU.mult, [pgk, "mask4"], [ck("G1")])
                        tt("dve", Cq["Lm"][:], pl_[0:64, 0:128].rearrange("p (h f) -> p h f", h=2), maskL[:], ALU.mult, [plk, "maskL"], [ck("Lm")])
                        if stop == 'G':
                            P.barrier()
                            P.emit()
                            return nc
                        NPc, LLc = Cq["NP"], Cq["LL"]
                        pd, pdk = nps()
                        for h in range(2):
                            mm(pd[0:64, h * 192:h * 192 + 64], Cq["Lm"][:, h, :], Cq["G1"][:, h, 0:64], True, True, [ck("Lm"), ck("G1")], [pdk])
                            mm(pd[0:64, h * 192 + 128:h * 192 + 192], Cq["G1"][:, h, 0:64], Cq["Lm"][:, h, :], True, True, [ck("Lm"), ck("G1")], [pdk])
                        pdv = pd[0:64, 0:384].rearrange("p (h f) -> p h f", h=2)
                        cp("dve", NPc[1][:, :, 0:64], pdv[:, :, 0:64], [pdk], [ck("NP1")])
                        cp("dve", LLc[1][:], pdv[:, :, 128:192], [pdk], [ck("LL1")])
                        for h in range(2):
                            tt("dve", NPc[1][:, h, 64:128], Cq["G1"][:, h, 0:64], ident[0:64, 0:64], ALU.add, [ck("G1"), "ident"], [ck("NP1")])
                        for i in range(1, 6):
                            cur, nxt = i % 2, (i + 1) % 2
                            pd, pdk = nps()
                            for h in range(2):
                                mm(pd[0:64, h * 192:h * 192 + 128], LLc[cur][:, h, :], NPc[cur][:, h, :], True, True,
                                   [ck("LL%d" % cur), ck("NP%d" % cur)], [pdk])
                                if i < 5:
                                    mm(pd[0:64, h * 192 + 128:h * 192 + 192], NPc[cur][:, h, 0:64], LLc[cur][:, h, :], True, True,
                                       [ck("LL%d" % cur), ck("NP%d" % cur)], [pdk])
                            pdv = pd[0:64, 0:384].rearrange("p (h f) -> p h f", h=2)
                            if i < 5:
                                cp("dve", NPc[nxt][:, :, 0:64], pdv[:, :, 0:64], [pdk], [ck("NP%d" % nxt)])
                                cp("dve", LLc[nxt][:], pdv[:, :, 128:192], [pdk], [ck("LL%d" % nxt)])
                                tt("dve", NPc[nxt][:, :, 64:128], pdv[:, :, 64:128], NPc[cur][:, :, 64:128], ALU.add,
                                   [pdk, ck("NP%d" % cur)], [ck("NP%d" % nxt)])
                            else:
                                tt("dve", Cq["TT"][:], pdv[:, :, 64:128], NPc[cur][:, :, 64:128], ALU.add, [pdk, ck("NP%d" % cur)], [ck("TT")])
                        if stop == 'B2a':
                            P.barrier()
                            P.emit()
                            return nc
                        px, pxk = nps()
                        mm(px[0:64, 0:128], S["AR"][:, c, 0, :], STbd[:], True, False, [sk("AR"), "STbd"], [pxk])
                        for h in range(2):
                            mm(px[0:64, h * 64:(h + 1) * 64], Cq["G1"][:, h, 128:192], S["Vtok"][:, c, h * 64:(h + 1) * 64], False, h == 1,
                               [ck("G1"), sk("Vtok")], [pxk])
                        cp("dve", Cq["Xs"][:], px[0:64, 0:128], [pxk], [ck("Xs")])
                        pu, puk = nps()
                        for h in range(2):
                            mm(pu[0:64, h * 64:(h + 1) * 64], Cq["TT"][:, h, :], Cq["Xs"][:, h * 64:(h + 1) * 64], True, True, [ck("TT"), ck("Xs")], [puk])
                        cp("dve", Cq["Us"][:], pu[0:64, 0:128], [puk], [ck("Us")])
                        import os as _os
                        OM = _os.environ.get("OWNMODE", "all")
                        if own and OM != "none":
                            for h in range(2):
                                cp("dve", Cq["Upad"][:, h, h * 64:(h + 1) * 64], Cq["Us"][:, h * 64:(h + 1) * 64], [ck("Us")], [ck("Upad")])
                            if OM != "upad":
                                py, pyk = nps()
                                if OM == "y1b":
                                    mm(py[:, 0:128], STbd[:], S["AR"][:, c, :, :], True, True, ["STbd", sk("AR")], [pyk])
                                    cp("dve", S["yT"][:, csl], py[:, 64:128], [pyk], [sk("yT")])
                                    continue
                                if OM == "y1c":
                                    mm(py[:, 0:64], STbd[:], S["AR"][:, c, 1, :], True, True, ["STbd", sk("AR")], [pyk])
                                    cp("dve", S["yT"][:, csl], py[:, 0:64], [pyk], [sk("yT")])
                                    continue
                                mm(py[:, 0:64], STbd[:], S["AR"][:, c, 1, :], True, OM == "y1", ["STbd", sk("AR")], [pyk])
                                if OM != "y1":
                                    for h in range(2):
                                        mm(py[:, 0:64], Cq["Upad"][:, h, :], Cq["G1"][:, h, 64:128], False, OM == "y2" and h == 1, [ck("Upad"), ck("G1")], [pyk])
                                        if OM != "y2":
                                            mm(py[:, 0:64], S["Vpad"][:, c, h, :], Cq["G1"][:, h, 192:256], False, h == 1, [sk("Vpad"), ck("G1")], [pyk])
                                cp("dve", S["yT"][:, csl], py[:, 0:64], [pyk], [sk("yT")])
                        pss, psk = nps()
                        mm(pss[:, 0:128], S["Btok"][:, c, :], Cq["Us"][:], True, False, [sk("Btok"), ck("Us")], [psk])
                        mm(pss[:, 0:128], S["Ktok"][:, c, :], S["Vtok"][:, c, :], False, False, [sk("Ktok"), sk("Vtok")], [psk])
                        mm(pss[:, 0:128], ident[:], STbd[:], False, True, ["ident", "STbd"], [psk])
                        stt(STbd[:], pss[:, 0:128], S["gC"][:, c:c + 1], bdm[:], ALU.mult, ALU.mult, [psk, sk("gC"), "bdm"], ["STbd"])
                        if _os3.environ.get("CHBAR", "1") == "1":
                            P.barrier()
                        if stop is not None and stop.startswith("Bk") and [int(v) for v in stop[2:].split("_")] == [j, b, c]:
                            P.barrier()
                            P.emit()
                            return nc
                    if stop == 'B2c' and own:
                        P.barrier()
                        P.emit()
                        return nc
                    if stop == 'B2b':
                        P.barrier()
                        P.emit()
                        return nc
                    if own:
                        pm, pmk = nps()
                        mm(pm[:], bdm[:], S["yT"][:], True, True, ["bdm", sk("yT")], [pmk])
                        stt(post["yc"][:], pm[:], -1.0 / 64, S["yT"][:], ALU.mult, ALU.add, [pmk, sk("yT")], ["po_yc"])
                        tt("dve", post["sq"][:], post["yc"][:], post["yc"][:], ALU.mult, ["po_yc"], ["po_sq"])
                        pm, pmk = nps()
                        mm(pm[:], bdm[:], post["sq"][:], True, True, ["bdm", "po_sq"], [pmk])
                        act(post["rs"][:], pm[:], AF.Ln, [pmk], ["po_rs"], bias=64e-5, scale=1.0 / 64)
                        act(post["rs"][:], post["rs"][:], AF.Exp, ["po_rs"], ["po_rs"], scale=-0.5)
                        tt("dve", post["yn"][:], post["yc"][:], post["rs"][:], ALU.mult, ["po_yc", "po_rs"], ["po_yn"])
                        ts("dve", post["yn"][:], post["yn"][:], pvc(35 + j), pvc(39 + j), ALU.mult, ALU.add, ["po_yn", "pv"], ["po_yn"])
                        stt(post["rk"][:], S["r_s"][:], pvc(31 + j), S["k2"][:], ALU.mult, ALU.mult, [sk("r_s"), sk("k2"), "pv"], ["po_rk"])
                        pm, pmk = nps()
                        mm(pm[:], bdm[:], post["rk"][:], True, True, ["bdm", "po_rk"], [pmk])
                        tt("dve", post["bon"][:], pm[:], S["v_s"][:], ALU.mult, [pmk, sk("v_s")], ["po_bon"])
                        tt("dve", post["bon"][:], post["bon"][:], post["yn"][:], ALU.add, ["po_bon", "po_yn"], ["po_bon"])
                        tt("dve", outb[:], post["bon"][:], S["g"][:], ALU.mult, ["po_bon", sk("g")], ["outb"])
                        bo = b - NBP
                        dma("sp", "outb", YT[j * 128:(j + 1) * 128, bo * 512:(bo + 1) * 512], outb[:], ["outb"], [])
                    if _os3.environ.get("BLKBAR") == "1":
                        P.barrier()
                    if stop is not None and stop.startswith("Bm") and [int(v) for v in stop[2:].split("_")] == [j, b]:
                        P.barrier()
                        P.emit()
                        return nc
                    if stop is not None and stop.startswith("Bn") and int(stop[2:]) == b:
                        P.barrier()
                        P.emit()
                        return nc
                if stop is not None and stop.startswith("Bj") and int(stop[2:]) == j:
                    P.barrier()
                    P.emit()
                    return nc
            P.barrier()

        if stop == 'B':
            P.barrier()
            P.emit()
            return nc
        with contextlib.ExitStack() as st:
            nmk = sbt(st, "nmk", [128, 4, 512])
            sel127 = sbt(st, "sel127", [128, 128])
            kbs = sbt(st, "kbs", [128, NTL])
            dma("sp", "g_C", nmk[:], nmaskd, (), ["nmk"], group="C")
            dma("sp", "g_C", sel127[:], sel127d, (), ["sel127"], group="C")
            dma("sp", "g_C", kbs[:], kbd, (), ["kbs"], group="C")
            fT = sbt(st, "fT", [8, TT])
            cnT = sbt(st, "cnT", [8, TT])
            cntok = sbt(st, "cntok", [128, NTL, 8])
            cref = sbt(st, "cref", [128, 8])
            biasq = sbt(st, "biasq", [128, NTL, 8])
            qahi = sbt(st, "qahi", [8, TO], BF16)
            qalo = sbt(st, "qalo", [8, TO], BF16)
            KTa = sbt(st, "KTa", [66, TT], BF16)
            QTa = sbt(st, "QTa", [66, TO], BF16)
            Vx = sbt(st, "Vx", [128, NTL, 65], BF16)
            ld = [sbt(st, "ld%d" % i, [64, 512]) for i in range(2)]
            sq2 = sbt(st, "sq2", [64, 512])
            rn2 = sbt(st, "rn2", [64, 512])
            Pt = [sbt(st, "Pt%d" % i, [128, 512], BF16) for i in range(3)]
            mt = [sbt(st, "mt%d" % i, [128, 512]) for i in range(2)]
            osb = sbt(st, "osb", [65, 512])
            ones65 = sbt(st, "ones65", [65, 64])
            yfx = sbt(st, "yfx", [64, 512], BF16)
            memset("dve", ones65[:], 1.0, ["ones65"])
            memset("dve", Vx[:], 1.0, ["Vx"])
            memset("dve", KTa[64:66, :], 1.0, ["KTa"])
            dma("sp", "g_C", fT[:], PT[3328:3336, 1:TT + 1], (), ["fT"], group="C")
            P.group_end("C", "g_C")
            nbf = sbt(st, "nbf", [8, 1])
            ts("dve", nbf[:], pv[0:8, 45:46], -1.0, None, ALU.mult, None, ["pv"], ["nbf"])
            act(fT[:], fT[:], AF.Exp, ["fT", "nbf"], ["fT"], bias=nbf[:, 0:1], scale=-1.0)
            act(fT[:], fT[:], AF.Ln, ["fT"], ["fT"], bias=1.0)
            P.op("dve", lambda e: e.tensor_tensor_scan(out=cnT[:], data0=ones65[0:8, 0:1].to_broadcast([8, TT]), data1=fT[:], initial=0.0, op0=ALU.mult, op1=ALU.add),
                 ["fT", "ones65"], ["cnT"])
            for g0 in range(0, NTL, 64):
                gn = min(64, NTL - g0)
                pt, pk = nps()
                for tl in range(gn):
                    tr(pt[:, tl * 8:(tl + 1) * 8], cnT[0:8, (g0 + tl) * 128:(g0 + tl + 1) * 128], ident[0:8, 0:8], ["cnT", "ident"], [pk], last=(tl == gn - 1))
                cp("act", cntok[:, g0:g0 + gn, :], pt[:, 0:gn * 8].rearrange("p (t h) -> p t h", h=8), [pk], ["cntok"])
            for h8 in range(8):
                tt("dve", cntok[:, :, h8], cntok[:, :, h8], kbs[:], ALU.add, ["cntok", "kbs"], ["cntok"])
            qa32 = fT[:, 0:TO]
            for jb in range(NBO):
                e_ = TP + (jb + 1) * 512 - 1
                ts("dve", fT[:, jb * 512:(jb + 1) * 512], cnT[0:8, TP + jb * 512:TP + (jb + 1) * 512], cnT[0:8, e_:e_ + 1], -8.0,
                   ALU.subtract, ALU.mult, ["cnT", "fT"], ["qa32", "fT"])
            cp("dve", qahi[:], qa32, ["qa32"], ["qahi"])
            tt("dve", qalo[:], qa32, qahi[:], ALU.subtract, ["qa32", "qahi"], ["qalo"])
            pti = [0]
            mti = [0]
            for hf in range(8):
                dma("sp", "qa_hi", QTa[64:65, :], qahi[hf:hf + 1, :], ["qahi"], ["QTa"])
                dma("sp", "qa_lo", QTa[65:66, :], qalo[hf:hf + 1, :], ["qalo"], ["QTa"])
                for kind, row0, dst, dk, nblk, toff, wcol in (("k", 2304 + hf * 64, KTa, "KTa", NB, 0, 44),
                                                              ("q", 1792 + hf * 64, QTa, "QTa", NBO, TP, 43)):
                    for b in range(nblk):
                        q = b % 2
                        lk = "ld%d" % q
                        dma("sp", lk, ld[q][:], PT[row0:row0 + 64, 1 + toff + b * 512:1 + toff + (b + 1) * 512], (), [lk])
                        tt("dve", sq2[:], ld[q][:], ld[q][:], ALU.mult, [lk], ["sq2"])
                        pt, pk = nps()
                        mm(pt[0:64, :], bdm[0:64, 0:64], sq2[:], True, True, ["bdm", "sq2"], [pk])
                        act(rn2[:], pt[0:64, :], AF.Ln, [pk], ["rn2"], bias=1e-6, scale=1.0 / 64)
                        act(rn2[:], rn2[:], AF.Exp, ["rn2"], ["rn2"], scale=-0.5)
                        stt(dst[0:64, b * 512:(b + 1) * 512], ld[q][:], pvc(wcol, 64), rn2[:], ALU.mult, ALU.mult, [lk, "pv", "rn2"], [dk])
                for b in range(NB):
                    q = b % 2
                    lk = "ld%d" % q
                    dma("sp", lk, ld[q][:], PT[2816 + hf * 64:2816 + (hf + 1) * 64, 1 + b * 512:1 + (b + 1) * 512], (), [lk])
                    pt, pk = nps()
                    for s in range(4):
                        tr(pt[:, s * 64:(s + 1) * 64], ld[q][:, s * 128:(s + 1) * 128], ident[0:64, 0:64], [lk, "ident"], [pk], last=(s == 3))
                    cp("act", Vx[:, b * 4:(b + 1) * 4, 0:64], pt[:, 0:256].rearrange("p (s d) -> p s d", s=4), [pk], ["Vx"])
                for jb in range(NBO):
                    nkt = (TP + (jb + 1) * 512) // 128
                    pt, pk = nps()
                    mm(pt[:, 0:8], sel127[:], cntok[:, nkt - 1, :], True, True, ["sel127", "cntok"], [pk])
                    cp("act", cref[:], pt[:, 0:8], [pk], ["cref"])
                    ts("dve", biasq[:, 0:nkt, hf], cntok[:, 0:nkt, hf], cref[:, hf:hf + 1], None, ALU.subtract, None, ["cntok", "cref"], ["biasq"])
                    po, pok = nps()
                    for kt in range(nkt):
                        psc, psk_ = nps()
                        if psk_ == pok:
                            psc, psk_ = nps()
                        mm(psc[:], KTa[0:66, kt * 128:(kt + 1) * 128], QTa[0:66, jb * 512:(jb + 1) * 512], True, True, ["KTa", "QTa"], [psk_])
                        pi = pti[0] % 3
                        pti[0] += 1
                        pkk = "Pt%d" % pi
                        o = kt - (nkt - 4)
                        if o >= 0:
                            mi = mti[0] % 2
                            mti[0] += 1
                            tt("dve", mt[mi][:], psc[:], nmk[:, o, :], ALU.add, [psk_, "nmk"], ["mt%d" % mi])
                            act(Pt[pi][:], mt[mi][:], AF.Exp, ["mt%d" % mi, "biasq"], [pkk], bias=biasq[:, kt, hf:hf + 1], scale=0.125)
                        else:
                            act(Pt[pi][:], psc[:], AF.Exp, [psk_, "biasq"], [pkk], bias=biasq[:, kt, hf:hf + 1], scale=0.125)
                        mm(po[0:65, :], Vx[:, kt, :], Pt[pi][:], kt == 0, kt == nkt - 1, ["Vx", pkk], [pok])
                    cp("act", osb[:], po[0:65, :], [pok], ["osb"])
                    P.op("dve", lambda e: e.reciprocal(out=osb[64:65, :], in_=osb[64:65, :]), ["osb"], ["osb"])
                    pt, pk = nps()
                    mm(pt[0:64, :], ones65[64:65, :], osb[64:65, :], True, True, ["ones65", "osb"], [pk])
                    tt("dve", yfx[:], osb[0:64, :], pt[0:64, :], ALU.mult, ["osb", pk], ["yfx"])
                    dma("sp", "yfx", YT[512 + hf * 64:512 + (hf + 1) * 64, jb * 512:(jb + 1) * 512], yfx[:], ["yfx"], [])
            P.barrier()
        if stop == 'C':
            P.barrier()
            P.emit()
            return nc
        TH = min(1024, TO)
        NH = TO // TH
        NTH = TH // 128
        with contextlib.ExitStack() as st:
            wo = sbt(st, "wo", [128, 8, D], BF16)
            for c in range(8):
                dma("pool", "g_D", wo[:, c, :], w_out[c * 128:(c + 1) * 128, :], (), ["wo%d" % c], group="D")
            rw32 = sbt(st, "rw32", [128, 8, 72])
            dma("pool", "g_D", rw32[:, :, 0:8], rgw.rearrange("(kt p) g -> p kt g", p=128), (), ["rw32a"], group="D")
            dma("pool", "g_D", rw32[:, :, 8:72], rew.rearrange("(kt p) g -> p kt g", p=128), (), ["rw32b"], group="D")
            rbs = sbt(st, "rbs", [1, 72])
            dma("pool", "g_D", rbs[:], rbd, (), ["rbs"], group="D")
            P.group_end("D", "g_D")
            ones1 = sbt(st, "ones1", [1, 128])
            memset("dve", ones1[:], 1.0, ["ones1"])
            acc = sbt(st, "acc", [128, NTH, D])
            h2T = sbt(st, "h2T", [128, 8, TH], BF16)
            gates = sbt(st, "gates", [128, NTH, 64])
            ym = [sbt(st, "ym%d" % i, [128, 8, 128], BF16) for i in range(2)]
            xt = [sbt(st, "xt%d" % i, [128, D]) for i in range(2)]
            xs2 = sbt(st, "xs2", [128, D])
            junk2 = sbt(st, "junk2", [128, D], BF16)
            ss2 = sbt(st, "ss2", [128, 1])
            h32 = sbt(st, "h32", [128, 8, 128])
            lg = sbt(st, "lg", [128, 72])
            sm = {nm: sbt(st, "sm_" + nm, [128, 8]) for nm in ("gm", "goh", "ge", "sel", "m1", "k1", "s2", "k2", "gl")}
            sc1 = {nm: sbt(st, "sc_" + nm, [128, 1]) for nm in ("gmax", "gsum", "m1", "m2", "w1", "w2")}
            e3 = sbt(st, "e3", [128, 8, 8])
            wg = [sbt(st, "wg%d" % i, [128, 8, 512], BF16) for i in range(2)]
            wu = [sbt(st, "wu%d" % i, [128, 8, 512], BF16) for i in range(2)]
            wdn = [sbt(st, "wdn%d" % i, [128, 4, D], BF16) for i in range(2)]
            sgl = [sbt(st, "sgl%d" % i, [128, 512]) for i in range(2)]
            hid = [sbt(st, "hid%d" % i, [128, 4, 512], BF16) for i in range(2)]
            for hh in range(NH):
                for i in range(NTH):
                    q = i % 2
                    tok0 = hh * TH + i * 128
                    dma("sp", "ym%d" % q, ym[q][:], YT[:, tok0:tok0 + 128].rearrange("(c p) t -> p c t", p=128), (), ["ym%d" % q])
                    dma("sp", "xt%d" % q, xt[q][:], xo[tok0:tok0 + 128, :], (), ["xt%d" % q])
                    for nh in range(2):
                        pt, pk = nps()
                        ns = slice(nh * 512, (nh + 1) * 512)
                        for c in range(8):
                            mm(pt[:], ym[q][:, c, :], wo[:, c, ns], c == 0, c == 7, ["ym%d" % q, "wo%d" % c], [pk])
                        tt("dve", acc[:, i, ns], pt[:], xt[q][:, ns], ALU.add, [pk, "xt%d" % q], ["acc%d" % i])
                    ak = "acc%d" % i
                    act(junk2[:], acc[:, i, :], AF.Square, [ak], ["junk2", "ss2"], accum_out=ss2[:, 0:1])
                    act(ss2[:], ss2[:], AF.Ln, ["ss2"], ["ss2"], bias=1e-6, scale=1.0 / D)
                    act(ss2[:], ss2[:], AF.Exp, ["ss2"], ["ss2"], scale=-0.5)
                    ts("dve", xs2[:], acc[:, i, :], ss2[:, 0:1], None, ALU.mult, None, [ak, "ss2"], ["xs2"])
                    for half in range(2):
                        pt, pk = nps()
                        for k4 in range(4):
                            kt = half * 4 + k4
                            tr(pt[:, k4 * 128:(k4 + 1) * 128], xs2[:, kt * 128:(kt + 1) * 128], ident[:], ["xs2", "ident"], [pk], last=(k4 == 3))
                        for k4 in range(4):
                            kt = half * 4 + k4
                            if k4 % 2 == 0:
                                P.op("act", lambda e, o=h32[:, kt, :], i_=pt[:, k4 * 128:(k4 + 1) * 128], sc=pvc(54 + kt): e.activation(out=o, in_=i_, func=AF.Copy, scale=sc),
                                     [pk, "pv"], ["h32"])
                            else:
                                ts("dve", h32[:, kt, :], pt[:, k4 * 128:(k4 + 1) * 128], pvc(54 + kt), None, ALU.mult, None, [pk, "pv"], ["h32"])
                    cp("dve", h2T[:, :, i * 128:(i + 1) * 128], h32[:], ["h32"], ["h2T"])
                    pt, pk = nps()
                    for kt in range(8):
                        mm(pt[:, 0:72], h32[:, kt, :], rw32[:, kt, :], kt == 0, False, ["h32", "rw32a", "rw32b"], [pk])
                    mm(pt[:, 0:72], ones1[0:1, :], rbs[0:1, :], False, True, ["ones1", "rbs"], [pk])
                    cp("act", lg[:], pt[:, 0:72], [pk], ["lg"])
                    R = ["lg"]
                    W = ["rt"]
                    P.op("dve", lambda e: e.reduce_max(out=sc1["gmax"][:], in_=lg[:, 0:8], axis=mybir.AxisListType.X), R, W)
                    ts("dve", sm["goh"][:], lg[:, 0:8], sc1["gmax"][:, 0:1], None, ALU.is_equal, None, R + W, W)
                    ts("dve", sm["gm"][:], lg[:, 0:8], sc1["gmax"][:, 0:1], None, ALU.subtract, None, R + W, W)
                    act(sm["ge"][:], sm["gm"][:], AF.Exp, W, W, accum_out=sc1["gsum"][:, 0:1])
                    P.op("dve", lambda e: e.reciprocal(out=sc1["gsum"][:], in_=sc1["gsum"][:]), ["rt"], ["rt"])
                    tt("dve", e3[:], lg[:, 8:72].rearrange("p (g e) -> p g e", e=8), sm["goh"][:].unsqueeze(2).to_broadcast([128, 8, 8]), ALU.mult, R + W, W)
                    P.op("dve", lambda e: e.reduce_sum(out=sm["sel"][:], in_=e3[:].rearrange("p g e -> p e g"), axis=mybir.AxisListType.X), W, W)
                    P.op("dve", lambda e: e.reduce_max(out=sc1["m1"][:], in_=sm["sel"][:], axis=mybir.AxisListType.X), W, W)
                    ts("dve", sm["k1"][:], sm["sel"][:], sc1["m1"][:, 0:1], None, ALU.is_equal, None, W, W)
                    stt(sm["s2"][:], sm["k1"][:], -1e30, sm["sel"][:], ALU.mult, ALU.add, W, W)
                    P.op("dve", lambda e: e.reduce_max(out=sc1["m2"][:], in_=sm["s2"][:], axis=mybir.AxisListType.X), W, W)
                    ts("dve", sm["k2"][:], sm["s2"][:], sc1["m2"][:, 0:1], None, ALU.is_equal, None, W, W)
                    tt("dve", sc1["w1"][:], sc1["m1"][:], sc1["m2"][:], ALU.subtract, W, W)
                    act(sc1["w1"][:], sc1["w1"][:], AF.Sigmoid, W, ["rt"])
                    ts("dve", sc1["w2"][:], sc1["w1"][:], -1.0, 1.0, ALU.mult, ALU.add, ["rt"], W)
                    tt("dve", sc1["w1"][:], sc1["w1"][:], sc1["gsum"][:], ALU.mult, W, W)
                    tt("dve", sc1["w2"][:], sc1["w2"][:], sc1["gsum"][:], ALU.mult, W, W)
                    ts("dve", sm["gl"][:], sm["k1"][:], sc1["w1"][:, 0:1], None, ALU.mult, None, W, W)
                    stt(sm["gl"][:], sm["k2"][:], sc1["w2"][:, 0:1], sm["gl"][:], ALU.mult, ALU.add, W, W)
                    tt("dve", gates[:, i, :].rearrange("p (g e) -> p g e", e=8), sm["goh"][:].unsqueeze(2).to_broadcast([128, 8, 8]),
                       sm["gl"][:].unsqueeze(1).to_broadcast([128, 8, 8]), ALU.mult, W, ["gates"])
                for ex in range(n_exp):
                    q = ex % 2
                    dma("pool", "wg%d" % q, wg[q][:], wgd[ex].rearrange("(kt p) f -> p kt f", p=128), (), ["wg%d" % q])
                    dma("pool", "wu%d" % q, wu[q][:], wud[ex].rearrange("(kt p) f -> p kt f", p=128), (), ["wu%d" % q])
                    dma("pool", "wdn%d" % q, wdn[q][:], wdd[ex].rearrange("(fc p) n -> p fc n", p=128), (), ["wdn%d" % q])
                    for sb_ in range(TH // 512):
                        hq = sb_ % 2
                        tsl = slice(sb_ * 512, (sb_ + 1) * 512)
                        for fc in range(4):
                            pg_, pgk = nps()
                            pu_, puk = nps()
                            for kt in range(8):
                                mm(pg_[:], wg[q][:, kt, fc * 128:(fc + 1) * 128], h2T[:, kt, tsl], kt == 0, kt == 7, ["wg%d" % q, "h2T"], [pgk])
                            for kt in range(8):
                                mm(pu_[:], wu[q][:, kt, fc * 128:(fc + 1) * 128], h2T[:, kt, tsl], kt == 0, kt == 7, ["wu%d" % q, "h2T"], [puk])
                            sq_ = fc % 2
                            act(sgl[sq_][:], pg_[:], AF.Silu, [pgk], ["sgl%d" % sq_])
                            tt("dve", hid[hq][:, fc, :], sgl[sq_][:], pu_[:], ALU.mult, ["sgl%d" % sq_, puk], ["hid%d_%d" % (hq, fc)])
                        for t4 in range(4):
                            ti = sb_ * 4 + t4
                            for nh in range(2):
                                po, pok = nps()
                                for fc in range(4):
                                    mm(po[:], hid[hq][:, fc, t4 * 128:(t4 + 1) * 128], wdn[q][:, fc, nh * 512:(nh + 1) * 512], fc == 0, fc == 3,
                                       ["hid%d_%d" % (hq, fc), "wdn%d" % q], [pok])
                                stt(acc[:, ti, nh * 512:(nh + 1) * 512], po[:], gates[:, ti, ex:ex + 1], acc[:, ti, nh * 512:(nh + 1) * 512],
                                    ALU.mult, ALU.add, [pok, "gates", "acc%d" % ti], ["acc%d" % ti])
                for i in range(NTH):
                    tok0 = hh * TH + i * 128
                    dma("sp", "ost%d" % (i % 4), out[tok0:tok0 + 128, :], acc[:, i, :], ["acc%d" % i], ["out%d" % i])
                P.barrier()
        P.barrier()
        P.emit()
    return nc


def make_core_inputs(inputs, c, T, consts, pvt):
    b, th = c // 2, c % 2
    TP = TO = T // 2
    x = inputs["x"]
    d = {}
    d["xp"] = np.ascontiguousarray(x[b, 0:TP]) if th == 1 else np.zeros((TP, D), np.float32)
    d["xo"] = np.ascontiguousarray(x[b, th * TO:(th + 1) * TO])
    d["w_in"] = inputs["w_in"][0]
    d["pv"] = pvt
    d["rw_w2"] = inputs["rw_w2"][0]
    d["rw_a2"] = inputs["rw_a2"][0]
    d["rw_g2"] = inputs["rw_g2"][0]
    d["w_out"] = inputs["w_out"][0]
    d["rgw"] = inputs["router_group_w"][0]
    d["rew"] = inputs["router_expert_w"][0]
    d["rb"] = np.concatenate([inputs["router_group_b"][0], inputs["router_expert_b"][0]])[None, :]
    d["wg"] = inputs["exp_w_gate"][0]
    d["wu"] = inputs["exp_w_up"][0]
    d["wd"] = inputs["exp_w_down"][0]
    kb = np.zeros((128, T // 128), np.float32)
    if th == 0:
        kb[:, :TP // 128] = -30000.0
    d["kb"] = kb
    d.update(consts)
    return d


def kernel(**inputs):
    inputs = {k: np.asarray(v) for k, v in inputs.items()}
    x = inputs["x"]
    B, T, _ = x.shape
    TP = TO = T // 2
    consts = host_consts()
    pvt = host_pv(inputs)
    nc = build(TP, TO)
    ncores = 2 * B
    in_maps = [make_core_inputs(inputs, c, T, consts, pvt) for c in range(ncores)]
    res = run_bass_kernel_spmd(nc, in_maps, core_ids=list(range(ncores)))
    out = np.empty((B, T, D), np.float32)
    for c in range(ncores):
        b, th = c // 2, c % 2
        out[b, th * TO:(th + 1) * TO] = res.results[c]["out"]
    return out
```

```python
import contextlib
import numpy as np
import ml_dtypes
import concourse.bass as bass
import concourse.mybir as mybir
from concourse.bass_utils import run_bass_kernel_spmd

F32 = mybir.dt.float32
BF16 = mybir.dt.bfloat16
AF = mybir.ActivationFunctionType
ALU = mybir.AluOpType

ENGS = ("pe", "act", "dve", "pool", "sp")
EPOCH = 1400
D = 1024
NCOL = 3336
NE = 64
DECAY_C = 0.6065306597126334


class Prog:
    def __init__(self, nc):
        self.nc = nc
        self.ops = {e: [] for e in ENGS}
        self.cnt = {}
        self.seen = {e: {} for e in ENGS}
        self.lastw = {}
        self.readers = {}
        self.eng_n = {e: 0 for e in ENGS}
        self.semkeys = []
        self.dma_n = {}
        self.groups = {}

    def _semkey(self, k):
        if k not in self.cnt:
            self.cnt[k] = 0
            self.semkeys.append(k)
        return k

    def _deps(self, eng, reads, writes):
        deps = {}

        def add(tok):
            if tok is None:
                return
            k, v = tok
            if deps.get(k, 0) < v:
                deps[k] = v
        for b in reads:
            add(self.lastw.get(b))
        for b in writes:
            add(self.lastw.get(b))
            for t in self.readers.get(b, ()):
                add(t)
        waits = []
        for k, v in deps.items():
            if eng == "pe" and k[0] == "pe":
                continue
            if self.seen[eng].get(k, 0) >= v:
                continue
            self.seen[eng][k] = v
            waits.append((k, v))
        return waits

    def _commit(self, tok, reads, writes):
        for b in writes:
            self.lastw[b] = tok
            self.readers[b] = []
        for b in reads:
            self.readers.setdefault(b, []).append(tok)

    def op(self, eng, fn, reads=(), writes=(), serial=False, inc=True):
        waits = self._deps(eng, reads, writes)
        k = self._semkey((eng, self.eng_n[eng] // EPOCH))
        if serial and self.cnt[k] > 0 and self.seen[eng].get(k, 0) < self.cnt[k]:
            self.seen[eng][k] = self.cnt[k]
            waits.append((k, self.cnt[k]))
        if inc:
            self.cnt[k] += 1
            tok = (k, self.cnt[k])
            self.ops[eng].append((waits, fn, (k, 1)))
            if self.cnt[k] >= EPOCH:
                self.eng_n[eng] += EPOCH
        else:
            tok = (k, self.cnt[k] + 1)
            self.ops[eng].append((waits, fn, None))
        self._commit(tok, reads, writes)
        return tok

    def dma(self, eng, slot, fn, reads=(), writes=(), group=None):
        waits = self._deps(eng, reads, writes)
        n = self.dma_n.get(slot, 0)
        self.dma_n[slot] = n + 1
        k = self._semkey(("dma", slot, n // 96))
        self.cnt[k] += 16
        tok = (k, self.cnt[k])
        self.ops[eng].append((waits, fn, (k, 16)))
        self._commit(tok, reads, writes)
        if group is not None:
            self.groups.setdefault(group, []).extend(writes)
        return tok

    def group_end(self, group, slot):
        n = self.dma_n[slot]
        k = ("dma", slot, (n - 1) // 96)
        assert (n - 1) // 96 == 0
        for b in self.groups.pop(group, []):
            self.lastw[b] = (k, self.cnt[k])

    def barrier(self, engs=ENGS):
        for e in engs:
            waits = []
            for k in self.semkeys:
                v = self.cnt[k]
                if v == 0 or self.seen[e].get(k, 0) >= v:
                    continue
                if e == "pe" and k[0] == "pe":
                    continue
                self.seen[e][k] = v
                waits.append((k, v))
            if waits:
                self.ops[e].append((waits, None, None))

    def emit(self):
        nc = self.nc
        with contextlib.ExitStack() as st:
            st.enter_context(nc.allow_non_contiguous_dma(reason="small strided setup / layout DMAs"))
            sems = {}
            print("n_sems", len(self.semkeys), {e: len(v) for e, v in self.ops.items()})
            for i, k in enumerate(self.semkeys):
                sems[k] = st.enter_context(nc.semaphore("s%d" % i))
            block = st.enter_context(nc.Block())
            engmap = {"pe": block.tensor, "act": block.scalar, "dve": block.vector,
                      "pool": block.gpsimd, "sp": block.sync}

            def mk(e):
                def body(engine):
                    for waits, fn, inc in self.ops[e]:
                        for (k, v) in waits:
                            engine.wait_ge(sems[k], v)
                        if fn is not None:
                            ins = fn(engine)
                            if inc is not None:
                                ins.then_inc(sems[inc[0]], inc[1])
                return body
            for e in ENGS:
                if self.ops[e]:
                    engmap[e](mk(e))


def host_consts():
    c = {}
    s = np.arange(64)[:, None]
    t = np.arange(64)[None, :]
    strict = (s < t).astype(np.float32)
    incl = (s <= t).astype(np.float32)
    m4 = np.concatenate([strict, incl, strict, incl], axis=1)
    c["mask4"] = np.ascontiguousarray(np.stack([m4, m4], axis=1))
    ml = (t < s).astype(np.float32)
    c["maskL"] = np.ascontiguousarray(np.stack([ml, ml], axis=1))
    bd = np.zeros((128, 128), np.float32)
    bd[:64, :64] = 1
    bd[64:, 64:] = 1
    c["bdmask"] = bd
    c["ident"] = np.eye(128, dtype=np.float32)
    rm = np.ones((128, 512), np.float32)
    rm[:, ::64] = 0
    c["rmask"] = rm
    sel = np.zeros((128, 128), np.float32)
    sel[127, :] = 1
    c["sel127"] = sel
    ss = np.arange(128)[:, None]
    qq = np.arange(512)[None, :]
    c["nmask"] = np.ascontiguousarray(np.stack([np.where((128 * o + ss) <= qq, 0.0, -240000.0) for o in range(4)], axis=1).astype(np.float32))
    return c


PV_N = 62


def host_pv(inp):
    pv = np.zeros((128, PV_N), np.float32)
    mu = inp["mu_shift"][0]

    def put(col, vec):
        pv[:len(vec), col] = vec
    for j in range(4):
        put(j, mu[j * 128:(j + 1) * 128])
        put(5 + j, mu[576 + j * 128:576 + (j + 1) * 128])
        put(9 + j, mu[1088 + j * 128:1088 + (j + 1) * 128])
        put(15 + j, inp["rw_w0"][0][j * 128:(j + 1) * 128])
        put(19 + j, inp["rw_a0"][0][j * 128:(j + 1) * 128])
        put(23 + j, inp["rw_k_k"][0][j * 128:(j + 1) * 128])
        put(27 + j, inp["rw_k_a"][0][j * 128:(j + 1) * 128])
        put(31 + j, inp["rw_r_k"][0].reshape(-1)[j * 128:(j + 1) * 128])
        put(35 + j, inp["rw_ln_w"][0][j * 128:(j + 1) * 128])
        put(39 + j, inp["rw_ln_b"][0][j * 128:(j + 1) * 128])
    put(4, mu[512:576])
    put(13, mu[1600:1664])
    put(14, mu[1664:1792])
    put(43, np.tile(inp["fox_q_norm_w"][0], 2))
    put(44, np.tile(inp["fox_k_norm_w"][0], 2))
    put(45, inp["fox_b_f"][0])
    for kt in range(8):
        put(46 + kt, inp["norm_mix_w"][0][kt * 128:(kt + 1) * 128])
        put(54 + kt, inp["norm_ffn_w"][0][kt * 128:(kt + 1) * 128])
    return pv


def build(TP, TO, n_exp=NE, dbg=False, stop=None):
    TT = TP + TO
    NB = TT // 512
    NBP = TP // 512
    NBO = TO // 512
    NTL = TT // 128
    nc = bass.Bass("TRN2", target_bir_lowering=False)

    def din(name, shape, dt=F32):
        return nc.dram_tensor(name, shape, dt, kind="ExternalInput").ap()
    xp = din("xp", [TP, D])
    xo = din("xo", [TO, D])
    w_in = din("w_in", [D, NCOL])
    pvd = din("pv", [128, PV_N])
    w2d = din("rw_w2", [64, 512])
    a2d = din("rw_a2", [64, 512])
    g2d = din("rw_g2", [128, 512])
    w_out = din("w_out", [D, D])
    rgw = din("rgw", [D, 8])
    rew = din("rew", [D, 64])
    rbd = din("rb", [1, 72])
    if stop is None:
        wgd = din("wg", [NE, D, 512])
        wud = din("wu", [NE, D, 512])
        wdd = din("wd", [NE, 512, D])
    kbd = din("kb", [128, NTL])
    mask4d = din("mask4", [64, 2, 256])
    maskLd = din("maskL", [64, 2, 64])
    bdd = din("bdmask", [128, 128])
    identd = din("ident", [128, 128])
    rmaskd = din("rmask", [128, 512])
    sel127d = din("sel127", [128, 128])
    nmaskd = din("nmask", [128, 4, 512])
    out = nc.dram_tensor("out", [TO, D], F32, kind="ExternalOutput").ap()
    PT = nc.dram_tensor("PTo", [NCOL, TT + 1], F32, kind="ExternalOutput" if dbg else "Internal").ap()
    YT = nc.dram_tensor("YTo", [D, TO], BF16, kind="ExternalOutput" if dbg else "Internal").ap()

    P = Prog(nc)

    pe_hist = {}
    pe_idx = [0]

    def pe_serial(bank, base):
        pe_idx[0] += 1
        import os as _os2
        if _os2.environ.get("PESER") == "1":
            return True
        ser = False
        h = pe_hist.get(bank)
        if h is not None and h[0] != base and pe_idx[0] - h[1] <= 80:
            ser = True
        pe_hist[bank] = (base, pe_idx[0])
        return ser

    def mm(o, lhsT, rhs, start, stop, reads, writes):
        ser = pe_serial(writes[0], lhsT.base_partition())
        return P.op("pe", lambda e: e.matmul(o, lhsT=lhsT, rhs=rhs, start=start, stop=stop), reads, writes, serial=ser, inc=bool(stop))

    def tr(o, i, idn, reads, writes, last=True):
        ser = pe_serial(writes[0], i.base_partition())
        return P.op("pe", lambda e: e.transpose(o, i, idn), reads, writes, serial=ser, inc=last)

    def act(o, i, func, reads, writes, bias=None, scale=None, accum_out=None):
        kw = {}
        if bias is not None:
            kw["bias"] = bias
        if scale is not None:
            kw["scale"] = scale
        if accum_out is not None:
            kw["accum_out"] = accum_out
        return P.op("act", lambda e: e.activation(out=o, in_=i, func=func, **kw), reads, writes)

    def ts(eng, o, i, s1, s2, op0, op1, reads, writes):
        if s2 is None:
            return P.op(eng, lambda e: e.tensor_scalar(out=o, in0=i, scalar1=s1, scalar2=None, op0=op0), reads, writes)
        return P.op(eng, lambda e: e.tensor_scalar(out=o, in0=i, scalar1=s1, scalar2=s2, op0=op0, op1=op1), reads, writes)

    def tt(eng, o, a, b, op, reads, writes):
        return P.op(eng, lambda e: e.tensor_tensor(out=o, in0=a, in1=b, op=op), reads, writes)

    def stt(o, a, s, b, op0, op1, reads, writes):
        return P.op("dve", lambda e: e.scalar_tensor_tensor(out=o, in0=a, scalar=s, in1=b, op0=op0, op1=op1), reads, writes)

    def cp(eng, o, i, reads, writes):
        if eng == "act":
            return P.op("act", lambda e: e.activation(out=o, in_=i, func=AF.Copy), reads, writes)
        return P.op(eng, lambda e: e.tensor_copy(out=o, in_=i), reads, writes)

    def dma(eng, slot, o, i, reads, writes, group=None):
        return P.dma(eng, slot, lambda e: e.dma_start(out=o, in_=i), reads, writes, group=group)

    def memset(eng, o, val, writes):
        return P.op(eng, lambda e: e.memset(o, val), (), writes)

    with contextlib.ExitStack() as top:
        def sbt(st, name, shape, dt=F32):
            return st.enter_context(nc.sbuf_tensor("s_" + name, shape, dt))
        psb = [top.enter_context(nc.psum_tensor("ps%d" % i, [128, 512], F32)) for i in range(8)]
        ps_i = [0]

        def nps():
            i = ps_i[0] % 8
            ps_i[0] += 1
            return psb[i], "ps%d" % i

        pv = sbt(top, "pv", [128, PV_N])
        pv1 = sbt(top, "pv1", [128, PV_N])
        ident = sbt(top, "ident", [128, 128])
        bdm = sbt(top, "bdm", [128, 128])
        dma("sp", "g_top", pv[:], pvd, (), ["pv"], group="top")
        dma("sp", "g_top", ident[:], identd, (), ["ident"], group="top")
        dma("sp", "g_top", bdm[:], bdd, (), ["bdm"], group="top")
        P.group_end("top", "g_top")
        ts("dve", pv1[:], pv[:], -1.0, 1.0, ALU.mult, ALU.add, ["pv"], ["pv1"])
        import os as _os4
        for _ in range(int(_os4.environ.get("PENOPS", "0"))):
            pt_, pk_ = nps()
            mm(pt_[:, 0:128], ident[:], bdm[:], True, True, ["ident", "bdm"], [pk_])
        for _ in range(int(_os4.environ.get("NOPS", "0"))):
            ts("dve", pv1[:], pv[:], -1.0, 1.0, ALU.mult, ALU.add, ["pv"], ["pv1"])
            P.op("act", lambda e: e.activation(out=pv1[:], in_=pv[:], func=AF.Copy), ["pv"], ["pv1"])
            ts("dve", pv1[:], pv[:], -1.0, 1.0, ALU.mult, ALU.add, ["pv"], ["pv1"])
            ts("dve", pv1[:], pv[:], -1.0, 1.0, ALU.mult, ALU.add, ["pv"], ["pv1"])

        def pvc(col, n=128):
            return pv[0:n, col:col + 1]

        def pv1c(col, n=128):
            return pv1[0:n, col:col + 1]

        chunks = []
        c0 = 0
        while c0 < NCOL:
            n = min(128, NCOL - c0)
            for bnd in (512, 576, 1600, 1664):
                if c0 < bnd < c0 + n:
                    n = bnd - c0
            chunks.append((c0, n))
            c0 += n
        with contextlib.ExitStack() as st:
            win = sbt(st, "win", [128, 8, NCOL], BF16)
            for kt in range(8):
                for pc in range(3):
                    dma("pool", "g_win", win[:, kt, pc * 1112:(pc + 1) * 1112], w_in[kt * 128:(kt + 1) * 128, pc * 1112:(pc + 1) * 1112], (), ["win%d_%d" % (kt, pc)], group="win")
            P.group_end("win", "g_win")
            for kt in range(8):
                P.lastw["win%d" % kt] = P.lastw["win0_0"]
            xtm = [sbt(st, "xtm%d" % i, [128, 4, D]) for i in range(2)]
            xs = sbt(st, "xs", [128, 4, D])
            junk = sbt(st, "junk", [128, D], BF16)
            ssq = sbt(st, "ssq", [128, 4])
            hT = [sbt(st, "hT%d" % i, [128, 8, 512], BF16) for i in range(2)]
            stg = [sbt(st, "stg%d" % i, [128, 512]) for i in range(4)]
            zc = sbt(st, "zc", [128, 1])
            memset("dve", zc[:], 0.0, ["zc"])
            for r0 in range(0, NCOL, 128):
                n = min(128, NCOL - r0)
                dma("sp", "zc", PT[r0:r0 + n, 0:1], zc[0:n, :], ["zc"], [])
            sgi = 0
            for b in range(NB):
                q = b % 2
                src = xp[b * 512:(b + 1) * 512, :] if b < NBP else xo[(b - NBP) * 512:(b - NBP + 1) * 512, :]
                dma("sp", "xtm%d" % q, xtm[q][:], src.rearrange("(s p) d -> p s d", p=128), (), ["xtm%d" % q])
                for s in range(4):
                    act(junk[:], xtm[q][:, s, :], AF.Square, ["xtm%d" % q], ["junk", "ssq"], accum_out=ssq[:, s:s + 1])
                act(ssq[:], ssq[:], AF.Ln, ["ssq"], ["ssq"], bias=1e-6, scale=1.0 / D)
                act(ssq[:], ssq[:], AF.Exp, ["ssq"], ["ssq"], scale=-0.5)
                for s in range(4):
                    ts("dve", xs[:, s, :], xtm[q][:, s, :], ssq[:, s:s + 1], None, ALU.mult, None, ["xtm%d" % q, "ssq"], ["xs"])
                for kt in range(8):
                    pt, pk = nps()
                    for s in range(4):
                        tr(pt[:, s * 128:(s + 1) * 128], xs[:, s, kt * 128:(kt + 1) * 128], ident[:], ["xs", "ident"], [pk], last=(s == 3))
                    if kt % 2 == 0:
                        P.op("act", lambda e, o=hT[q][:, kt, :], i=pt[:], sc=pvc(46 + kt): e.activation(out=o, in_=i, func=AF.Copy, scale=sc),
                             [pk, "pv"], ["hT%d" % q])
                    else:
                        ts("dve", hT[q][:, kt, :], pt[:], pvc(46 + kt), None, ALU.mult, None, [pk, "pv"], ["hT%d" % q])
                for (c0, n) in chunks:
                    if b < NBP and 1792 <= c0 < 2304:
                        continue
                    pt, pk = nps()
                    for kt in range(8):
                        mm(pt[0:n, :], win[:, kt, c0:c0 + n], hT[q][:, kt, :], kt == 0, kt == 7, ["win%d" % kt, "hT%d" % q], [pk])
                    si = sgi % 4
                    sgi += 1
                    cp("act" if si % 2 == 0 else "dve", stg[si][0:n, :], pt[0:n, :], [pk], ["stg%d" % si])
                    dma("sp", "stg%d" % si, PT[c0:c0 + n, 1 + b * 512:1 + (b + 1) * 512], stg[si][0:n, :], ["stg%d" % si], [])
            P.barrier()
        if dbg:
            pass
        if stop == 'A':
            P.barrier()
            P.emit()
            return nc

        with contextlib.ExitStack() as st:
            w2s = sbt(st, "w2s", [64, 512])
            a2s = sbt(st, "a2s", [64, 512])
            g2s = sbt(st, "g2s", [128, 512])
            mask4 = sbt(st, "mask4", [64, 2, 256])
            maskL = sbt(st, "maskL", [64, 2, 64])
            rmask = sbt(st, "rmask", [128, 512])
            for nm, t_, d_ in (("w2s", w2s, w2d), ("a2s", a2s, a2d), ("g2s", g2s, g2d), ("mask4", mask4, mask4d),
                               ("maskL", maskL, maskLd), ("rmask", rmask, rmaskd)):
                dma("sp", "g_B", t_[:], d_, (), [nm], group="B")
            P.group_end("B", "g_B")
            raw = [sbt(st, "raw%d" % i, [128, 513]) for i in range(3)]
            tmp = [sbt(st, "tmp%d" % i, [128, 512]) for i in range(2)]
            names = ["pw_s", "pa_s", "pg_s", "th", "sg", "k_s", "kk", "sq", "rn", "kkn", "a_t", "ka", "sig", "cums", "dcs",
                     "E1", "E2", "E3", "t2"]
            T = {nm: sbt(st, nm, [128, 512]) for nm in names}
            SET = []
            for q in range(2):
                S = {}
                S["AR"] = sbt(st, "AR%d" % q, [128, 8, 2, 64])
                for nm in ("Bt", "Kt", "r_s", "k2", "v_s", "g", "yT"):
                    S[nm] = sbt(st, "%s%d" % (nm, q), [128, 512])
                for nm in ("Btok", "Ktok", "Vtok"):
                    S[nm] = sbt(st, "%s%d" % (nm, q), [64, 8, 128])
                S["Vpad"] = sbt(st, "Vpad%d" % q, [64, 8, 2, 128])
                S["gC"] = sbt(st, "gC%d" % q, [128, 8])
                memset("dve", S["Vpad"][:], 0.0, ["Vpad%d" % q])
                SET.append(S)
            CH = []
            for cq in range(2):
                Cq = {}
                Cq["G1"] = sbt(st, "G1%d" % cq, [64, 2, 256])
                Cq["Lm"] = sbt(st, "Lm%d" % cq, [64, 2, 64])
                Cq["NP"] = [sbt(st, "NP%d_%d" % (cq, i), [64, 2, 128]) for i in range(2)]
                Cq["LL"] = [sbt(st, "LL%d_%d" % (cq, i), [64, 2, 64]) for i in range(2)]
                Cq["TT"] = sbt(st, "TTm%d" % cq, [64, 2, 64])
                Cq["Xs"] = sbt(st, "Xs%d" % cq, [64, 128])
                Cq["Us"] = sbt(st, "Us%d" % cq, [64, 128])
                Cq["Upad"] = sbt(st, "Upad%d" % cq, [64, 2, 128])
                memset("dve", Cq["Upad"][:], 0.0, ["Upad%d" % cq])
                CH.append(Cq)
            STbd = sbt(st, "STbd", [128, 128])
            post = {nm: sbt(st, "po_" + nm, [128, 512]) for nm in ("yc", "sq", "rs", "yn", "rk", "bon")}
            outb = sbt(st, "outb", [128, 512], BF16)
            rawi = [0]

            def shift_tile(row0, n, mucol, b, dst, dkey):
                ri = rawi[0] % 3
                rawi[0] += 1
                rk_ = "raw%d" % ri
                dma("sp", rk_, raw[ri][0:n, :], PT[row0:row0 + n, b * 512:b * 512 + 513], (), [rk_])
                tq = ri % 2
                P.op("act", lambda e, o=tmp[tq][0:n, :], i=raw[ri][0:n, 0:512], sc=pvc(mucol, n): e.activation(out=o, in_=i, func=AF.Copy, scale=sc),
                     [rk_, "pv"], ["tmp%d" % tq])
                stt(dst[0:n, :], raw[ri][0:n, 1:513], pv1c(mucol, n), tmp[tq][0:n, :], ALU.mult, ALU.add,
                    [rk_, "pv1", "tmp%d" % tq], [dkey])

            import os as _os3
            for _ in range(int(_os3.environ.get("PSSHIFT", "0"))):
                nps()
            for j in range(int(_os3.environ.get("JSTART", "0")), 4):
                memset("dve", STbd[:], 0.0, ["STbd"])
                for b in range(NB):
                    q = b % 2
                    S = SET[q]
                    sk = lambda nm: "%s%d" % (nm, q)
                    own = b >= NBP
                    shift_tile(512, 64, 4, b, T["pw_s"], "pw_s")
                    shift_tile(1600, 64, 13, b, T["pa_s"], "pa_s")
                    shift_tile(1664, 128, 14, b, T["pg_s"], "pg_s")
                    shift_tile(j * 128, 128, j, b, S["r_s"], sk("r_s"))
                    shift_tile(576 + j * 128, 128, 5 + j, b, T["k_s"], "k_s")
                    shift_tile(1088 + j * 128, 128, 9 + j, b, S["v_s"], sk("v_s"))
                    act(T["th"][0:64, :], T["pw_s"][0:64, :], AF.Tanh, ["pw_s"], ["th"])
                    pt, pk = nps()
                    mm(pt[:], w2s[0:64, j * 128:(j + 1) * 128], T["th"][0:64, :], True, True, ["w2s", "th"], [pk])
                    act(T["sig"][:], pt[:], AF.Sigmoid, [pk, "pv"], ["sig"], bias=pvc(15 + j))
                    pt, pk = nps()
                    mm(pt[:], a2s[0:64, j * 128:(j + 1) * 128], T["pa_s"][0:64, :], True, True, ["a2s", "pa_s"], [pk])
                    act(T["a_t"][:], pt[:], AF.Sigmoid, [pk, "pv"], ["a_t"], bias=pvc(19 + j))
                    act(T["sg"][:], T["pg_s"][:], AF.Sigmoid, ["pg_s"], ["sg"])
                    pt, pk = nps()
                    mm(pt[:], g2s[:, j * 128:(j + 1) * 128], T["sg"][:], True, True, ["g2s", "sg"], [pk])
                    cp("dve", S["g"][:], pt[:], [pk], [sk("g")])
                    ts("dve", T["kk"][:], T["k_s"][:], pvc(23 + j), None, ALU.mult, None, ["k_s", "pv"], ["kk"])
                    tt("dve", T["sq"][:], T["kk"][:], T["kk"][:], ALU.mult, ["kk"], ["sq"])
                    pt, pk = nps()
                    mm(pt[:], bdm[:], T["sq"][:], True, True, ["bdm", "sq"], [pk])
                    act(T["rn"][:], pt[:], AF.Ln, [pk], ["rn"], bias=1e-16)
                    act(T["rn"][:], T["rn"][:], AF.Exp, ["rn"], ["rn"], scale=-0.5)
                    tt("dve", T["kkn"][:], T["kk"][:], T["rn"][:], ALU.mult, ["kk", "rn"], ["kkn"])
                    ts("dve", T["ka"][:], T["a_t"][:], pvc(27 + j), pv1c(27 + j), ALU.mult, ALU.add, ["a_t", "pv", "pv1"], ["ka"])
                    tt("dve", S["k2"][:], T["k_s"][:], T["ka"][:], ALU.mult, ["k_s", "ka"], [sk("k2")])
                    P.op("dve", lambda e, o=T["cums"][:], m=rmask[:], d1=T["sig"][:]: e.tensor_tensor_scan(out=o, data0=m, data1=d1, initial=0.0, op0=ALU.mult, op1=ALU.add),
                         ["rmask", "sig"], ["cums"])
                    tt("dve", T["dcs"][:], T["cums"][:], T["sig"][:], ALU.subtract, ["cums", "sig"], ["dcs"])
                    act(T["E1"][:], T["cums"][:], AF.Exp, ["cums"], ["E1"], scale=DECAY_C)
                    act(T["E2"][:], T["cums"][:], AF.Exp, ["cums"], ["E2"], scale=-DECAY_C)
                    act(T["E3"][:], T["dcs"][:], AF.Exp, ["dcs"], ["E3"], scale=-DECAY_C)
                    stt(S["AR"][:, :, 0, :], T["kkn"][:].rearrange("p (c t) -> p c t", t=64), -1.0,
                        T["E3"][:].rearrange("p (c t) -> p c t", t=64), ALU.mult, ALU.mult, ["kkn", "E3"], [sk("AR")])
                    tt("dve", S["AR"][:, :, 1, :], S["r_s"][:].rearrange("p (c t) -> p c t", t=64),
                       T["E2"][:].rearrange("p (c t) -> p c t", t=64), ALU.mult, [sk("r_s"), "E2"], [sk("AR")])
                    tt("dve", T["t2"][:], T["kkn"][:], T["a_t"][:], ALU.mult, ["kkn", "a_t"], ["t2"])
                    tt("dve", S["Bt"][:], T["t2"][:], T["E1"][:], ALU.mult, ["t2", "E1"], [sk("Bt")])
                    tt("dve", S["Kt"][:], S["k2"][:], T["E1"][:], ALU.mult, [sk("k2"), "E1"], [sk("Kt")])
                    cp("dve", S["gC"][:], T["E2"][:].rearrange("p (c t) -> p c t", t=64)[:, :, 63], ["E2"], [sk("gC")])
                    for nm_src, nm_dst in (("Bt", "Btok"), ("Kt", "Ktok"), ("v_s", "Vtok")):
                        for half in range(2):
                            pt, pk = nps()
                            for c4 in range(4):
                                c = half * 4 + c4
                                tr(pt[0:64, c4 * 128:(c4 + 1) * 128], S[nm_src][:, c * 64:(c + 1) * 64], ident[:], [sk(nm_src), "ident"], [pk], last=(c4 == 3))
                            cp("dve" if half == 0 else "dve", S[nm_dst][:, half * 4:(half + 1) * 4, :],
                               pt[0:64, :].rearrange("p (c f) -> p c f", f=128), [pk], [sk(nm_dst)])
                    for h in range(2):
                        cp("dve", S["Vpad"][:, :, h, h * 64:(h + 1) * 64], S["Vtok"][:, :, h * 64:(h + 1) * 64], [sk("Vtok")], [sk("Vpad")])
                    if stop is not None and stop.startswith("Bk") and [int(v) for v in stop[2:].split("_")] == [j, b, -1]:
                        P.barrier()
                        P.emit()
                        return nc
                    if stop == 'B1':
                        P.barrier()
                        P.emit()
                        return nc
                    for c in range(8):
                        cq = c % 2
                        Cq = CH[cq]
                        ck = lambda nm: "%s%d" % (nm, cq)
                        csl = slice(c * 64, (c + 1) * 64)
                        pg_, pgk = nps()
                        pl_, plk = nps()
                        for h in range(2):
                            hs = slice(h * 64, (h + 1) * 64)
                            mm(pg_[0:64, h * 256:h * 256 + 128], S["Bt"][hs, csl], S["AR"][hs, c, :, :], True, True, [sk("Bt"), sk("AR")], [pgk])
                            mm(pg_[0:64, h * 256 + 128:h * 256 + 256], S["Kt"][hs, csl], S["AR"][hs, c, :, :], True, True, [sk("Kt"), sk("AR")], [pgk])
                            mm(pl_[0:64, h * 64:(h + 1) * 64], S["AR"][hs, c, 0, :], S["Bt"][hs, csl], True, True, [sk("AR"), sk("Bt")], [plk])
                        tt("dve", Cq["G1"][:], pg_[0:64, :].rearrange("p (h f) -> p h f", h=2), mask4[:], ALU.mult, [pgk, "mask4"], [ck("G1")])
                        tt("dve", Cq["Lm"][:], pl_[0:64, 0:128].rearrange("p (h f) -> p h f", h=2), maskL[:], ALU.mult, [plk, "maskL"], [ck("Lm")])
                        if stop == 'G':
                            P.barrier()
                            P.emit()
                            return nc
                        NPc, LLc = Cq["NP"], Cq["LL"]
                        pd, pdk = nps()
                        for h in range(2):
                            mm(pd[0:64, h * 192:h * 192 + 64], Cq["Lm"][:, h, :], Cq["G1"][:, h, 0:64], True, True, [ck("Lm"), ck("G1")], [pdk])
                            mm(pd[0:64, h * 192 + 128:h * 192 + 192], Cq["G1"][:, h, 0:64], Cq["Lm"][:, h, :], True, True, [ck("Lm"), ck("G1")], [pdk])
                        pdv = pd[0:64, 0:384].rearrange("p (h f) -> p h f", h=2)
                        cp("dve", NPc[1][:, :, 0:64], pdv[:, :, 0:64], [pdk], [ck("NP1")])
                        cp("dve", LLc[1][:], pdv[:, :, 128:192], [pdk], [ck("LL1")])
                        for h in range(2):
                            tt("dve", NPc[1][:, h, 64:128], Cq["G1"][:, h, 0:64], ident[0:64, 0:64], ALU.add, [ck("G1"), "ident"], [ck("NP1")])
                        for i in range(1, 6):
                            cur, nxt = i % 2, (i + 1) % 2
                            pd, pdk = nps()
                            for h in range(2):
                                mm(pd[0:64, h * 192:h * 192 + 128], LLc[cur][:, h, :], NPc[cur][:, h, :], True, True,
                                   [ck("LL%d" % cur), ck("NP%d" % cur)], [pdk])
                                if i < 5:
                                    mm(pd[0:64, h * 192 + 128:h * 192 + 192], NPc[cur][:, h, 0:64], LLc[cur][:, h, :], True, True,
                                       [ck("LL%d" % cur), ck("NP%d" % cur)], [pdk])
                            pdv = pd[0:64, 0:384].rearrange("p (h f) -> p h f", h=2)
                            if i < 5:
                                cp("dve", NPc[nxt][:, :, 0:64], pdv[:, :, 0:64], [pdk], [ck("NP%d" % nxt)])
                                cp("dve", LLc[nxt][:], pdv[:, :, 128:192], [pdk], [ck("LL%d" % nxt)])
                                tt("dve", NPc[nxt][:, :, 64:128], pdv[:, :, 64:128], NPc[cur][:, :, 64:128], ALU.add,
                                   [pdk, ck("NP%d" % cur)], [ck("NP%d" % nxt)])
                            else:
                                tt("dve", Cq["TT"][:], pdv[:, :, 64:128], NPc[cur][:, :, 64:128], ALU.add, [pdk, ck("NP%d" % cur)], [ck("TT")])
                        if stop == 'B2a':
                            P.barrier()
                            P.emit()
                            return nc
                        px, pxk = nps()
                        mm(px[0:64, 0:128], S["AR"][:, c, 0, :], STbd[:], True, False, [sk("AR"), "STbd"], [pxk])
                        for h in range(2):
                            mm(px[0:64, h * 64:(h + 1) * 64], Cq["G1"][:, h, 128:192], S["Vtok"][:, c, h * 64:(h + 1) * 64], False, h == 1,
                               [ck("G1"), sk("Vtok")], [pxk])
                        cp("dve", Cq["Xs"][:], px[0:64, 0:128], [pxk], [ck("Xs")])
                        pu, puk = nps()
                        for h in range(2):
                            mm(pu[0:64, h * 64:(h + 1) * 64], Cq["TT"][:, h, :], Cq["Xs"][:, h * 64:(h + 1) * 64], True, True, [ck("TT"), ck("Xs")], [puk])
                        cp("dve", Cq["Us"][:], pu[0:64, 0:128], [puk], [ck("Us")])
                        import os as _os
                        OM = _os.environ.get("OWNMODE", "all")
                        if own and OM != "none":
                            for h in range(2):
                                cp("dve", Cq["Upad"][:, h, h * 64:(h + 1) * 64], Cq["Us"][:, h * 64:(h + 1) * 64], [ck("Us")], [ck("Upad")])
                            if OM != "upad":
                                py, pyk = nps()
                                if OM == "y1b":
                                    mm(py[:, 0:128], STbd[:], S["AR"][:, c, :, :], True, True, ["STbd", sk("AR")], [pyk])
                                    cp("dve", S["yT"][:, csl], py[:, 64:128], [pyk], [sk("yT")])
                                    continue
                                if OM == "y1c":
                                    mm(py[:, 0:64], STbd[:], S["AR"][:, c, 1, :], True, True, ["STbd", sk("AR")], [pyk])
                                    cp("dve", S["yT"][:, csl], py[:, 0:64], [pyk], [sk("yT")])
                                    continue
                                mm(py[:, 0:64], STbd[:], S["AR"][:, c, 1, :], True, OM == "y1", ["STbd", sk("AR")], [pyk])
                                if OM != "y1":
                                    for h in range(2):
                                        mm(py[:, 0:64], Cq["Upad"][:, h, :], Cq["G1"][:, h, 64:128], False, OM == "y2" and h == 1, [ck("Upad"), ck("G1")], [pyk])
                                        if OM != "y2":
                                            mm(py[:, 0:64], S["Vpad"][:, c, h, :], Cq["G1"][:, h, 192:256], False, h == 1, [sk("Vpad"), ck("G1")], [pyk])
                                cp("dve", S["yT"][:, csl], py[:, 0:64], [pyk], [sk("yT")])
                        pss, psk = nps()
                        mm(pss[:, 0:128], S["Btok"][:, c, :], Cq["Us"][:], True, False, [sk("Btok"), ck("Us")], [psk])
                        mm(pss[:, 0:128], S["Ktok"][:, c, :], S["Vtok"][:, c, :], False, False, [sk("Ktok"), sk("Vtok")], [psk])
                        mm(pss[:, 0:128], ident[:], STbd[:], False, True, ["ident", "STbd"], [psk])
                        stt(STbd[:], pss[:, 0:128], S["gC"][:, c:c + 1], bdm[:], ALU.mult, ALU.mult, [psk, sk("gC"), "bdm"], ["STbd"])
                        if _os3.environ.get("CHBAR", "0") == "1":
                            P.barrier()
                        if stop is not None and stop.startswith("Bk") and [int(v) for v in stop[2:].split("_")] == [j, b, c]:
                            P.barrier()
                            P.emit()
                            return nc
                    if stop == 'B2c' and own:
                        P.barrier()
                        P.emit()
                        return nc
                    if stop == 'B2b':
                        P.barrier()
                        P.emit()
                        return nc
                    if own:
                        pm, pmk = nps()
                        mm(pm[:], bdm[:], S["yT"][:], True, True, ["bdm", sk("yT")], [pmk])
                        stt(post["yc"][:], pm[:], -1.0 / 64, S["yT"][:], ALU.mult, ALU.add, [pmk, sk("yT")], ["po_yc"])
                        tt("dve", post["sq"][:], post["yc"][:], post["yc"][:], ALU.mult, ["po_yc"], ["po_sq"])
                        pm, pmk = nps()
                        mm(pm[:], bdm[:], post["sq"][:], True, True, ["bdm", "po_sq"], [pmk])
                        act(post["rs"][:], pm[:], AF.Ln, [pmk], ["po_rs"], bias=64e-5, scale=1.0 / 64)
                        act(post["rs"][:], post["rs"][:], AF.Exp, ["po_rs"], ["po_rs"], scale=-0.5)
                        tt("dve", post["yn"][:], post["yc"][:], post["rs"][:], ALU.mult, ["po_yc", "po_rs"], ["po_yn"])
                        ts("dve", post["yn"][:], post["yn"][:], pvc(35 + j), pvc(39 + j), ALU.mult, ALU.add, ["po_yn", "pv"], ["po_yn"])
                        stt(post["rk"][:], S["r_s"][:], pvc(31 + j), S["k2"][:], ALU.mult, ALU.mult, [sk("r_s"), sk("k2"), "pv"], ["po_rk"])
                        pm, pmk = nps()
                        mm(pm[:], bdm[:], post["rk"][:], True, True, ["bdm", "po_rk"], [pmk])
                        tt("dve", post["bon"][:], pm[:], S["v_s"][:], ALU.mult, [pmk, sk("v_s")], ["po_bon"])
                        tt("dve", post["bon"][:], post["bon"][:], post["yn"][:], ALU.add, ["po_bon", "po_yn"], ["po_bon"])
                        tt("dve", outb[:], post["bon"][:], S["g"][:], ALU.mult, ["po_bon", sk("g")], ["outb"])
                        bo = b - NBP
                        dma("sp", "outb", YT[j * 128:(j + 1) * 128, bo * 512:(bo + 1) * 512], outb[:], ["outb"], [])
                    if _os3.environ.get("BLKBAR") == "1":
                        P.barrier()
                    if stop is not None and stop.startswith("Bm") and [int(v) for v in stop[2:].split("_")] == [j, b]:
                        P.barrier()
                        P.emit()
                        return nc
                    if stop is not None and stop.startswith("Bn") and int(stop[2:]) == b:
                        P.barrier()
                        P.emit()
                        return nc
                if stop is not None and stop.startswith("Bj") and int(stop[2:]) == j:
                    P.barrier()
                    P.emit()
                    return nc
            P.barrier()

        if stop == 'B':
            P.barrier()
            P.emit()
            return nc
        with contextlib.ExitStack() as st:
            nmk = sbt(st, "nmk", [128, 4, 512])
            sel127 = sbt(st, "sel127", [128, 128])
            kbs = sbt(st, "kbs", [128, NTL])
            dma("sp", "g_C", nmk[:], nmaskd, (), ["nmk"], group="C")
            dma("sp", "g_C", sel127[:], sel127d, (), ["sel127"], group="C")
            dma("sp", "g_C", kbs[:], kbd, (), ["kbs"], group="C")
            fT = sbt(st, "fT", [8, TT])
            cnT = sbt(st, "cnT", [8, TT])
            cntok = sbt(st, "cntok", [128, NTL, 8])
            cref = sbt(st, "cref", [128, 8])
            biasq = sbt(st, "biasq", [128, NTL, 8])
            qahi = sbt(st, "qahi", [8, TO], BF16)
            qalo = sbt(st, "qalo", [8, TO], BF16)
            KTa = sbt(st, "KTa", [66, TT], BF16)
            QTa = sbt(st, "QTa", [66, TO], BF16)
            Vx = sbt(st, "Vx", [128, NTL, 65], BF16)
            ld = [sbt(st, "ld%d" % i, [64, 512]) for i in range(2)]
            sq2 = sbt(st, "sq2", [64, 512])
            rn2 = sbt(st, "rn2", [64, 512])
            Pt = [sbt(st, "Pt%d" % i, [128, 512], BF16) for i in range(3)]
            mt = [sbt(st, "mt%d" % i, [128, 512]) for i in range(2)]
            osb = sbt(st, "osb", [65, 512])
            ones65 = sbt(st, "ones65", [65, 64])
            yfx = sbt(st, "yfx", [64, 512], BF16)
            memset("dve", ones65[:], 1.0, ["ones65"])
            memset("dve", Vx[:], 1.0, ["Vx"])
            memset("dve", KTa[64:66, :], 1.0, ["KTa"])
            dma("sp", "g_C", fT[:], PT[3328:3336, 1:TT + 1], (), ["fT"], group="C")
            P.group_end("C", "g_C")
            nbf = sbt(st, "nbf", [8, 1])
            ts("dve", nbf[:], pv[0:8, 45:46], -1.0, None, ALU.mult, None, ["pv"], ["nbf"])
            act(fT[:], fT[:], AF.Exp, ["fT", "nbf"], ["fT"], bias=nbf[:, 0:1], scale=-1.0)
            act(fT[:], fT[:], AF.Ln, ["fT"], ["fT"], bias=1.0)
            P.op("dve", lambda e: e.tensor_tensor_scan(out=cnT[:], data0=ones65[0:8, 0:1].to_broadcast([8, TT]), data1=fT[:], initial=0.0, op0=ALU.mult, op1=ALU.add),
                 ["fT", "ones65"], ["cnT"])
            for g0 in range(0, NTL, 64):
                gn = min(64, NTL - g0)
                pt, pk = nps()
                for tl in range(gn):
                    tr(pt[:, tl * 8:(tl + 1) * 8], cnT[0:8, (g0 + tl) * 128:(g0 + tl + 1) * 128], ident[0:8, 0:8], ["cnT", "ident"], [pk], last=(tl == gn - 1))
                cp("act", cntok[:, g0:g0 + gn, :], pt[:, 0:gn * 8].rearrange("p (t h) -> p t h", h=8), [pk], ["cntok"])
            for h8 in range(8):
                tt("dve", cntok[:, :, h8], cntok[:, :, h8], kbs[:], ALU.add, ["cntok", "kbs"], ["cntok"])
            qa32 = fT[:, 0:TO]
            for jb in range(NBO):
                e_ = TP + (jb + 1) * 512 - 1
                ts("dve", fT[:, jb * 512:(jb + 1) * 512], cnT[0:8, TP + jb * 512:TP + (jb + 1) * 512], cnT[0:8, e_:e_ + 1], -8.0,
                   ALU.subtract, ALU.mult, ["cnT", "fT"], ["qa32", "fT"])
            cp("dve", qahi[:], qa32, ["qa32"], ["qahi"])
            tt("dve", qalo[:], qa32, qahi[:], ALU.subtract, ["qa32", "qahi"], ["qalo"])
            pti = [0]
            mti = [0]
            for hf in range(8):
                dma("sp", "qa_hi", QTa[64:65, :], qahi[hf:hf + 1, :], ["qahi"], ["QTa"])
                dma("sp", "qa_lo", QTa[65:66, :], qalo[hf:hf + 1, :], ["qalo"], ["QTa"])
                for kind, row0, dst, dk, nblk, toff, wcol in (("k", 2304 + hf * 64, KTa, "KTa", NB, 0, 44),
                                                              ("q", 1792 + hf * 64, QTa, "QTa", NBO, TP, 43)):
                    for b in range(nblk):
                        q = b % 2
                        lk = "ld%d" % q
                        dma("sp", lk, ld[q][:], PT[row0:row0 + 64, 1 + toff + b * 512:1 + toff + (b + 1) * 512], (), [lk])
                        tt("dve", sq2[:], ld[q][:], ld[q][:], ALU.mult, [lk], ["sq2"])
                        pt, pk = nps()
                        mm(pt[0:64, :], bdm[0:64, 0:64], sq2[:], True, True, ["bdm", "sq2"], [pk])
                        act(rn2[:], pt[0:64, :], AF.Ln, [pk], ["rn2"], bias=1e-6, scale=1.0 / 64)
                        act(rn2[:], rn2[:], AF.Exp, ["rn2"], ["rn2"], scale=-0.5)
                        stt(dst[0:64, b * 512:(b + 1) * 512], ld[q][:], pvc(wcol, 64), rn2[:], ALU.mult, ALU.mult, [lk, "pv", "rn2"], [dk])
                for b in range(NB):
                    q = b % 2
                    lk = "ld%d" % q
                    dma("sp", lk, ld[q][:], PT[2816 + hf * 64:2816 + (hf + 1) * 64, 1 + b * 512:1 + (b + 1) * 512], (), [lk])
                    pt, pk = nps()
                    for s in range(4):
                        tr(pt[:, s * 64:(s + 1) * 64], ld[q][:, s * 128:(s + 1) * 128], ident[0:64, 0:64], [lk, "ident"], [pk], last=(s == 3))
                    cp("act", Vx[:, b * 4:(b + 1) * 4, 0:64], pt[:, 0:256].rearrange("p (s d) -> p s d", s=4), [pk], ["Vx"])
                for jb in range(NBO):
                    nkt = (TP + (jb + 1) * 512) // 128
                    pt, pk = nps()
                    mm(pt[:, 0:8], sel127[:], cntok[:, nkt - 1, :], True, True, ["sel127", "cntok"], [pk])
                    cp("act", cref[:], pt[:, 0:8], [pk], ["cref"])
                    ts("dve", biasq[:, 0:nkt, hf], cntok[:, 0:nkt, hf], cref[:, hf:hf + 1], None, ALU.subtract, None, ["cntok", "cref"], ["biasq"])
                    po, pok = nps()
                    for kt in range(nkt):
                        psc, psk_ = nps()
                        if psk_ == pok:
                            psc, psk_ = nps()
                        mm(psc[:], KTa[0:66, kt * 128:(kt + 1) * 128], QTa[0:66, jb * 512:(jb + 1) * 512], True, True, ["KTa", "QTa"], [psk_])
                        pi = pti[0] % 3
                        pti[0] += 1
                        pkk = "Pt%d" % pi
                        o = kt - (nkt - 4)
                        if o >= 0:
                            mi = mti[0] % 2
                            mti[0] += 1
                            tt("dve", mt[mi][:], psc[:], nmk[:, o, :], ALU.add, [psk_, "nmk"], ["mt%d" % mi])
                            act(Pt[pi][:], mt[mi][:], AF.Exp, ["mt%d" % mi, "biasq"], [pkk], bias=biasq[:, kt, hf:hf + 1], scale=0.125)
                        else:
                            act(Pt[pi][:], psc[:], AF.Exp, [psk_, "biasq"], [pkk], bias=biasq[:, kt, hf:hf + 1], scale=0.125)
                        mm(po[0:65, :], Vx[:, kt, :], Pt[pi][:], kt == 0, kt == nkt - 1, ["Vx", pkk], [pok])
                    cp("act", osb[:], po[0:65, :], [pok], ["osb"])
                    P.op("dve", lambda e: e.reciprocal(out=osb[64:65, :], in_=osb[64:65, :]), ["osb"], ["osb"])
                    pt, pk = nps()
                    mm(pt[0:64, :], ones65[64:65, :], osb[64:65, :], True, True, ["ones65", "osb"], [pk])
                    tt("dve", yfx[:], osb[0:64, :], pt[0:64, :], ALU.mult, ["osb", pk], ["yfx"])
                    dma("sp", "yfx", YT[512 + hf * 64:512 + (hf + 1) * 64, jb * 512:(jb + 1) * 512], yfx[:], ["yfx"], [])
            P.barrier()
        if stop == 'C':
            P.barrier()
            P.emit()
            return nc
        TH = min(2048, TO)
        NH = TO // TH
        NTH = TH // 128
        with contextlib.ExitStack() as st:
            wo = sbt(st, "wo", [128, 8, D], BF16)
            for c in range(8):
                dma("pool", "g_D", wo[:, c, :], w_out[c * 128:(c + 1) * 128, :], (), ["wo%d" % c], group="D")
            rw32 = sbt(st, "rw32", [128, 8, 72])
            dma("pool", "g_D", rw32[:, :, 0:8], rgw.rearrange("(kt p) g -> p kt g", p=128), (), ["rw32a"], group="D")
            dma("pool", "g_D", rw32[:, :, 8:72], rew.rearrange("(kt p) g -> p kt g", p=128), (), ["rw32b"], group="D")
            rbs = sbt(st, "rbs", [1, 72])
            dma("pool", "g_D", rbs[:], rbd, (), ["rbs"], group="D")
            P.group_end("D", "g_D")
            ones1 = sbt(st, "ones1", [1, 128])
            memset("dve", ones1[:], 1.0, ["ones1"])
            acc = sbt(st, "acc", [128, NTH, D])
            h2T = sbt(st, "h2T", [128, 8, TH], BF16)
            gates = sbt(st, "gates", [128, NTH, 64])
            ym = [sbt(st, "ym%d" % i, [128, 8, 128], BF16) for i in range(2)]
            xt = [sbt(st, "xt%d" % i, [128, D]) for i in range(2)]
            xs2 = sbt(st, "xs2", [128, D])
            junk2 = sbt(st, "junk2", [128, D], BF16)
            ss2 = sbt(st, "ss2", [128, 1])
            h32 = sbt(st, "h32", [128, 8, 128])
            lg = sbt(st, "lg", [128, 72])
            sm = {nm: sbt(st, "sm_" + nm, [128, 8]) for nm in ("gm", "goh", "ge", "sel", "m1", "k1", "s2", "k2", "gl")}
            sc1 = {nm: sbt(st, "sc_" + nm, [128, 1]) for nm in ("gmax", "gsum", "m1", "m2", "w1", "w2")}
            e3 = sbt(st, "e3", [128, 8, 8])
            wg = [sbt(st, "wg%d" % i, [128, 8, 512], BF16) for i in range(2)]
            wu = [sbt(st, "wu%d" % i, [128, 8, 512], BF16) for i in range(2)]
            wdn = [sbt(st, "wdn%d" % i, [128, 4, D], BF16) for i in range(2)]
            sgl = [sbt(st, "sgl%d" % i, [128, 512]) for i in range(2)]
            hid = [sbt(st, "hid%d" % i, [128, 4, 512], BF16) for i in range(2)]
            for hh in range(NH):
                for i in range(NTH):
                    q = i % 2
                    tok0 = hh * TH + i * 128
                    dma("sp", "ym%d" % q, ym[q][:], YT[:, tok0:tok0 + 128].rearrange("(c p) t -> p c t", p=128), (), ["ym%d" % q])
                    dma("sp", "xt%d" % q, xt[q][:], xo[tok0:tok0 + 128, :], (), ["xt%d" % q])
                    for nh in range(2):
                        pt, pk = nps()
                        ns = slice(nh * 512, (nh + 1) * 512)
                        for c in range(8):
                            mm(pt[:], ym[q][:, c, :], wo[:, c, ns], c == 0, c == 7, ["ym%d" % q, "wo%d" % c], [pk])
                        tt("dve", acc[:, i, ns], pt[:], xt[q][:, ns], ALU.add, [pk, "xt%d" % q], ["acc%d" % i])
                    ak = "acc%d" % i
                    act(junk2[:], acc[:, i, :], AF.Square, [ak], ["junk2", "ss2"], accum_out=ss2[:, 0:1])
                    act(ss2[:], ss2[:], AF.Ln, ["ss2"], ["ss2"], bias=1e-6, scale=1.0 / D)
                    act(ss2[:], ss2[:], AF.Exp, ["ss2"], ["ss2"], scale=-0.5)
                    ts("dve", xs2[:], acc[:, i, :], ss2[:, 0:1], None, ALU.mult, None, [ak, "ss2"], ["xs2"])
                    for half in range(2):
                        pt, pk = nps()
                        for k4 in range(4):
                            kt = half * 4 + k4
                            tr(pt[:, k4 * 128:(k4 + 1) * 128], xs2[:, kt * 128:(kt + 1) * 128], ident[:], ["xs2", "ident"], [pk], last=(k4 == 3))
                        for k4 in range(4):
                            kt = half * 4 + k4
                            if k4 % 2 == 0:
                                P.op("act", lambda e, o=h32[:, kt, :], i_=pt[:, k4 * 128:(k4 + 1) * 128], sc=pvc(54 + kt): e.activation(out=o, in_=i_, func=AF.Copy, scale=sc),
                                     [pk, "pv"], ["h32"])
                            else:
                                ts("dve", h32[:, kt, :], pt[:, k4 * 128:(k4 + 1) * 128], pvc(54 + kt), None, ALU.mult, None, [pk, "pv"], ["h32"])
                    cp("dve", h2T[:, :, i * 128:(i + 1) * 128], h32[:], ["h32"], ["h2T"])
                    pt, pk = nps()
                    for kt in range(8):
                        mm(pt[:, 0:72], h32[:, kt, :], rw32[:, kt, :], kt == 0, False, ["h32", "rw32a", "rw32b"], [pk])
                    mm(pt[:, 0:72], ones1[0:1, :], rbs[0:1, :], False, True, ["ones1", "rbs"], [pk])
                    cp("act", lg[:], pt[:, 0:72], [pk], ["lg"])
                    R = ["lg"]
                    W = ["rt"]
                    P.op("dve", lambda e: e.reduce_max(out=sc1["gmax"][:], in_=lg[:, 0:8], axis=mybir.AxisListType.X), R, W)
                    ts("dve", sm["goh"][:], lg[:, 0:8], sc1["gmax"][:, 0:1], None, ALU.is_equal, None, R + W, W)
                    ts("dve", sm["gm"][:], lg[:, 0:8], sc1["gmax"][:, 0:1], None, ALU.subtract, None, R + W, W)
                    act(sm["ge"][:], sm["gm"][:], AF.Exp, W, W, accum_out=sc1["gsum"][:, 0:1])
                    P.op("dve", lambda e: e.reciprocal(out=sc1["gsum"][:], in_=sc1["gsum"][:]), ["rt"], ["rt"])
                    tt("dve", e3[:], lg[:, 8:72].rearrange("p (g e) -> p g e", e=8), sm["goh"][:].unsqueeze(2).to_broadcast([128, 8, 8]), ALU.mult, R + W, W)
                    P.op("dve", lambda e: e.reduce_sum(out=sm["sel"][:], in_=e3[:].rearrange("p g e -> p e g"), axis=mybir.AxisListType.X), W, W)
                    P.op("dve", lambda e: e.reduce_max(out=sc1["m1"][:], in_=sm["sel"][:], axis=mybir.AxisListType.X), W, W)
                    ts("dve", sm["k1"][:], sm["sel"][:], sc1["m1"][:, 0:1], None, ALU.is_equal, None, W, W)
                    stt(sm["s2"][:], sm["k1"][:], -1e30, sm["sel"][:], ALU.mult, ALU.add, W, W)
                    P.op("dve", lambda e: e.reduce_max(out=sc1["m2"][:], in_=sm["s2"][:], axis=mybir.AxisListType.X), W, W)
                    ts("dve", sm["k2"][:], sm["s2"][:], sc1["m2"][:, 0:1], None, ALU.is_equal, None, W, W)
                    tt("dve", sc1["w1"][:], sc1["m1"][:], sc1["m2"][:], ALU.subtract, W, W)
                    act(sc1["w1"][:], sc1["w1"][:], AF.Sigmoid, W, ["rt"])
                    ts("dve", sc1["w2"][:], sc1["w1"][:], -1.0, 1.0, ALU.mult, ALU.add, ["rt"], W)
                    tt("dve", sc1["w1"][:], sc1["w1"][:], sc1["gsum"][:], ALU.mult, W, W)
                    tt("dve", sc1["w2"][:], sc1["w2"][:], sc1["gsum"][:], ALU.mult, W, W)
                    ts("dve", sm["gl"][:], sm["k1"][:], sc1["w1"][:, 0:1], None, ALU.mult, None, W, W)
                    stt(sm["gl"][:], sm["k2"][:], sc1["w2"][:, 0:1], sm["gl"][:], ALU.mult, ALU.add, W, W)
                    tt("dve", gates[:, i, :].rearrange("p (g e) -> p g e", e=8), sm["goh"][:].unsqueeze(2).to_broadcast([128, 8, 8]),
                       sm["gl"][:].unsqueeze(1).to_broadcast([128, 8, 8]), ALU.mult, W, ["gates"])
                for ex in range(n_exp):
                    q = ex % 2
                    dma("pool", "wg%d" % q, wg[q][:], wgd[ex].rearrange("(kt p) f -> p kt f", p=128), (), ["wg%d" % q])
                    dma("pool", "wu%d" % q, wu[q][:], wud[ex].rearrange("(kt p) f -> p kt f", p=128), (), ["wu%d" % q])
                    dma("pool", "wdn%d" % q, wdn[q][:], wdd[ex].rearrange("(fc p) n -> p fc n", p=128), (), ["wdn%d" % q])
                    for sb_ in range(TH // 512):
                        hq = sb_ % 2
                        tsl = slice(sb_ * 512, (sb_ + 1) * 512)
                        for fc in range(4):
                            pg_, pgk = nps()
                            pu_, puk = nps()
                            for kt in range(8):
                                mm(pg_[:], wg[q][:, kt, fc * 128:(fc + 1) * 128], h2T[:, kt, tsl], kt == 0, kt == 7, ["wg%d" % q, "h2T"], [pgk])
                            for kt in range(8):
                                mm(pu_[:], wu[q][:, kt, fc * 128:(fc + 1) * 128], h2T[:, kt, tsl], kt == 0, kt == 7, ["wu%d" % q, "h2T"], [puk])
                            sq_ = fc % 2
                            act(sgl[sq_][:], pg_[:], AF.Silu, [pgk], ["sgl%d" % sq_])
                            tt("dve", hid[hq][:, fc, :], sgl[sq_][:], pu_[:], ALU.mult, ["sgl%d" % sq_, puk], ["hid%d_%d" % (hq, fc)])
                        for t4 in range(4):
                            ti = sb_ * 4 + t4
                            for nh in range(2):
                                po, pok = nps()
                                for fc in range(4):
                                    mm(po[:], hid[hq][:, fc, t4 * 128:(t4 + 1) * 128], wdn[q][:, fc, nh * 512:(nh + 1) * 512], fc == 0, fc == 3,
                                       ["hid%d_%d" % (hq, fc), "wdn%d" % q], [pok])
                                stt(acc[:, ti, nh * 512:(nh + 1) * 512], po[:], gates[:, ti, ex:ex + 1], acc[:, ti, nh * 512:(nh + 1) * 512],
                                    ALU.mult, ALU.add, [pok, "gates", "acc%d" % ti], ["acc%d" % ti])
                for i in range(NTH):
                    tok0 = hh * TH + i * 128
                    dma("sp", "ost%d" % (i % 4), out[tok0:tok0 + 128, :], acc[:, i, :], ["acc%d" % i], ["out%d" % i])
                P.barrier()
        P.barrier()
        P.emit()
    return nc


def make_core_inputs(inputs, c, T, consts, pvt):
    b, th = c // 2, c % 2
    TP = TO = T // 2
    x = inputs["x"]
    d = {}
    d["xp"] = np.ascontiguousarray(x[b, 0:TP]) if th == 1 else np.zeros((TP, D), np.float32)
    d["xo"] = np.ascontiguousarray(x[b, th * TO:(th + 1) * TO])
    d["w_in"] = inputs["w_in"][0]
    d["pv"] = pvt
    d["rw_w2"] = inputs["rw_w2"][0]
    d["rw_a2"] = inputs["rw_a2"][0]
    d["rw_g2"] = inputs["rw_g2"][0]
    d["w_out"] = inputs["w_out"][0]
    d["rgw"] = inputs["router_group_w"][0]
    d["rew"] = inputs["router_expert_w"][0]
    d["rb"] = np.concatenate([inputs["router_group_b"][0], inputs["router_expert_b"][0]])[None, :]
    d["wg"] = inputs["exp_w_gate"][0]
    d["wu"] = inputs["exp_w_up"][0]
    d["wd"] = inputs["exp_w_down"][0]
    kb = np.zeros((128, T // 128), np.float32)
    if th == 0:
        kb[:, :TP // 128] = -30000.0
    d["kb"] = kb
    d.update(consts)
    return d


def kernel(**inputs):
    inputs = {k: np.asarray(v) for k, v in inputs.items()}
    x = inputs["x"]
    B, T, _ = x.shape
    TP = TO = T // 2
    consts = host_consts()
    pvt = host_pv(inputs)
    nc = build(TP, TO)
    ncores = 2 * B
    in_maps = [make_core_inputs(inputs, c, T, consts, pvt) for c in range(ncores)]
    res = run_bass_kernel_spmd(nc, in_maps, core_ids=list(range(ncores)))
    out = np.empty((B, T, D), np.float32)
    for c in range(ncores):
        b, th = c // 2, c % 2
        out[b, th * TO:(th + 1) * TO] = res.results[c]["out"]
    return out
```

```python
import contextlib
import numpy as np
import ml_dtypes
import concourse.bass as bass
import concourse.mybir as mybir
from concourse.bass_utils import run_bass_kernel_spmd

F32 = mybir.dt.float32
BF16 = mybir.dt.bfloat16
AF = mybir.ActivationFunctionType
ALU = mybir.AluOpType

ENGS = ("pe", "act", "dve", "pool", "sp")
EPOCH = 1400
D = 1024
NCOL = 3336
NE = 64
DECAY_C = 0.6065306597126334


class Prog:
    def __init__(self, nc):
        self.nc = nc
        self.ops = {e: [] for e in ENGS}
        self.cnt = {}
        self.seen = {e: {} for e in ENGS}
        self.lastw = {}
        self.readers = {}
        self.eng_n = {e: 0 for e in ENGS}
        self.semkeys = []
        self.dma_n = {}
        self.groups = {}

    def _semkey(self, k):
        if k not in self.cnt:
            self.cnt[k] = 0
            self.semkeys.append(k)
        return k

    def _deps(self, eng, reads, writes):
        deps = {}

        def add(tok):
            if tok is None:
                return
            k, v = tok
            if deps.get(k, 0) < v:
                deps[k] = v
        for b in reads:
            add(self.lastw.get(b))
        for b in writes:
            add(self.lastw.get(b))
            for t in self.readers.get(b, ()):
                add(t)
        waits = []
        for k, v in deps.items():
            if eng == "pe" and k[0] == "pe":
                continue
            if self.seen[eng].get(k, 0) >= v:
                continue
            self.seen[eng][k] = v
            waits.append((k, v))
        return waits

    def _commit(self, tok, reads, writes):
        for b in writes:
            self.lastw[b] = tok
            self.readers[b] = []
        for b in reads:
            self.readers.setdefault(b, []).append(tok)

    def op(self, eng, fn, reads=(), writes=(), serial=False, inc=True):
        waits = self._deps(eng, reads, writes)
        k = self._semkey((eng, self.eng_n[eng] // EPOCH))
        if serial and self.cnt[k] > 0 and self.seen[eng].get(k, 0) < self.cnt[k]:
            self.seen[eng][k] = self.cnt[k]
            waits.append((k, self.cnt[k]))
        if inc:
            self.cnt[k] += 1
            tok = (k, self.cnt[k])
            self.ops[eng].append((waits, fn, (k, 1)))
            if self.cnt[k] >= EPOCH:
                self.eng_n[eng] += EPOCH
        else:
            tok = (k, self.cnt[k] + 1)
            self.ops[eng].append((waits, fn, None))
        self._commit(tok, reads, writes)
        return tok

    def dma(self, eng, slot, fn, reads=(), writes=(), group=None):
        waits = self._deps(eng, reads, writes)
        n = self.dma_n.get(slot, 0)
        self.dma_n[slot] = n + 1
        k = self._semkey(("dma", slot, n // 96))
        self.cnt[k] += 16
        tok = (k, self.cnt[k])
        self.ops[eng].append((waits, fn, (k, 16)))
        self._commit(tok, reads, writes)
        if group is not None:
            self.groups.setdefault(group, []).extend(writes)
        return tok

    def group_end(self, group, slot):
        n = self.dma_n[slot]
        k = ("dma", slot, (n - 1) // 96)
        assert (n - 1) // 96 == 0
        for b in self.groups.pop(group, []):
            self.lastw[b] = (k, self.cnt[k])

    def barrier(self, engs=ENGS):
        for e in engs:
            waits = []
            for k in self.semkeys:
                v = self.cnt[k]
                if v == 0 or self.seen[e].get(k, 0) >= v:
                    continue
                if e == "pe" and k[0] == "pe":
                    continue
                self.seen[e][k] = v
                waits.append((k, v))
            if waits:
                self.ops[e].append((waits, None, None))

    def emit(self):
        nc = self.nc
        with contextlib.ExitStack() as st:
            st.enter_context(nc.allow_non_contiguous_dma(reason="small strided setup / layout DMAs"))
            sems = {}
            print("n_sems", len(self.semkeys), {e: len(v) for e, v in self.ops.items()})
            for i, k in enumerate(self.semkeys):
                sems[k] = st.enter_context(nc.semaphore("s%d" % i))
            block = st.enter_context(nc.Block())
            engmap = {"pe": block.tensor, "act": block.scalar, "dve": block.vector,
                      "pool": block.gpsimd, "sp": block.sync}

            def mk(e):
                def body(engine):
                    for waits, fn, inc in self.ops[e]:
                        for (k, v) in waits:
                            engine.wait_ge(sems[k], v)
                        if fn is not None:
                            ins = fn(engine)
                            if inc is not None:
                                ins.then_inc(sems[inc[0]], inc[1])
                return body
            for e in ENGS:
                if self.ops[e]:
                    engmap[e](mk(e))


def host_consts():
    c = {}
    s = np.arange(64)[:, None]
    t = np.arange(64)[None, :]
    strict = (s < t).astype(np.float32)
    incl = (s <= t).astype(np.float32)
    m4 = np.concatenate([strict, incl, strict, incl], axis=1)
    c["mask4"] = np.ascontiguousarray(np.stack([m4, m4], axis=1))
    ml = (t < s).astype(np.float32)
    c["maskL"] = np.ascontiguousarray(np.stack([ml, ml], axis=1))
    bd = np.zeros((128, 128), np.float32)
    bd[:64, :64] = 1
    bd[64:, 64:] = 1
    c["bdmask"] = bd
    c["ident"] = np.eye(128, dtype=np.float32)
    rm = np.ones((128, 512), np.float32)
    rm[:, ::64] = 0
    c["rmask"] = rm
    sel = np.zeros((128, 128), np.float32)
    sel[127, :] = 1
    c["sel127"] = sel
    ss = np.arange(128)[:, None]
    qq = np.arange(512)[None, :]
    c["nmask"] = np.ascontiguousarray(np.stack([np.where((128 * o + ss) <= qq, 0.0, -240000.0) for o in range(4)], axis=1).astype(np.float32))
    return c


PV_N = 62


def host_pv(inp):
    pv = np.zeros((128, PV_N), np.float32)
    mu = inp["mu_shift"][0]

    def put(col, vec):
        pv[:len(vec), col] = vec
    for j in range(4):
        put(j, mu[j * 128:(j + 1) * 128])
        put(5 + j, mu[576 + j * 128:576 + (j + 1) * 128])
        put(9 + j, mu[1088 + j * 128:1088 + (j + 1) * 128])
        put(15 + j, inp["rw_w0"][0][j * 128:(j + 1) * 128])
        put(19 + j, inp["rw_a0"][0][j * 128:(j + 1) * 128])
        put(23 + j, inp["rw_k_k"][0][j * 128:(j + 1) * 128])
        put(27 + j, inp["rw_k_a"][0][j * 128:(j + 1) * 128])
        put(31 + j, inp["rw_r_k"][0].reshape(-1)[j * 128:(j + 1) * 128])
        put(35 + j, inp["rw_ln_w"][0][j * 128:(j + 1) * 128])
        put(39 + j, inp["rw_ln_b"][0][j * 128:(j + 1) * 128])
    put(4, mu[512:576])
    put(13, mu[1600:1664])
    put(14, mu[1664:1792])
    put(43, np.tile(inp["fox_q_norm_w"][0], 2))
    put(44, np.tile(inp["fox_k_norm_w"][0], 2))
    put(45, inp["fox_b_f"][0])
    for kt in range(8):
        put(46 + kt, inp["norm_mix_w"][0][kt * 128:(kt + 1) * 128])
        put(54 + kt, inp["norm_ffn_w"][0][kt * 128:(kt + 1) * 128])
    return pv


def build(TP, TO, n_exp=NE, dbg=False, stop=None):
    TT = TP + TO
    NB = TT // 512
    NBP = TP // 512
    NBO = TO // 512
    NTL = TT // 128
    nc = bass.Bass("TRN2", target_bir_lowering=False)

    def din(name, shape, dt=F32):
        return nc.dram_tensor(name, shape, dt, kind="ExternalInput").ap()
    xp = din("xp", [TP, D])
    xo = din("xo", [TO, D])
    w_in = din("w_in", [D, NCOL])
    pvd = din("pv", [128, PV_N])
    w2d = din("rw_w2", [64, 512])
    a2d = din("rw_a2", [64, 512])
    g2d = din("rw_g2", [128, 512])
    w_out = din("w_out", [D, D])
    rgw = din("rgw", [D, 8])
    rew = din("rew", [D, 64])
    rbd = din("rb", [1, 72])
    if stop is None:
        wgd = din("wg", [NE, D, 512])
        wud = din("wu", [NE, D, 512])
        wdd = din("wd", [NE, 512, D])
    kbd = din("kb", [128, NTL])
    mask4d = din("mask4", [64, 2, 256])
    maskLd = din("maskL", [64, 2, 64])
    bdd = din("bdmask", [128, 128])
    identd = din("ident", [128, 128])
    rmaskd = din("rmask", [128, 512])
    sel127d = din("sel127", [128, 128])
    nmaskd = din("nmask", [128, 4, 512])
    out = nc.dram_tensor("out", [TO, D], F32, kind="ExternalOutput").ap()
    PT = nc.dram_tensor("PTo", [NCOL, TT + 1], F32, kind="ExternalOutput" if dbg else "Internal").ap()
    YT = nc.dram_tensor("YTo", [D, TO], BF16, kind="ExternalOutput" if dbg else "Internal").ap()

    P = Prog(nc)

    pe_hist = {}
    pe_idx = [0]

    def pe_serial(bank, base):
        pe_idx[0] += 1
        import os as _os2
        if _os2.environ.get("PESER") == "1":
            return True
        ser = False
        h = pe_hist.get(bank)
        if h is not None and h[0] != base and pe_idx[0] - h[1] <= 80:
            ser = True
        pe_hist[bank] = (base, pe_idx[0])
        return ser

    def mm(o, lhsT, rhs, start, stop, reads, writes):
        ser = pe_serial(writes[0], lhsT.base_partition())
        return P.op("pe", lambda e: e.matmul(o, lhsT=lhsT, rhs=rhs, start=start, stop=stop), reads, writes, serial=ser, inc=bool(stop))

    def tr(o, i, idn, reads, writes, last=True):
        ser = pe_serial(writes[0], i.base_partition())
        return P.op("pe", lambda e: e.transpose(o, i, idn), reads, writes, serial=ser, inc=last)

    def act(o, i, func, reads, writes, bias=None, scale=None, accum_out=None):
        kw = {}
        if bias is not None:
            kw["bias"] = bias
        if scale is not None:
            kw["scale"] = scale
        if accum_out is not None:
            kw["accum_out"] = accum_out
        return P.op("act", lambda e: e.activation(out=o, in_=i, func=func, **kw), reads, writes)

    def ts(eng, o, i, s1, s2, op0, op1, reads, writes):
        if s2 is None:
            return P.op(eng, lambda e: e.tensor_scalar(out=o, in0=i, scalar1=s1, scalar2=None, op0=op0), reads, writes)
        return P.op(eng, lambda e: e.tensor_scalar(out=o, in0=i, scalar1=s1, scalar2=s2, op0=op0, op1=op1), reads, writes)

    def tt(eng, o, a, b, op, reads, writes):
        return P.op(eng, lambda e: e.tensor_tensor(out=o, in0=a, in1=b, op=op), reads, writes)

    def stt(o, a, s, b, op0, op1, reads, writes):
        return P.op("dve", lambda e: e.scalar_tensor_tensor(out=o, in0=a, scalar=s, in1=b, op0=op0, op1=op1), reads, writes)

    def cp(eng, o, i, reads, writes):
        if eng == "act":
            return P.op("act", lambda e: e.activation(out=o, in_=i, func=AF.Copy), reads, writes)
        return P.op(eng, lambda e: e.tensor_copy(out=o, in_=i), reads, writes)

    def dma(eng, slot, o, i, reads, writes, group=None):
        return P.dma(eng, slot, lambda e: e.dma_start(out=o, in_=i), reads, writes, group=group)

    def memset(eng, o, val, writes):
        return P.op(eng, lambda e: e.memset(o, val), (), writes)

    with contextlib.ExitStack() as top:
        def sbt(st, name, shape, dt=F32):
            return st.enter_context(nc.sbuf_tensor("s_" + name, shape, dt))
        psb = [top.enter_context(nc.psum_tensor("ps%d" % i, [128, 512], F32)) for i in range(8)]
        ps_i = [0]

        def nps():
            i = ps_i[0] % 8
            ps_i[0] += 1
            return psb[i], "ps%d" % i

        pv = sbt(top, "pv", [128, PV_N])
        pv1 = sbt(top, "pv1", [128, PV_N])
        ident = sbt(top, "ident", [128, 128])
        bdm = sbt(top, "bdm", [128, 128])
        dma("sp", "g_top", pv[:], pvd, (), ["pv"], group="top")
        dma("sp", "g_top", ident[:], identd, (), ["ident"], group="top")
        dma("sp", "g_top", bdm[:], bdd, (), ["bdm"], group="top")
        P.group_end("top", "g_top")
        ts("dve", pv1[:], pv[:], -1.0, 1.0, ALU.mult, ALU.add, ["pv"], ["pv1"])
        import os as _os4
        for _ in range(int(_os4.environ.get("PENOPS", "0"))):
            pt_, pk_ = nps()
            mm(pt_[:, 0:128], ident[:], bdm[:], True, True, ["ident", "bdm"], [pk_])
        for _ in range(int(_os4.environ.get("NOPS", "0"))):
            ts("dve", pv1[:], pv[:], -1.0, 1.0, ALU.mult, ALU.add, ["pv"], ["pv1"])
            P.op("act", lambda e: e.activation(out=pv1[:], in_=pv[:], func=AF.Copy), ["pv"], ["pv1"])
            ts("dve", pv1[:], pv[:], -1.0, 1.0, ALU.mult, ALU.add, ["pv"], ["pv1"])
            ts("dve", pv1[:], pv[:], -1.0, 1.0, ALU.mult, ALU.add, ["pv"], ["pv1"])

        def pvc(col, n=128):
            return pv[0:n, col:col + 1]

        def pv1c(col, n=128):
            return pv1[0:n, col:col + 1]

        chunks = []
        c0 = 0
        while c0 < NCOL:
            n = min(128, NCOL - c0)
            for bnd in (512, 576, 1600, 1664):
                if c0 < bnd < c0 + n:
                    n = bnd - c0
            chunks.append((c0, n))
            c0 += n
        with contextlib.ExitStack() as st:
            win = sbt(st, "win", [128, 8, NCOL], BF16)
            for kt in range(8):
                for pc in range(3):
                    dma("pool", "g_win", win[:, kt, pc * 1112:(pc + 1) * 1112], w_in[kt * 128:(kt + 1) * 128, pc * 1112:(pc + 1) * 1112], (), ["win%d_%d" % (kt, pc)], group="win")
            P.group_end("win", "g_win")
            for kt in range(8):
                P.lastw["win%d" % kt] = P.lastw["win0_0"]
            xtm = [sbt(st, "xtm%d" % i, [128, 4, D]) for i in range(2)]
            xs = sbt(st, "xs", [128, 4, D])
            junk = sbt(st, "junk", [128, D], BF16)
            ssq = sbt(st, "ssq", [128, 4])
            hT = [sbt(st, "hT%d" % i, [128, 8, 512], BF16) for i in range(2)]
            stg = [sbt(st, "stg%d" % i, [128, 512]) for i in range(4)]
            zc = sbt(st, "zc", [128, 1])
            memset("dve", zc[:], 0.0, ["zc"])
            for r0 in range(0, NCOL, 128):
                n = min(128, NCOL - r0)
                dma("sp", "zc", PT[r0:r0 + n, 0:1], zc[0:n, :], ["zc"], [])
            sgi = 0
            for b in range(NB):
                q = b % 2
                src = xp[b * 512:(b + 1) * 512, :] if b < NBP else xo[(b - NBP) * 512:(b - NBP + 1) * 512, :]
                dma("sp", "xtm%d" % q, xtm[q][:], src.rearrange("(s p) d -> p s d", p=128), (), ["xtm%d" % q])
                for s in range(4):
                    act(junk[:], xtm[q][:, s, :], AF.Square, ["xtm%d" % q], ["junk", "ssq"], accum_out=ssq[:, s:s + 1])
                act(ssq[:], ssq[:], AF.Ln, ["ssq"], ["ssq"], bias=1e-6, scale=1.0 / D)
                act(ssq[:], ssq[:], AF.Exp, ["ssq"], ["ssq"], scale=-0.5)
                for s in range(4):
                    ts("dve", xs[:, s, :], xtm[q][:, s, :], ssq[:, s:s + 1], None, ALU.mult, None, ["xtm%d" % q, "ssq"], ["xs"])
                for kt in range(8):
                    pt, pk = nps()
                    for s in range(4):
                        tr(pt[:, s * 128:(s + 1) * 128], xs[:, s, kt * 128:(kt + 1) * 128], ident[:], ["xs", "ident"], [pk], last=(s == 3))
                    if kt % 2 == 0:
                        P.op("act", lambda e, o=hT[q][:, kt, :], i=pt[:], sc=pvc(46 + kt): e.activation(out=o, in_=i, func=AF.Copy, scale=sc),
                             [pk, "pv"], ["hT%d" % q])
                    else:
                        ts("dve", hT[q][:, kt, :], pt[:], pvc(46 + kt), None, ALU.mult, None, [pk, "pv"], ["hT%d" % q])
                for (c0, n) in chunks:
                    if b < NBP and 1792 <= c0 < 2304:
                        continue
                    pt, pk = nps()
                    for kt in range(8):
                        mm(pt[0:n, :], win[:, kt, c0:c0 + n], hT[q][:, kt, :], kt == 0, kt == 7, ["win%d" % kt, "hT%d" % q], [pk])
                    si = sgi % 4
                    sgi += 1
                    cp("act" if si % 2 == 0 else "dve", stg[si][0:n, :], pt[0:n, :], [pk], ["stg%d" % si])
                    dma("sp", "stg%d" % si, PT[c0:c0 + n, 1 + b * 512:1 + (b + 1) * 512], stg[si][0:n, :], ["stg%d" % si], [])
            P.barrier()
        if dbg:
            pass
        if stop == 'A':
            P.barrier()
            P.emit()
            return nc

        with contextlib.ExitStack() as st:
            w2s = sbt(st, "w2s", [64, 512])
            a2s = sbt(st, "a2s", [64, 512])
            g2s = sbt(st, "g2s", [128, 512])
            mask4 = sbt(st, "mask4", [64, 2, 256])
            maskL = sbt(st, "maskL", [64, 2, 64])
            rmask = sbt(st, "rmask", [128, 512])
            for nm, t_, d_ in (("w2s", w2s, w2d), ("a2s", a2s, a2d), ("g2s", g2s, g2d), ("mask4", mask4, mask4d),
                               ("maskL", maskL, maskLd), ("rmask", rmask, rmaskd)):
                dma("sp", "g_B", t_[:], d_, (), [nm], group="B")
            P.group_end("B", "g_B")
            raw = [sbt(st, "raw%d" % i, [128, 513]) for i in range(3)]
            tmp = [sbt(st, "tmp%d" % i, [128, 512]) for i in range(2)]
            names = ["pw_s", "pa_s", "pg_s", "th", "sg", "k_s", "kk", "sq", "rn", "kkn", "a_t", "ka", "sig", "cums", "dcs",
                     "E1", "E2", "E3", "t2"]
            T = {nm: sbt(st, nm, [128, 512]) for nm in names}
            SET = []
            for q in range(2):
                S = {}
                S["AR"] = sbt(st, "AR%d" % q, [128, 8, 2, 64])
                for nm in ("Bt", "Kt", "r_s", "k2", "v_s", "g", "yT"):
                    S[nm] = sbt(st, "%s%d" % (nm, q), [128, 512])
                for nm in ("Btok", "Ktok", "Vtok"):
                    S[nm] = sbt(st, "%s%d" % (nm, q), [64, 8, 128])
                S["Vpad"] = sbt(st, "Vpad%d" % q, [64, 8, 2, 128])
                S["gC"] = sbt(st, "gC%d" % q, [128, 8])
                memset("dve", S["Vpad"][:], 0.0, ["Vpad%d" % q])
                SET.append(S)
            CH = []
            for cq in range(2):
                Cq = {}
                Cq["G1"] = sbt(st, "G1%d" % cq, [64, 2, 256])
                Cq["Lm"] = sbt(st, "Lm%d" % cq, [64, 2, 64])
                Cq["NP"] = [sbt(st, "NP%d_%d" % (cq, i), [64, 2, 128]) for i in range(2)]
                Cq["LL"] = [sbt(st, "LL%d_%d" % (cq, i), [64, 2, 64]) for i in range(2)]
                Cq["TT"] = sbt(st, "TTm%d" % cq, [64, 2, 64])
                Cq["Xs"] = sbt(st, "Xs%d" % cq, [64, 128])
                Cq["Us"] = sbt(st, "Us%d" % cq, [64, 128])
                Cq["Upad"] = sbt(st, "Upad%d" % cq, [64, 2, 128])
                memset("dve", Cq["Upad"][:], 0.0, ["Upad%d" % cq])
                CH.append(Cq)
            STbd = sbt(st, "STbd", [128, 128])
            post = {nm: sbt(st, "po_" + nm, [128, 512]) for nm in ("yc", "sq", "rs", "yn", "rk", "bon")}
            outb = sbt(st, "outb", [128, 512], BF16)
            rawi = [0]

            def shift_tile(row0, n, mucol, b, dst, dkey):
                ri = rawi[0] % 3
                rawi[0] += 1
                rk_ = "raw%d" % ri
                dma("sp", rk_, raw[ri][0:n, :], PT[row0:row0 + n, b * 512:b * 512 + 513], (), [rk_])
                tq = ri % 2
                P.op("act", lambda e, o=tmp[tq][0:n, :], i=raw[ri][0:n, 0:512], sc=pvc(mucol, n): e.activation(out=o, in_=i, func=AF.Copy, scale=sc),
                     [rk_, "pv"], ["tmp%d" % tq])
                stt(dst[0:n, :], raw[ri][0:n, 1:513], pv1c(mucol, n), tmp[tq][0:n, :], ALU.mult, ALU.add,
                    [rk_, "pv1", "tmp%d" % tq], [dkey])

            import os as _os3
            for _ in range(int(_os3.environ.get("PSSHIFT", "0"))):
                nps()
            for j in range(int(_os3.environ.get("JSTART", "0")), 4):
                memset("dve", STbd[:], 0.0, ["STbd"])
                for b in range(NB):
                    q = b % 2
                    S = SET[q]
                    sk = lambda nm: "%s%d" % (nm, q)
                    own = b >= NBP
                    shift_tile(512, 64, 4, b, T["pw_s"], "pw_s")
                    shift_tile(1600, 64, 13, b, T["pa_s"], "pa_s")
                    shift_tile(1664, 128, 14, b, T["pg_s"], "pg_s")
                    shift_tile(j * 128, 128, j, b, S["r_s"], sk("r_s"))
                    shift_tile(576 + j * 128, 128, 5 + j, b, T["k_s"], "k_s")
                    shift_tile(1088 + j * 128, 128, 9 + j, b, S["v_s"], sk("v_s"))
                    act(T["th"][0:64, :], T["pw_s"][0:64, :], AF.Tanh, ["pw_s"], ["th"])
                    pt, pk = nps()
                    mm(pt[:], w2s[0:64, j * 128:(j + 1) * 128], T["th"][0:64, :], True, True, ["w2s", "th"], [pk])
                    act(T["sig"][:], pt[:], AF.Sigmoid, [pk, "pv"], ["sig"], bias=pvc(15 + j))
                    pt, pk = nps()
                    mm(pt[:], a2s[0:64, j * 128:(j + 1) * 128], T["pa_s"][0:64, :], True, True, ["a2s", "pa_s"], [pk])
                    act(T["a_t"][:], pt[:], AF.Sigmoid, [pk, "pv"], ["a_t"], bias=pvc(19 + j))
                    act(T["sg"][:], T["pg_s"][:], AF.Sigmoid, ["pg_s"], ["sg"])
                    pt, pk = nps()
                    mm(pt[:], g2s[:, j * 128:(j + 1) * 128], T["sg"][:], True, True, ["g2s", "sg"], [pk])
                    cp("dve", S["g"][:], pt[:], [pk], [sk("g")])
                    ts("dve", T["kk"][:], T["k_s"][:], pvc(23 + j), None, ALU.mult, None, ["k_s", "pv"], ["kk"])
                    tt("dve", T["sq"][:], T["kk"][:], T["kk"][:], ALU.mult, ["kk"], ["sq"])
                    pt, pk = nps()
                    mm(pt[:], bdm[:], T["sq"][:], True, True, ["bdm", "sq"], [pk])
                    act(T["rn"][:], pt[:], AF.Ln, [pk], ["rn"], bias=1e-16)
                    act(T["rn"][:], T["rn"][:], AF.Exp, ["rn"], ["rn"], scale=-0.5)
                    tt("dve", T["kkn"][:], T["kk"][:], T["rn"][:], ALU.mult, ["kk", "rn"], ["kkn"])
                    ts("dve", T["ka"][:], T["a_t"][:], pvc(27 + j), pv1c(27 + j), ALU.mult, ALU.add, ["a_t", "pv", "pv1"], ["ka"])
                    tt("dve", S["k2"][:], T["k_s"][:], T["ka"][:], ALU.mult, ["k_s", "ka"], [sk("k2")])
                    P.op("dve", lambda e, o=T["cums"][:], m=rmask[:], d1=T["sig"][:]: e.tensor_tensor_scan(out=o, data0=m, data1=d1, initial=0.0, op0=ALU.mult, op1=ALU.add),
                         ["rmask", "sig"], ["cums"])
                    tt("dve", T["dcs"][:], T["cums"][:], T["sig"][:], ALU.subtract, ["cums", "sig"], ["dcs"])
                    act(T["E1"][:], T["cums"][:], AF.Exp, ["cums"], ["E1"], scale=DECAY_C)
                    act(T["E2"][:], T["cums"][:], AF.Exp, ["cums"], ["E2"], scale=-DECAY_C)
                    act(T["E3"][:], T["dcs"][:], AF.Exp, ["dcs"], ["E3"], scale=-DECAY_C)
                    stt(S["AR"][:, :, 0, :], T["kkn"][:].rearrange("p (c t) -> p c t", t=64), -1.0,
                        T["E3"][:].rearrange("p (c t) -> p c t", t=64), ALU.mult, ALU.mult, ["kkn", "E3"], [sk("AR")])
                    tt("dve", S["AR"][:, :, 1, :], S["r_s"][:].rearrange("p (c t) -> p c t", t=64),
                       T["E2"][:].rearrange("p (c t) -> p c t", t=64), ALU.mult, [sk("r_s"), "E2"], [sk("AR")])
                    tt("dve", T["t2"][:], T["kkn"][:], T["a_t"][:], ALU.mult, ["kkn", "a_t"], ["t2"])
                    tt("dve", S["Bt"][:], T["t2"][:], T["E1"][:], ALU.mult, ["t2", "E1"], [sk("Bt")])
                    tt("dve", S["Kt"][:], S["k2"][:], T["E1"][:], ALU.mult, [sk("k2"), "E1"], [sk("Kt")])
                    cp("dve", S["gC"][:], T["E2"][:].rearrange("p (c t) -> p c t", t=64)[:, :, 63], ["E2"], [sk("gC")])
                    for nm_src, nm_dst in (("Bt", "Btok"), ("Kt", "Ktok"), ("v_s", "Vtok")):
                        for half in range(2):
                            pt, pk = nps()
                            for c4 in range(4):
                                c = half * 4 + c4
                                tr(pt[0:64, c4 * 128:(c4 + 1) * 128], S[nm_src][:, c * 64:(c + 1) * 64], ident[:], [sk(nm_src), "ident"], [pk], last=(c4 == 3))
                            cp("dve" if half == 0 else "dve", S[nm_dst][:, half * 4:(half + 1) * 4, :],
                               pt[0:64, :].rearrange("p (c f) -> p c f", f=128), [pk], [sk(nm_dst)])
                    for h in range(2):
                        cp("dve", S["Vpad"][:, :, h, h * 64:(h + 1) * 64], S["Vtok"][:, :, h * 64:(h + 1) * 64], [sk("Vtok")], [sk("Vpad")])
                    if stop is not None and stop.startswith("Bk") and [int(v) for v in stop[2:].split("_")] == [j, b, -1]:
                        P.barrier()
                        P.emit()
                        return nc
                    if stop == 'B1':
                        P.barrier()
                        P.emit()
                        return nc
                    def make_chunk(c, S=S, sk=sk, own=own):
                        cq = c % 2
                        Cq = CH[cq]
                        ck = lambda nm, cq=cq: "%s%d" % (nm, cq)
                        csl = slice(c * 64, (c + 1) * 64)
                        NPc, LLc = Cq["NP"], Cq["LL"]

                        def gram():
                            pg_, pgk = nps()
                            pl_, plk = nps()
                            for h in range(2):
                                hs = slice(h * 64, (h + 1) * 64)
                                mm(pg_[0:64, h * 256:h * 256 + 128], S["Bt"][hs, csl], S["AR"][hs, c, :, :], True, True, [sk("Bt"), sk("AR")], [pgk])
                                mm(pg_[0:64, h * 256 + 128:h * 256 + 256], S["Kt"][hs, csl], S["AR"][hs, c, :, :], True, True, [sk("Kt"), sk("AR")], [pgk])
                                mm(pl_[0:64, h * 64:(h + 1) * 64], S["AR"][hs, c, 0, :], S["Bt"][hs, csl], True, True, [sk("AR"), sk("Bt")], [plk])
                            tt("dve", Cq["G1"][:], pg_[0:64, :].rearrange("p (h f) -> p h f", h=2), mask4[:], ALU.mult, [pgk, "mask4"], [ck("G1")])
                            tt("dve", Cq["Lm"][:], pl_[0:64, 0:128].rearrange("p (h f) -> p h f", h=2), maskL[:], ALU.mult, [plk, "maskL"], [ck("Lm")])

                        def d0():
                            pd, pdk = nps()
                            for h in range(2):
                                mm(pd[0:64, h * 192:h * 192 + 64], Cq["Lm"][:, h, :], Cq["G1"][:, h, 0:64], True, True, [ck("Lm"), ck("G1")], [pdk])
                                mm(pd[0:64, h * 192 + 128:h * 192 + 192], Cq["G1"][:, h, 0:64], Cq["Lm"][:, h, :], True, True, [ck("Lm"), ck("G1")], [pdk])
                            pdv = pd[0:64, 0:384].rearrange("p (h f) -> p h f", h=2)
                            cp("dve", NPc[1][:, :, 0:64], pdv[:, :, 0:64], [pdk], [ck("NP1")])
                            cp("dve", LLc[1][:], pdv[:, :, 128:192], [pdk], [ck("LL1")])
                            for h in range(2):
                                tt("dve", NPc[1][:, h, 64:128], Cq["G1"][:, h, 0:64], ident[0:64, 0:64], ALU.add, [ck("G1"), "ident"], [ck("NP1")])

                        def dstage(i):
                            def f():
                                cur, nxt = i % 2, (i + 1) % 2
                                pd, pdk = nps()
                                for h in range(2):
                                    mm(pd[0:64, h * 192:h * 192 + 128], LLc[cur][:, h, :], NPc[cur][:, h, :], True, True,
                                       [ck("LL%d" % cur), ck("NP%d" % cur)], [pdk])
                                    if i < 5:
                                        mm(pd[0:64, h * 192 + 128:h * 192 + 192], NPc[cur][:, h, 0:64], LLc[cur][:, h, :], True, True,
                                           [ck("LL%d" % cur), ck("NP%d" % cur)], [pdk])
                                pdv = pd[0:64, 0:384].rearrange("p (h f) -> p h f", h=2)
                                if i < 5:
                                    cp("dve", NPc[nxt][:, :, 0:64], pdv[:, :, 0:64], [pdk], [ck("NP%d" % nxt)])
                                    cp("dve", LLc[nxt][:], pdv[:, :, 128:192], [pdk], [ck("LL%d" % nxt)])
                                    tt("dve", NPc[nxt][:, :, 64:128], pdv[:, :, 64:128], NPc[cur][:, :, 64:128], ALU.add,
                                       [pdk, ck("NP%d" % cur)], [ck("NP%d" % nxt)])
                                else:
                                    tt("dve", Cq["TT"][:], pdv[:, :, 64:128], NPc[cur][:, :, 64:128], ALU.add, [pdk, ck("NP%d" % cur)], [ck("TT")])
                            return f

                        def sx():
                            px, pxk = nps()
                            mm(px[0:64, 0:128], S["AR"][:, c, 0, :], STbd[:], True, False, [sk("AR"), "STbd"], [pxk])
                            for h in range(2):
                                mm(px[0:64, h * 64:(h + 1) * 64], Cq["G1"][:, h, 128:192], S["Vtok"][:, c, h * 64:(h + 1) * 64], False, h == 1,
                                   [ck("G1"), sk("Vtok")], [pxk])
                            cp("dve", Cq["Xs"][:], px[0:64, 0:128], [pxk], [ck("Xs")])

                        def su():
                            pu, puk = nps()
                            for h in range(2):
                                mm(pu[0:64, h * 64:(h + 1) * 64], Cq["TT"][:, h, :], Cq["Xs"][:, h * 64:(h + 1) * 64], True, True, [ck("TT"), ck("Xs")], [puk])
                            cp("dve", Cq["Us"][:], pu[0:64, 0:128], [puk], [ck("Us")])

                        def sy():
                            if not own:
                                return
                            for h in range(2):
                                cp("dve", Cq["Upad"][:, h, h * 64:(h + 1) * 64], Cq["Us"][:, h * 64:(h + 1) * 64], [ck("Us")], [ck("Upad")])
                            py, pyk = nps()
                            mm(py[:, 0:64], STbd[:], S["AR"][:, c, 1, :], True, False, ["STbd", sk("AR")], [pyk])
                            for h in range(2):
                                mm(py[:, 0:64], Cq["Upad"][:, h, :], Cq["G1"][:, h, 64:128], False, False, [ck("Upad"), ck("G1")], [pyk])
                                mm(py[:, 0:64], S["Vpad"][:, c, h, :], Cq["G1"][:, h, 192:256], False, h == 1, [sk("Vpad"), ck("G1")], [pyk])
                            cp("dve", S["yT"][:, csl], py[:, 0:64], [pyk], [sk("yT")])

                        def ss():
                            pss, psk = nps()
                            mm(pss[:, 0:128], S["Btok"][:, c, :], Cq["Us"][:], True, False, [sk("Btok"), ck("Us")], [psk])
                            mm(pss[:, 0:128], S["Ktok"][:, c, :], S["Vtok"][:, c, :], False, False, [sk("Ktok"), sk("Vtok")], [psk])
                            mm(pss[:, 0:128], ident[:], STbd[:], False, True, ["ident", "STbd"], [psk])
                            stt(STbd[:], pss[:, 0:128], S["gC"][:, c:c + 1], bdm[:], ALU.mult, ALU.mult, [psk, sk("gC"), "bdm"], ["STbd"])
                        return [gram, d0] + [dstage(i) for i in range(1, 6)], [sx, su, sy, ss]

                    chunks_ = [make_chunk(c) for c in range(8)]
                    for f in chunks_[0][0]:
                        f()
                    for c in range(8):
                        Dn = chunks_[c + 1][0] if c + 1 < 8 else []
                        Sc = chunks_[c][1]
                        order = []
                        for k_ in range(max(len(Dn), len(Sc))):
                            if k_ < len(Dn):
                                order.append(Dn[k_])
                            if k_ < len(Sc):
                                order.append(Sc[k_])
                        for f in order:
                            f()
                    if own:
                        pm, pmk = nps()
                        mm(pm[:], bdm[:], S["yT"][:], True, True, ["bdm", sk("yT")], [pmk])
                        stt(post["yc"][:], pm[:], -1.0 / 64, S["yT"][:], ALU.mult, ALU.add, [pmk, sk("yT")], ["po_yc"])
                        tt("dve", post["sq"][:], post["yc"][:], post["yc"][:], ALU.mult, ["po_yc"], ["po_sq"])
                        pm, pmk = nps()
                        mm(pm[:], bdm[:], post["sq"][:], True, True, ["bdm", "po_sq"], [pmk])
                        act(post["rs"][:], pm[:], AF.Ln, [pmk], ["po_rs"], bias=64e-5, scale=1.0 / 64)
                        act(post["rs"][:], post["rs"][:], AF.Exp, ["po_rs"], ["po_rs"], scale=-0.5)
                        tt("dve", post["yn"][:], post["yc"][:], post["rs"][:], ALU.mult, ["po_yc", "po_rs"], ["po_yn"])
                        ts("dve", post["yn"][:], post["yn"][:], pvc(35 + j), pvc(39 + j), ALU.mult, ALU.add, ["po_yn", "pv"], ["po_yn"])
                        stt(post["rk"][:], S["r_s"][:], pvc(31 + j), S["k2"][:], ALU.mult, ALU.mult, [sk("r_s"), sk("k2"), "pv"], ["po_rk"])
                        pm, pmk = nps()
                        mm(pm[:], bdm[:], post["rk"][:], True, True, ["bdm", "po_rk"], [pmk])
                        tt("dve", post["bon"][:], pm[:], S["v_s"][:], ALU.mult, [pmk, sk("v_s")], ["po_bon"])
                        tt("dve", post["bon"][:], post["bon"][:], post["yn"][:], ALU.add, ["po_bon", "po_yn"], ["po_bon"])
                        tt("dve", outb[:], post["bon"][:], S["g"][:], ALU.mult, ["po_bon", sk("g")], ["outb"])
                        bo = b - NBP
                        dma("sp", "outb", YT[j * 128:(j + 1) * 128, bo * 512:(bo + 1) * 512], outb[:], ["outb"], [])
                    if _os3.environ.get("BLKBAR") == "1":
                        P.barrier()
                    if stop is not None and stop.startswith("Bm") and [int(v) for v in stop[2:].split("_")] == [j, b]:
                        P.barrier()
                        P.emit()
                        return nc
                    if stop is not None and stop.startswith("Bn") and int(stop[2:]) == b:
                        P.barrier()
                        P.emit()
                        return nc
                if stop is not None and stop.startswith("Bj") and int(stop[2:]) == j:
                    P.barrier()
                    P.emit()
                    return nc
            P.barrier()

        if stop == 'B':
            P.barrier()
            P.emit()
            return nc
        with contextlib.ExitStack() as st:
            nmk = sbt(st, "nmk", [128, 4, 512])
            sel127 = sbt(st, "sel127", [128, 128])
            kbs = sbt(st, "kbs", [128, NTL])
            dma("sp", "g_C", nmk[:], nmaskd, (), ["nmk"], group="C")
            dma("sp", "g_C", sel127[:], sel127d, (), ["sel127"], group="C")
            dma("sp", "g_C", kbs[:], kbd, (), ["kbs"], group="C")
            fT = sbt(st, "fT", [8, TT])
            cnT = sbt(st, "cnT", [8, TT])
            cntok = sbt(st, "cntok", [128, NTL, 8])
            cref = sbt(st, "cref", [128, 8])
            biasq = sbt(st, "biasq", [128, NTL, 8])
            qahi = sbt(st, "qahi", [8, TO], BF16)
            qalo = sbt(st, "qalo", [8, TO], BF16)
            KTa = sbt(st, "KTa", [66, TT], BF16)
            QTa = sbt(st, "QTa", [66, TO], BF16)
            Vx = sbt(st, "Vx", [128, NTL, 65], BF16)
            ld = [sbt(st, "ld%d" % i, [64, 512]) for i in range(2)]
            sq2 = sbt(st, "sq2", [64, 512])
            rn2 = sbt(st, "rn2", [64, 512])
            Pt = [sbt(st, "Pt%d" % i, [128, 512], BF16) for i in range(3)]
            mt = [sbt(st, "mt%d" % i, [128, 512]) for i in range(2)]
            osb = sbt(st, "osb", [65, 512])
            ones65 = sbt(st, "ones65", [65, 64])
            yfx = sbt(st, "yfx", [64, 512], BF16)
            memset("dve", ones65[:], 1.0, ["ones65"])
            memset("dve", Vx[:], 1.0, ["Vx"])
            memset("dve", KTa[64:66, :], 1.0, ["KTa"])
            dma("sp", "g_C", fT[:], PT[3328:3336, 1:TT + 1], (), ["fT"], group="C")
            P.group_end("C", "g_C")
            nbf = sbt(st, "nbf", [8, 1])
            ts("dve", nbf[:], pv[0:8, 45:46], -1.0, None, ALU.mult, None, ["pv"], ["nbf"])
            act(fT[:], fT[:], AF.Exp, ["fT", "nbf"], ["fT"], bias=nbf[:, 0:1], scale=-1.0)
            act(fT[:], fT[:], AF.Ln, ["fT"], ["fT"], bias=1.0)
            P.op("dve", lambda e: e.tensor_tensor_scan(out=cnT[:], data0=ones65[0:8, 0:1].to_broadcast([8, TT]), data1=fT[:], initial=0.0, op0=ALU.mult, op1=ALU.add),
                 ["fT", "ones65"], ["cnT"])
            for g0 in range(0, NTL, 64):
                gn = min(64, NTL - g0)
                pt, pk = nps()
                for tl in range(gn):
                    tr(pt[:, tl * 8:(tl + 1) * 8], cnT[0:8, (g0 + tl) * 128:(g0 + tl + 1) * 128], ident[0:8, 0:8], ["cnT", "ident"], [pk], last=(tl == gn - 1))
                cp("act", cntok[:, g0:g0 + gn, :], pt[:, 0:gn * 8].rearrange("p (t h) -> p t h", h=8), [pk], ["cntok"])
            for h8 in range(8):
                tt("dve", cntok[:, :, h8], cntok[:, :, h8], kbs[:], ALU.add, ["cntok", "kbs"], ["cntok"])
            qa32 = fT[:, 0:TO]
            for jb in range(NBO):
                e_ = TP + (jb + 1) * 512 - 1
                ts("dve", fT[:, jb * 512:(jb + 1) * 512], cnT[0:8, TP + jb * 512:TP + (jb + 1) * 512], cnT[0:8, e_:e_ + 1], -8.0,
                   ALU.subtract, ALU.mult, ["cnT", "fT"], ["qa32", "fT"])
            cp("dve", qahi[:], qa32, ["qa32"], ["qahi"])
            tt("dve", qalo[:], qa32, qahi[:], ALU.subtract, ["qa32", "qahi"], ["qalo"])
            pti = [0]
            mti = [0]
            for hf in range(8):
                dma("sp", "qa_hi", QTa[64:65, :], qahi[hf:hf + 1, :], ["qahi"], ["QTa"])
                dma("sp", "qa_lo", QTa[65:66, :], qalo[hf:hf + 1, :], ["qalo"], ["QTa"])
                for kind, row0, dst, dk, nblk, toff, wcol in (("k", 2304 + hf * 64, KTa, "KTa", NB, 0, 44),
                                                              ("q", 1792 + hf * 64, QTa, "QTa", NBO, TP, 43)):
                    for b in range(nblk):
                        q = b % 2
                        lk = "ld%d" % q
                        dma("sp", lk, ld[q][:], PT[row0:row0 + 64, 1 + toff + b * 512:1 + toff + (b + 1) * 512], (), [lk])
                        tt("dve", sq2[:], ld[q][:], ld[q][:], ALU.mult, [lk], ["sq2"])
                        pt, pk = nps()
                        mm(pt[0:64, :], bdm[0:64, 0:64], sq2[:], True, True, ["bdm", "sq2"], [pk])
                        act(rn2[:], pt[0:64, :], AF.Ln, [pk], ["rn2"], bias=1e-6, scale=1.0 / 64)
                        act(rn2[:], rn2[:], AF.Exp, ["rn2"], ["rn2"], scale=-0.5)
                        stt(dst[0:64, b * 512:(b + 1) * 512], ld[q][:], pvc(wcol, 64), rn2[:], ALU.mult, ALU.mult, [lk, "pv", "rn2"], [dk])
                for b in range(NB):
                    q = b % 2
                    lk = "ld%d" % q
                    dma("sp", lk, ld[q][:], PT[2816 + hf * 64:2816 + (hf + 1) * 64, 1 + b * 512:1 + (b + 1) * 512], (), [lk])
                    pt, pk = nps()
                    for s in range(4):
                        tr(pt[:, s * 64:(s + 1) * 64], ld[q][:, s * 128:(s + 1) * 128], ident[0:64, 0:64], [lk, "ident"], [pk], last=(s == 3))
                    cp("act", Vx[:, b * 4:(b + 1) * 4, 0:64], pt[:, 0:256].rearrange("p (s d) -> p s d", s=4), [pk], ["Vx"])
                for jb in range(NBO):
                    nkt = (TP + (jb + 1) * 512) // 128
                    pt, pk = nps()
                    mm(pt[:, 0:8], sel127[:], cntok[:, nkt - 1, :], True, True, ["sel127", "cntok"], [pk])
                    cp("act", cref[:], pt[:, 0:8], [pk], ["cref"])
                    ts("dve", biasq[:, 0:nkt, hf], cntok[:, 0:nkt, hf], cref[:, hf:hf + 1], None, ALU.subtract, None, ["cntok", "cref"], ["biasq"])
                    po, pok = nps()
                    for kt in range(nkt):
                        psc, psk_ = nps()
                        if psk_ == pok:
                            psc, psk_ = nps()
                        mm(psc[:], KTa[0:66, kt * 128:(kt + 1) * 128], QTa[0:66, jb * 512:(jb + 1) * 512], True, True, ["KTa", "QTa"], [psk_])
                        pi = pti[0] % 3
                        pti[0] += 1
                        pkk = "Pt%d" % pi
                        o = kt - (nkt - 4)
                        if o >= 0:
                            mi = mti[0] % 2
                            mti[0] += 1
                            tt("dve", mt[mi][:], psc[:], nmk[:, o, :], ALU.add, [psk_, "nmk"], ["mt%d" % mi])
                            act(Pt[pi][:], mt[mi][:], AF.Exp, ["mt%d" % mi, "biasq"], [pkk], bias=biasq[:, kt, hf:hf + 1], scale=0.125)
                        else:
                            act(Pt[pi][:], psc[:], AF.Exp, [psk_, "biasq"], [pkk], bias=biasq[:, kt, hf:hf + 1], scale=0.125)
                        mm(po[0:65, :], Vx[:, kt, :], Pt[pi][:], kt == 0, kt == nkt - 1, ["Vx", pkk], [pok])
                    cp("act", osb[:], po[0:65, :], [pok], ["osb"])
                    P.op("dve", lambda e: e.reciprocal(out=osb[64:65, :], in_=osb[64:65, :]), ["osb"], ["osb"])
                    pt, pk = nps()
                    mm(pt[0:64, :], ones65[64:65, :], osb[64:65, :], True, True, ["ones65", "osb"], [pk])
                    tt("dve", yfx[:], osb[0:64, :], pt[0:64, :], ALU.mult, ["osb", pk], ["yfx"])
                    dma("sp", "yfx", YT[512 + hf * 64:512 + (hf + 1) * 64, jb * 512:(jb + 1) * 512], yfx[:], ["yfx"], [])
            P.barrier()
        if stop == 'C':
            P.barrier()
            P.emit()
            return nc
        TH = min(2048, TO)
        NH = TO // TH
        NTH = TH // 128
        with contextlib.ExitStack() as st:
            wo = sbt(st, "wo", [128, 8, D], BF16)
            for c in range(8):
                dma("pool", "g_D", wo[:, c, :], w_out[c * 128:(c + 1) * 128, :], (), ["wo%d" % c], group="D")
            rw32 = sbt(st, "rw32", [128, 8, 72])
            dma("pool", "g_D", rw32[:, :, 0:8], rgw.rearrange("(kt p) g -> p kt g", p=128), (), ["rw32a"], group="D")
            dma("pool", "g_D", rw32[:, :, 8:72], rew.rearrange("(kt p) g -> p kt g", p=128), (), ["rw32b"], group="D")
            rbs = sbt(st, "rbs", [1, 72])
            dma("pool", "g_D", rbs[:], rbd, (), ["rbs"], group="D")
            P.group_end("D", "g_D")
            ones1 = sbt(st, "ones1", [1, 128])
            memset("dve", ones1[:], 1.0, ["ones1"])
            acc = sbt(st, "acc", [128, NTH, D])
            h2T = sbt(st, "h2T", [128, 8, TH], BF16)
            gates = sbt(st, "gates", [128, NTH, 64])
            ym = [sbt(st, "ym%d" % i, [128, 8, 128], BF16) for i in range(2)]
            xt = [sbt(st, "xt%d" % i, [128, D]) for i in range(2)]
            xs2 = sbt(st, "xs2", [128, D])
            junk2 = sbt(st, "junk2", [128, D], BF16)
            ss2 = sbt(st, "ss2", [128, 1])
            h32 = sbt(st, "h32", [128, 8, 128])
            lg = sbt(st, "lg", [128, 72])
            sm = {nm: sbt(st, "sm_" + nm, [128, 8]) for nm in ("gm", "goh", "ge", "sel", "m1", "k1", "s2", "k2", "gl")}
            sc1 = {nm: sbt(st, "sc_" + nm, [128, 1]) for nm in ("gmax", "gsum", "m1", "m2", "w1", "w2")}
            e3 = sbt(st, "e3", [128, 8, 8])
            wg = [sbt(st, "wg%d" % i, [128, 8, 512], BF16) for i in range(2)]
            wu = [sbt(st, "wu%d" % i, [128, 8, 512], BF16) for i in range(2)]
            wdn = [sbt(st, "wdn%d" % i, [128, 4, D], BF16) for i in range(2)]
            sgl = [sbt(st, "sgl%d" % i, [128, 512]) for i in range(2)]
            hid = [sbt(st, "hid%d" % i, [128, 4, 512], BF16) for i in range(2)]
            for hh in range(NH):
                for i in range(NTH):
                    q = i % 2
                    tok0 = hh * TH + i * 128
                    dma("sp", "ym%d" % q, ym[q][:], YT[:, tok0:tok0 + 128].rearrange("(c p) t -> p c t", p=128), (), ["ym%d" % q])
                    dma("sp", "xt%d" % q, xt[q][:], xo[tok0:tok0 + 128, :], (), ["xt%d" % q])
                    for nh in range(2):
                        pt, pk = nps()
                        ns = slice(nh * 512, (nh + 1) * 512)
                        for c in range(8):
                            mm(pt[:], ym[q][:, c, :], wo[:, c, ns], c == 0, c == 7, ["ym%d" % q, "wo%d" % c], [pk])
                        tt("dve", acc[:, i, ns], pt[:], xt[q][:, ns], ALU.add, [pk, "xt%d" % q], ["acc%d" % i])
                    ak = "acc%d" % i
                    act(junk2[:], acc[:, i, :], AF.Square, [ak], ["junk2", "ss2"], accum_out=ss2[:, 0:1])
                    act(ss2[:], ss2[:], AF.Ln, ["ss2"], ["ss2"], bias=1e-6, scale=1.0 / D)
                    act(ss2[:], ss2[:], AF.Exp, ["ss2"], ["ss2"], scale=-0.5)
                    ts("dve", xs2[:], acc[:, i, :], ss2[:, 0:1], None, ALU.mult, None, [ak, "ss2"], ["xs2"])
                    for half in range(2):
                        pt, pk = nps()
                        for k4 in range(4):
                            kt = half * 4 + k4
                            tr(pt[:, k4 * 128:(k4 + 1) * 128], xs2[:, kt * 128:(kt + 1) * 128], ident[:], ["xs2", "ident"], [pk], last=(k4 == 3))
                        for k4 in range(4):
                            kt = half * 4 + k4
                            if k4 % 2 == 0:
                                P.op("act", lambda e, o=h32[:, kt, :], i_=pt[:, k4 * 128:(k4 + 1) * 128], sc=pvc(54 + kt): e.activation(out=o, in_=i_, func=AF.Copy, scale=sc),
                                     [pk, "pv"], ["h32"])
                            else:
                                ts("dve", h32[:, kt, :], pt[:, k4 * 128:(k4 + 1) * 128], pvc(54 + kt), None, ALU.mult, None, [pk, "pv"], ["h32"])
                    cp("dve", h2T[:, :, i * 128:(i + 1) * 128], h32[:], ["h32"], ["h2T"])
                    pt, pk = nps()
                    for kt in range(8):
                        mm(pt[:, 0:72], h32[:, kt, :], rw32[:, kt, :], kt == 0, False, ["h32", "rw32a", "rw32b"], [pk])
                    mm(pt[:, 0:72], ones1[0:1, :], rbs[0:1, :], False, True, ["ones1", "rbs"], [pk])
                    cp("act", lg[:], pt[:, 0:72], [pk], ["lg"])
                    R = ["lg"]
                    W = ["rt"]
                    P.op("dve", lambda e: e.reduce_max(out=sc1["gmax"][:], in_=lg[:, 0:8], axis=mybir.AxisListType.X), R, W)
                    ts("dve", sm["goh"][:], lg[:, 0:8], sc1["gmax"][:, 0:1], None, ALU.is_equal, None, R + W, W)
                    ts("dve", sm["gm"][:], lg[:, 0:8], sc1["gmax"][:, 0:1], None, ALU.subtract, None, R + W, W)
                    act(sm["ge"][:], sm["gm"][:], AF.Exp, W, W, accum_out=sc1["gsum"][:, 0:1])
                    P.op("dve", lambda e: e.reciprocal(out=sc1["gsum"][:], in_=sc1["gsum"][:]), ["rt"], ["rt"])
                    tt("dve", e3[:], lg[:, 8:72].rearrange("p (g e) -> p g e", e=8), sm["goh"][:].unsqueeze(2).to_broadcast([128, 8, 8]), ALU.mult, R + W, W)
                    P.op("dve", lambda e: e.reduce_sum(out=sm["sel"][:], in_=e3[:].rearrange("p g e -> p e g"), axis=mybir.AxisListType.X), W, W)
                    P.op("dve", lambda e: e.reduce_max(out=sc1["m1"][:], in_=sm["sel"][:], axis=mybir.AxisListType.X), W, W)
                    ts("dve", sm["k1"][:], sm["sel"][:], sc1["m1"][:, 0:1], None, ALU.is_equal, None, W, W)
                    stt(sm["s2"][:], sm["k1"][:], -1e30, sm["sel"][:], ALU.mult, ALU.add, W, W)
                    P.op("dve", lambda e: e.reduce_max(out=sc1["m2"][:], in_=sm["s2"][:], axis=mybir.AxisListType.X), W, W)
                    ts("dve", sm["k2"][:], sm["s2"][:], sc1["m2"][:, 0:1], None, ALU.is_equal, None, W, W)
                    tt("dve", sc1["w1"][:], sc1["m1"][:], sc1["m2"][:], ALU.subtract, W, W)
                    act(sc1["w1"][:], sc1["w1"][:], AF.Sigmoid, W, ["rt"])
                    ts("dve", sc1["w2"][:], sc1["w1"][:], -1.0, 1.0, ALU.mult, ALU.add, ["rt"], W)
                    tt("dve", sc1["w1"][:], sc1["w1"][:], sc1["gsum"][:], ALU.mult, W, W)
                    tt("dve", sc1["w2"][:], sc1["w2"][:], sc1["gsum"][:], ALU.mult, W, W)
                    ts("dve", sm["gl"][:], sm["k1"][:], sc1["w1"][:, 0:1], None, ALU.mult, None, W, W)
                    stt(sm["gl"][:], sm["k2"][:], sc1["w2"][:, 0:1], sm["gl"][:], ALU.mult, ALU.add, W, W)
                    tt("dve", gates[:, i, :].rearrange("p (g e) -> p g e", e=8), sm["goh"][:].unsqueeze(2).to_broadcast([128, 8, 8]),
                       sm["gl"][:].unsqueeze(1).to_broadcast([128, 8, 8]), ALU.mult, W, ["gates"])
                for ex in range(n_exp):
                    q = ex % 2
                    dma("pool", "wg%d" % q, wg[q][:], wgd[ex].rearrange("(kt p) f -> p kt f", p=128), (), ["wg%d" % q])
                    dma("pool", "wu%d" % q, wu[q][:], wud[ex].rearrange("(kt p) f -> p kt f", p=128), (), ["wu%d" % q])
                    dma("pool", "wdn%d" % q, wdn[q][:], wdd[ex].rearrange("(fc p) n -> p fc n", p=128), (), ["wdn%d" % q])
                    for sb_ in range(TH // 512):
                        hq = sb_ % 2
                        tsl = slice(sb_ * 512, (sb_ + 1) * 512)
                        for fc in range(4):
                            pg_, pgk = nps()
                            pu_, puk = nps()
                            for kt in range(8):
                                mm(pg_[:], wg[q][:, kt, fc * 128:(fc + 1) * 128], h2T[:, kt, tsl], kt == 0, kt == 7, ["wg%d" % q, "h2T"], [pgk])
                            for kt in range(8):
                                mm(pu_[:], wu[q][:, kt, fc * 128:(fc + 1) * 128], h2T[:, kt, tsl], kt == 0, kt == 7, ["wu%d" % q, "h2T"], [puk])
                            sq_ = fc % 2
                            act(sgl[sq_][:], pg_[:], AF.Silu, [pgk], ["sgl%d" % sq_])
                            tt("dve", hid[hq][:, fc, :], sgl[sq_][:], pu_[:], ALU.mult, ["sgl%d" % sq_, puk], ["hid%d_%d" % (hq, fc)])
                        for t4 in range(4):
                            ti = sb_ * 4 + t4
                            for nh in range(2):
                                po, pok = nps()
                                for fc in range(4):
                                    mm(po[:], hid[hq][:, fc, t4 * 128:(t4 + 1) * 128], wdn[q][:, fc, nh * 512:(nh + 1) * 512], fc == 0, fc == 3,
                                       ["hid%d_%d" % (hq, fc), "wdn%d" % q], [pok])
                                stt(acc[:, ti, nh * 512:(nh + 1) * 512], po[:], gates[:, ti, ex:ex + 1], acc[:, ti, nh * 512:(nh + 1) * 512],
                                    ALU.mult, ALU.add, [pok, "gates", "acc%d" % ti], ["acc%d" % ti])
                for i in range(NTH):
                    tok0 = hh * TH + i * 128
                    dma("sp", "ost%d" % (i % 4), out[tok0:tok0 + 128, :], acc[:, i, :], ["acc%d" % i], ["out%d" % i])
                P.barrier()
        P.barrier()
        P.emit()
    return nc


def make_core_inputs(inputs, c, T, consts, pvt):
    b, th = c // 2, c % 2
    TP = TO = T // 2
    x = inputs["x"]
    d = {}
    d["xp"] = np.ascontiguousarray(x[b, 0:TP]) if th == 1 else np.zeros((TP, D), np.float32)
    d["xo"] = np.ascontiguousarray(x[b, th * TO:(th + 1) * TO])
    d["w_in"] = inputs["w_in"][0]
    d["pv"] = pvt
    d["rw_w2"] = inputs["rw_w2"][0]
    d["rw_a2"] = inputs["rw_a2"][0]
    d["rw_g2"] = inputs["rw_g2"][0]
    d["w_out"] = inputs["w_out"][0]
    d["rgw"] = inputs["router_group_w"][0]
    d["rew"] = inputs["router_expert_w"][0]
    d["rb"] = np.concatenate([inputs["router_group_b"][0], inputs["router_expert_b"][0]])[None, :]
    d["wg"] = inputs["exp_w_gate"][0]
    d["wu"] = inputs["exp_w_up"][0]
    d["wd"] = inputs["exp_w_down"][0]
    kb = np.zeros((128, T // 128), np.float32)
    if th == 0:
        kb[:, :TP // 128] = -30000.0
    d["kb"] = kb
    d.update(consts)
    return d


def kernel(**inputs):
    inputs = {k: np.asarray(v) for k, v in inputs.items()}
    x = inputs["x"]
    B, T, _ = x.shape
    TP = TO = T // 2
    consts = host_consts()
    pvt = host_pv(inputs)
    nc = build(TP, TO)
    ncores = 2 * B
    in_maps = [make_core_inputs(inputs, c, T, consts, pvt) for c in range(ncores)]
    res = run_bass_kernel_spmd(nc, in_maps, core_ids=list(range(ncores)))
    out = np.empty((B, T, D), np.float32)
    for c in range(ncores):
        b, th = c // 2, c % 2
        out[b, th * TO:(th + 1) * TO] = res.results[c]["out"]
    return out
```

```python
import contextlib
import numpy as np
import ml_dtypes
import concourse.bass as bass
import concourse.mybir as mybir
from concourse.bass_utils import run_bass_kernel_spmd

F32 = mybir.dt.float32
BF16 = mybir.dt.bfloat16
AF = mybir.ActivationFunctionType
ALU = mybir.AluOpType

ENGS = ("pe", "act", "dve", "pool", "sp")
EPOCH = 1400
D = 1024
NCOL = 3336
NE = 64
DECAY_C = 0.6065306597126334


class Prog:
    def __init__(self, nc):
        self.nc = nc
        self.ops = {e: [] for e in ENGS}
        self.cnt = {}
        self.seen = {e: {} for e in ENGS}
        self.lastw = {}
        self.readers = {}
        self.eng_n = {e: 0 for e in ENGS}
        self.semkeys = []
        self.dma_n = {}
        self.groups = {}

    def _semkey(self, k):
        if k not in self.cnt:
            self.cnt[k] = 0
            self.semkeys.append(k)
        return k

    def _deps(self, eng, reads, writes):
        deps = {}

        def add(tok):
            if tok is None:
                return
            k, v = tok
            if deps.get(k, 0) < v:
                deps[k] = v
        for b in reads:
            add(self.lastw.get(b))
        for b in writes:
            add(self.lastw.get(b))
            for t in self.readers.get(b, ()):
                add(t)
        waits = []
        for k, v in deps.items():
            if eng == "pe" and k[0] == "pe":
                continue
            if self.seen[eng].get(k, 0) >= v:
                continue
            self.seen[eng][k] = v
            waits.append((k, v))
        return waits

    def _commit(self, tok, reads, writes):
        for b in writes:
            self.lastw[b] = tok
            self.readers[b] = []
        for b in reads:
            self.readers.setdefault(b, []).append(tok)

    def op(self, eng, fn, reads=(), writes=(), serial=False, inc=True):
        waits = self._deps(eng, reads, writes)
        k = self._semkey((eng, self.eng_n[eng] // EPOCH))
        if serial and self.cnt[k] > 0 and self.seen[eng].get(k, 0) < self.cnt[k]:
            self.seen[eng][k] = self.cnt[k]
            waits.append((k, self.cnt[k]))
        if inc:
            self.cnt[k] += 1
            tok = (k, self.cnt[k])
            self.ops[eng].append((waits, fn, (k, 1)))
            if self.cnt[k] >= EPOCH:
                self.eng_n[eng] += EPOCH
        else:
            tok = (k, self.cnt[k] + 1)
            self.ops[eng].append((waits, fn, None))
        self._commit(tok, reads, writes)
        return tok

    def dma(self, eng, slot, fn, reads=(), writes=(), group=None):
        waits = self._deps(eng, reads, writes)
        n = self.dma_n.get(slot, 0)
        self.dma_n[slot] = n + 1
        k = self._semkey(("dma", slot, n // 96))
        self.cnt[k] += 16
        tok = (k, self.cnt[k])
        self.ops[eng].append((waits, fn, (k, 16)))
        self._commit(tok, reads, writes)
        if group is not None:
            self.groups.setdefault(group, []).extend(writes)
        return tok

    def group_end(self, group, slot):
        n = self.dma_n[slot]
        k = ("dma", slot, (n - 1) // 96)
        assert (n - 1) // 96 == 0
        for b in self.groups.pop(group, []):
            self.lastw[b] = (k, self.cnt[k])

    def barrier(self, engs=ENGS):
        for e in engs:
            waits = []
            for k in self.semkeys:
                v = self.cnt[k]
                if v == 0 or self.seen[e].get(k, 0) >= v:
                    continue
                if e == "pe" and k[0] == "pe":
                    continue
                self.seen[e][k] = v
                waits.append((k, v))
            if waits:
                self.ops[e].append((waits, None, None))

    def emit(self):
        nc = self.nc
        with contextlib.ExitStack() as st:
            st.enter_context(nc.allow_non_contiguous_dma(reason="small strided setup / layout DMAs"))
            sems = {}
            print("n_sems", len(self.semkeys), {e: len(v) for e, v in self.ops.items()})
            for i, k in enumerate(self.semkeys):
                sems[k] = st.enter_context(nc.semaphore("s%d" % i))
            block = st.enter_context(nc.Block())
            engmap = {"pe": block.tensor, "act": block.scalar, "dve": block.vector,
                      "pool": block.gpsimd, "sp": block.sync}

            def mk(e):
                def body(engine):
                    for waits, fn, inc in self.ops[e]:
                        for (k, v) in waits:
                            engine.wait_ge(sems[k], v)
                        if fn is not None:
                            ins = fn(engine)
                            if inc is not None:
                                ins.then_inc(sems[inc[0]], inc[1])
                return body
            for e in ENGS:
                if self.ops[e]:
                    engmap[e](mk(e))


def host_consts():
    c = {}
    s = np.arange(64)[:, None]
    t = np.arange(64)[None, :]
    strict = (s < t).astype(np.float32)
    incl = (s <= t).astype(np.float32)
    m4 = np.concatenate([strict, incl, strict, incl], axis=1)
    c["mask4"] = np.ascontiguousarray(np.stack([m4, m4], axis=1))
    ml = (t < s).astype(np.float32)
    c["maskL"] = np.ascontiguousarray(np.stack([ml, ml], axis=1))
    bd = np.zeros((128, 128), np.float32)
    bd[:64, :64] = 1
    bd[64:, 64:] = 1
    c["bdmask"] = bd
    c["ident"] = np.eye(128, dtype=np.float32)
    rm = np.ones((128, 512), np.float32)
    rm[:, ::64] = 0
    c["rmask"] = rm
    sel = np.zeros((128, 128), np.float32)
    sel[127, :] = 1
    c["sel127"] = sel
    ss = np.arange(128)[:, None]
    qq = np.arange(512)[None, :]
    c["nmask"] = np.ascontiguousarray(np.stack([np.where((128 * o + ss) <= qq, 0.0, -240000.0) for o in range(4)], axis=1).astype(np.float32))
    return c


PV_N = 62


def host_pv(inp):
    pv = np.zeros((128, PV_N), np.float32)
    mu = inp["mu_shift"][0]

    def put(col, vec):
        pv[:len(vec), col] = vec
    for j in range(4):
        put(j, mu[j * 128:(j + 1) * 128])
        put(5 + j, mu[576 + j * 128:576 + (j + 1) * 128])
        put(9 + j, mu[1088 + j * 128:1088 + (j + 1) * 128])
        put(15 + j, inp["rw_w0"][0][j * 128:(j + 1) * 128])
        put(19 + j, inp["rw_a0"][0][j * 128:(j + 1) * 128])
        put(23 + j, inp["rw_k_k"][0][j * 128:(j + 1) * 128])
        put(27 + j, inp["rw_k_a"][0][j * 128:(j + 1) * 128])
        put(31 + j, inp["rw_r_k"][0].reshape(-1)[j * 128:(j + 1) * 128])
        put(35 + j, inp["rw_ln_w"][0][j * 128:(j + 1) * 128])
        put(39 + j, inp["rw_ln_b"][0][j * 128:(j + 1) * 128])
    put(4, mu[512:576])
    put(13, mu[1600:1664])
    put(14, mu[1664:1792])
    put(43, np.tile(inp["fox_q_norm_w"][0], 2))
    put(44, np.tile(inp["fox_k_norm_w"][0], 2))
    put(45, inp["fox_b_f"][0])
    for kt in range(8):
        put(46 + kt, inp["norm_mix_w"][0][kt * 128:(kt + 1) * 128])
        put(54 + kt, inp["norm_ffn_w"][0][kt * 128:(kt + 1) * 128])
    return pv


def build(TP, TO, n_exp=NE, dbg=False, stop=None):
    TT = TP + TO
    NB = TT // 512
    NBP = TP // 512
    NBO = TO // 512
    NTL = TT // 128
    nc = bass.Bass("TRN2", target_bir_lowering=False)

    def din(name, shape, dt=F32):
        return nc.dram_tensor(name, shape, dt, kind="ExternalInput").ap()
    xp = din("xp", [TP, D])
    xo = din("xo", [TO, D])
    w_in = din("w_in", [D, NCOL])
    pvd = din("pv", [128, PV_N])
    w2d = din("rw_w2", [64, 512])
    a2d = din("rw_a2", [64, 512])
    g2d = din("rw_g2", [128, 512])
    w_out = din("w_out", [D, D])
    rgw = din("rgw", [D, 8])
    rew = din("rew", [D, 64])
    rbd = din("rb", [1, 72])
    if stop is None:
        wgd = din("wg", [NE, D, 512])
        wud = din("wu", [NE, D, 512])
        wdd = din("wd", [NE, 512, D])
    kbd = din("kb", [128, NTL])
    mask4d = din("mask4", [64, 2, 256])
    maskLd = din("maskL", [64, 2, 64])
    bdd = din("bdmask", [128, 128])
    identd = din("ident", [128, 128])
    rmaskd = din("rmask", [128, 512])
    sel127d = din("sel127", [128, 128])
    nmaskd = din("nmask", [128, 4, 512])
    out = nc.dram_tensor("out", [TO, D], F32, kind="ExternalOutput").ap()
    PT = nc.dram_tensor("PTo", [NCOL, TT + 1], F32, kind="ExternalOutput" if dbg else "Internal").ap()
    YT = nc.dram_tensor("YTo", [D, TO], BF16, kind="ExternalOutput" if dbg else "Internal").ap()

    P = Prog(nc)

    pe_hist = {}
    pe_idx = [0]

    def pe_serial(bank, base):
        pe_idx[0] += 1
        import os as _os2
        if _os2.environ.get("PESER") == "1":
            return True
        ser = False
        h = pe_hist.get(bank)
        if h is not None and h[0] != base and pe_idx[0] - h[1] <= 80:
            ser = True
        pe_hist[bank] = (base, pe_idx[0])
        return ser

    def mm(o, lhsT, rhs, start, stop, reads, writes):
        ser = pe_serial(writes[0], lhsT.base_partition())
        return P.op("pe", lambda e: e.matmul(o, lhsT=lhsT, rhs=rhs, start=start, stop=stop), reads, writes, serial=ser, inc=bool(stop))

    def tr(o, i, idn, reads, writes, last=True):
        ser = pe_serial(writes[0], i.base_partition())
        return P.op("pe", lambda e: e.transpose(o, i, idn), reads, writes, serial=ser, inc=last)

    def act(o, i, func, reads, writes, bias=None, scale=None, accum_out=None):
        kw = {}
        if bias is not None:
            kw["bias"] = bias
        if scale is not None:
            kw["scale"] = scale
        if accum_out is not None:
            kw["accum_out"] = accum_out
        return P.op("act", lambda e: e.activation(out=o, in_=i, func=func, **kw), reads, writes)

    def ts(eng, o, i, s1, s2, op0, op1, reads, writes):
        if s2 is None:
            return P.op(eng, lambda e: e.tensor_scalar(out=o, in0=i, scalar1=s1, scalar2=None, op0=op0), reads, writes)
        return P.op(eng, lambda e: e.tensor_scalar(out=o, in0=i, scalar1=s1, scalar2=s2, op0=op0, op1=op1), reads, writes)

    def tt(eng, o, a, b, op, reads, writes):
        return P.op(eng, lambda e: e.tensor_tensor(out=o, in0=a, in1=b, op=op), reads, writes)

    def stt(o, a, s, b, op0, op1, reads, writes):
        return P.op("dve", lambda e: e.scalar_tensor_tensor(out=o, in0=a, scalar=s, in1=b, op0=op0, op1=op1), reads, writes)

    def cp(eng, o, i, reads, writes):
        if eng == "act":
            return P.op("act", lambda e: e.activation(out=o, in_=i, func=AF.Copy), reads, writes)
        return P.op(eng, lambda e: e.tensor_copy(out=o, in_=i), reads, writes)

    def dma(eng, slot, o, i, reads, writes, group=None):
        return P.dma(eng, slot, lambda e: e.dma_start(out=o, in_=i), reads, writes, group=group)

    def memset(eng, o, val, writes):
        return P.op(eng, lambda e: e.memset(o, val), (), writes)

    with contextlib.ExitStack() as top:
        def sbt(st, name, shape, dt=F32):
            return st.enter_context(nc.sbuf_tensor("s_" + name, shape, dt))
        psb = [top.enter_context(nc.psum_tensor("ps%d" % i, [128, 512], F32)) for i in range(8)]
        ps_i = [0]

        def nps():
            i = ps_i[0] % 8
            ps_i[0] += 1
            return psb[i], "ps%d" % i

        pv = sbt(top, "pv", [128, PV_N])
        pv1 = sbt(top, "pv1", [128, PV_N])
        ident = sbt(top, "ident", [128, 128])
        bdm = sbt(top, "bdm", [128, 128])
        dma("sp", "g_top", pv[:], pvd, (), ["pv"], group="top")
        dma("sp", "g_top", ident[:], identd, (), ["ident"], group="top")
        dma("sp", "g_top", bdm[:], bdd, (), ["bdm"], group="top")
        P.group_end("top", "g_top")
        ts("dve", pv1[:], pv[:], -1.0, 1.0, ALU.mult, ALU.add, ["pv"], ["pv1"])
        import os as _os4
        for _ in range(int(_os4.environ.get("PENOPS", "0"))):
            pt_, pk_ = nps()
            mm(pt_[:, 0:128], ident[:], bdm[:], True, True, ["ident", "bdm"], [pk_])
        for _ in range(int(_os4.environ.get("NOPS", "0"))):
            ts("dve", pv1[:], pv[:], -1.0, 1.0, ALU.mult, ALU.add, ["pv"], ["pv1"])
            P.op("act", lambda e: e.activation(out=pv1[:], in_=pv[:], func=AF.Copy), ["pv"], ["pv1"])
            ts("dve", pv1[:], pv[:], -1.0, 1.0, ALU.mult, ALU.add, ["pv"], ["pv1"])
            ts("dve", pv1[:], pv[:], -1.0, 1.0, ALU.mult, ALU.add, ["pv"], ["pv1"])

        def pvc(col, n=128):
            return pv[0:n, col:col + 1]

        def pv1c(col, n=128):
            return pv1[0:n, col:col + 1]

        chunks = []
        c0 = 0
        while c0 < NCOL:
            n = min(128, NCOL - c0)
            for bnd in (512, 576, 1600, 1664):
                if c0 < bnd < c0 + n:
                    n = bnd - c0
            chunks.append((c0, n))
            c0 += n
        with contextlib.ExitStack() as st:
            win = sbt(st, "win", [128, 8, NCOL], BF16)
            for kt in range(8):
                for pc in range(3):
                    dma("pool", "g_win", win[:, kt, pc * 1112:(pc + 1) * 1112], w_in[kt * 128:(kt + 1) * 128, pc * 1112:(pc + 1) * 1112], (), ["win%d_%d" % (kt, pc)], group="win")
            P.group_end("win", "g_win")
            for kt in range(8):
                P.lastw["win%d" % kt] = P.lastw["win0_0"]
            xtm = [sbt(st, "xtm%d" % i, [128, 4, D]) for i in range(2)]
            xs = sbt(st, "xs", [128, 4, D])
            junk = sbt(st, "junk", [128, D], BF16)
            ssq = sbt(st, "ssq", [128, 4])
            hT = [sbt(st, "hT%d" % i, [128, 8, 512], BF16) for i in range(2)]
            stg = [sbt(st, "stg%d" % i, [128, 512]) for i in range(4)]
            zc = sbt(st, "zc", [128, 1])
            memset("dve", zc[:], 0.0, ["zc"])
            for r0 in range(0, NCOL, 128):
                n = min(128, NCOL - r0)
                dma("sp", "zc", PT[r0:r0 + n, 0:1], zc[0:n, :], ["zc"], [])
            sgi = 0
            for b in range(NB):
                q = b % 2
                src = xp[b * 512:(b + 1) * 512, :] if b < NBP else xo[(b - NBP) * 512:(b - NBP + 1) * 512, :]
                dma("sp", "xtm%d" % q, xtm[q][:], src.rearrange("(s p) d -> p s d", p=128), (), ["xtm%d" % q])
                for s in range(4):
                    act(junk[:], xtm[q][:, s, :], AF.Square, ["xtm%d" % q], ["junk", "ssq"], accum_out=ssq[:, s:s + 1])
                act(ssq[:], ssq[:], AF.Ln, ["ssq"], ["ssq"], bias=1e-6, scale=1.0 / D)
                act(ssq[:], ssq[:], AF.Exp, ["ssq"], ["ssq"], scale=-0.5)
                for s in range(4):
                    ts("dve", xs[:, s, :], xtm[q][:, s, :], ssq[:, s:s + 1], None, ALU.mult, None, ["xtm%d" % q, "ssq"], ["xs"])
                for kt in range(8):
                    pt, pk = nps()
                    for s in range(4):
                        tr(pt[:, s * 128:(s + 1) * 128], xs[:, s, kt * 128:(kt + 1) * 128], ident[:], ["xs", "ident"], [pk], last=(s == 3))
                    if kt % 2 == 0:
                        P.op("act", lambda e, o=hT[q][:, kt, :], i=pt[:], sc=pvc(46 + kt): e.activation(out=o, in_=i, func=AF.Copy, scale=sc),
                             [pk, "pv"], ["hT%d" % q])
                    else:
                        ts("dve", hT[q][:, kt, :], pt[:], pvc(46 + kt), None, ALU.mult, None, [pk, "pv"], ["hT%d" % q])
                for (c0, n) in chunks:
                    if b < NBP and 1792 <= c0 < 2304:
                        continue
                    pt, pk = nps()
                    for kt in range(8):
                        mm(pt[0:n, :], win[:, kt, c0:c0 + n], hT[q][:, kt, :], kt == 0, kt == 7, ["win%d" % kt, "hT%d" % q], [pk])
                    si = sgi % 4
                    sgi += 1
                    cp("act" if si % 2 == 0 else "dve", stg[si][0:n, :], pt[0:n, :], [pk], ["stg%d" % si])
                    dma("sp", "stg%d" % si, PT[c0:c0 + n, 1 + b * 512:1 + (b + 1) * 512], stg[si][0:n, :], ["stg%d" % si], [])
            P.barrier()
        if dbg:
            pass
        if stop == 'A':
            P.barrier()
            P.emit()
            return nc

        with contextlib.ExitStack() as st:
            w2s = sbt(st, "w2s", [64, 512])
            a2s = sbt(st, "a2s", [64, 512])
            g2s = sbt(st, "g2s", [128, 512])
            mask4 = sbt(st, "mask4", [64, 2, 256])
            maskL = sbt(st, "maskL", [64, 2, 64])
            rmask = sbt(st, "rmask", [128, 512])
            for nm, t_, d_ in (("w2s", w2s, w2d), ("a2s", a2s, a2d), ("g2s", g2s, g2d), ("mask4", mask4, mask4d),
                               ("maskL", maskL, maskLd), ("rmask", rmask, rmaskd)):
                dma("sp", "g_B", t_[:], d_, (), [nm], group="B")
            P.group_end("B", "g_B")
            raw = [sbt(st, "raw%d" % i, [128, 513]) for i in range(3)]
            tmp = [sbt(st, "tmp%d" % i, [128, 512]) for i in range(2)]
            names = ["pw_s", "pa_s", "pg_s", "th", "sg", "k_s", "kk", "sq", "rn", "kkn", "a_t", "ka", "sig", "cums", "dcs",
                     "E1", "E2", "E3", "t2"]
            T = {nm: sbt(st, nm, [128, 512]) for nm in names}
            SET = []
            for q in range(2):
                S = {}
                S["AR"] = sbt(st, "AR%d" % q, [128, 8, 2, 64])
                for nm in ("Bt", "Kt", "r_s", "k2", "v_s", "g", "yT"):
                    S[nm] = sbt(st, "%s%d" % (nm, q), [128, 512])
                for nm in ("Btok", "Ktok", "Vtok"):
                    S[nm] = sbt(st, "%s%d" % (nm, q), [64, 8, 128])
                S["Vpad"] = sbt(st, "Vpad%d" % q, [64, 8, 2, 128])
                S["gC"] = sbt(st, "gC%d" % q, [128, 8])
                memset("dve", S["Vpad"][:], 0.0, ["Vpad%d" % q])
                SET.append(S)
            CH = []
            for cq in range(2):
                Cq = {}
                Cq["G1"] = sbt(st, "G1%d" % cq, [64, 2, 256])
                Cq["Lm"] = sbt(st, "Lm%d" % cq, [64, 2, 64])
                Cq["NP"] = [sbt(st, "NP%d_%d" % (cq, i), [64, 2, 128]) for i in range(2)]
                Cq["LL"] = [sbt(st, "LL%d_%d" % (cq, i), [64, 2, 64]) for i in range(2)]
                Cq["TT"] = sbt(st, "TTm%d" % cq, [64, 2, 64])
                Cq["Xs"] = sbt(st, "Xs%d" % cq, [64, 128])
                Cq["Us"] = sbt(st, "Us%d" % cq, [64, 128])
                Cq["Upad"] = sbt(st, "Upad%d" % cq, [64, 2, 128])
                memset("dve", Cq["Upad"][:], 0.0, ["Upad%d" % cq])
                CH.append(Cq)
            STbd = sbt(st, "STbd", [128, 128])
            post = {nm: sbt(st, "po_" + nm, [128, 512]) for nm in ("yc", "sq", "rs", "yn", "rk", "bon")}
            outb = sbt(st, "outb", [128, 512], BF16)
            rawi = [0]

            def shift_tile(row0, n, mucol, b, dst, dkey):
                ri = rawi[0] % 3
                rawi[0] += 1
                rk_ = "raw%d" % ri
                dma("sp", rk_, raw[ri][0:n, :], PT[row0:row0 + n, b * 512:b * 512 + 513], (), [rk_])
                tq = ri % 2
                P.op("act", lambda e, o=tmp[tq][0:n, :], i=raw[ri][0:n, 0:512], sc=pvc(mucol, n): e.activation(out=o, in_=i, func=AF.Copy, scale=sc),
                     [rk_, "pv"], ["tmp%d" % tq])
                stt(dst[0:n, :], raw[ri][0:n, 1:513], pv1c(mucol, n), tmp[tq][0:n, :], ALU.mult, ALU.add,
                    [rk_, "pv1", "tmp%d" % tq], [dkey])

            import os as _os3
            for _ in range(int(_os3.environ.get("PSSHIFT", "0"))):
                nps()
            for j in range(int(_os3.environ.get("JSTART", "0")), 4):
                memset("dve", STbd[:], 0.0, ["STbd"])
                for b in range(NB):
                    q = b % 2
                    S = SET[q]
                    sk = lambda nm: "%s%d" % (nm, q)
                    own = b >= NBP
                    shift_tile(512, 64, 4, b, T["pw_s"], "pw_s")
                    shift_tile(1600, 64, 13, b, T["pa_s"], "pa_s")
                    shift_tile(1664, 128, 14, b, T["pg_s"], "pg_s")
                    shift_tile(j * 128, 128, j, b, S["r_s"], sk("r_s"))
                    shift_tile(576 + j * 128, 128, 5 + j, b, T["k_s"], "k_s")
                    shift_tile(1088 + j * 128, 128, 9 + j, b, S["v_s"], sk("v_s"))
                    act(T["th"][0:64, :], T["pw_s"][0:64, :], AF.Tanh, ["pw_s"], ["th"])
                    pt, pk = nps()
                    mm(pt[:], w2s[0:64, j * 128:(j + 1) * 128], T["th"][0:64, :], True, True, ["w2s", "th"], [pk])
                    act(T["sig"][:], pt[:], AF.Sigmoid, [pk, "pv"], ["sig"], bias=pvc(15 + j))
                    pt, pk = nps()
                    mm(pt[:], a2s[0:64, j * 128:(j + 1) * 128], T["pa_s"][0:64, :], True, True, ["a2s", "pa_s"], [pk])
                    act(T["a_t"][:], pt[:], AF.Sigmoid, [pk, "pv"], ["a_t"], bias=pvc(19 + j))
                    act(T["sg"][:], T["pg_s"][:], AF.Sigmoid, ["pg_s"], ["sg"])
                    pt, pk = nps()
                    mm(pt[:], g2s[:, j * 128:(j + 1) * 128], T["sg"][:], True, True, ["g2s", "sg"], [pk])
                    cp("dve", S["g"][:], pt[:], [pk], [sk("g")])
                    ts("dve", T["kk"][:], T["k_s"][:], pvc(23 + j), None, ALU.mult, None, ["k_s", "pv"], ["kk"])
                    tt("dve", T["sq"][:], T["kk"][:], T["kk"][:], ALU.mult, ["kk"], ["sq"])
                    pt, pk = nps()
                    mm(pt[:], bdm[:], T["sq"][:], True, True, ["bdm", "sq"], [pk])
                    act(T["rn"][:], pt[:], AF.Ln, [pk], ["rn"], bias=1e-16)
                    act(T["rn"][:], T["rn"][:], AF.Exp, ["rn"], ["rn"], scale=-0.5)
                    tt("dve", T["kkn"][:], T["kk"][:], T["rn"][:], ALU.mult, ["kk", "rn"], ["kkn"])
                    ts("dve", T["ka"][:], T["a_t"][:], pvc(27 + j), pv1c(27 + j), ALU.mult, ALU.add, ["a_t", "pv", "pv1"], ["ka"])
                    tt("dve", S["k2"][:], T["k_s"][:], T["ka"][:], ALU.mult, ["k_s", "ka"], [sk("k2")])
                    P.op("dve", lambda e, o=T["cums"][:], m=rmask[:], d1=T["sig"][:]: e.tensor_tensor_scan(out=o, data0=m, data1=d1, initial=0.0, op0=ALU.mult, op1=ALU.add),
                         ["rmask", "sig"], ["cums"])
                    tt("dve", T["dcs"][:], T["cums"][:], T["sig"][:], ALU.subtract, ["cums", "sig"], ["dcs"])
                    act(T["E1"][:], T["cums"][:], AF.Exp, ["cums"], ["E1"], scale=DECAY_C)
                    act(T["E2"][:], T["cums"][:], AF.Exp, ["cums"], ["E2"], scale=-DECAY_C)
                    act(T["E3"][:], T["dcs"][:], AF.Exp, ["dcs"], ["E3"], scale=-DECAY_C)
                    stt(S["AR"][:, :, 0, :], T["kkn"][:].rearrange("p (c t) -> p c t", t=64), -1.0,
                        T["E3"][:].rearrange("p (c t) -> p c t", t=64), ALU.mult, ALU.mult, ["kkn", "E3"], [sk("AR")])
                    tt("dve", S["AR"][:, :, 1, :], S["r_s"][:].rearrange("p (c t) -> p c t", t=64),
                       T["E2"][:].rearrange("p (c t) -> p c t", t=64), ALU.mult, [sk("r_s"), "E2"], [sk("AR")])
                    tt("dve", T["t2"][:], T["kkn"][:], T["a_t"][:], ALU.mult, ["kkn", "a_t"], ["t2"])
                    tt("dve", S["Bt"][:], T["t2"][:], T["E1"][:], ALU.mult, ["t2", "E1"], [sk("Bt")])
                    tt("dve", S["Kt"][:], S["k2"][:], T["E1"][:], ALU.mult, [sk("k2"), "E1"], [sk("Kt")])
                    cp("dve", S["gC"][:], T["E2"][:].rearrange("p (c t) -> p c t", t=64)[:, :, 63], ["E2"], [sk("gC")])
                    for nm_src, nm_dst in (("Bt", "Btok"), ("Kt", "Ktok"), ("v_s", "Vtok")):
                        for half in range(2):
                            pt, pk = nps()
                            for c4 in range(4):
                                c = half * 4 + c4
                                tr(pt[0:64, c4 * 128:(c4 + 1) * 128], S[nm_src][:, c * 64:(c + 1) * 64], ident[:], [sk(nm_src), "ident"], [pk], last=(c4 == 3))
                            cp("dve" if half == 0 else "dve", S[nm_dst][:, half * 4:(half + 1) * 4, :],
                               pt[0:64, :].rearrange("p (c f) -> p c f", f=128), [pk], [sk(nm_dst)])
                    for h in range(2):
                        cp("dve", S["Vpad"][:, :, h, h * 64:(h + 1) * 64], S["Vtok"][:, :, h * 64:(h + 1) * 64], [sk("Vtok")], [sk("Vpad")])
                    if stop is not None and stop.startswith("Bk") and [int(v) for v in stop[2:].split("_")] == [j, b, -1]:
                        P.barrier()
                        P.emit()
                        return nc
                    if stop == 'B1':
                        P.barrier()
                        P.emit()
                        return nc
                    def make_chunk(c, S=S, sk=sk, own=own):
                        cq = c % 2
                        Cq = CH[cq]
                        ck = lambda nm, cq=cq: "%s%d" % (nm, cq)
                        csl = slice(c * 64, (c + 1) * 64)
                        NPc, LLc = Cq["NP"], Cq["LL"]

                        def gram():
                            pg_, pgk = nps()
                            pl_, plk = nps()
                            for h in range(2):
                                hs = slice(h * 64, (h + 1) * 64)
                                mm(pg_[0:64, h * 256:h * 256 + 128], S["Bt"][hs, csl], S["AR"][hs, c, :, :], True, True, [sk("Bt"), sk("AR")], [pgk])
                                mm(pg_[0:64, h * 256 + 128:h * 256 + 256], S["Kt"][hs, csl], S["AR"][hs, c, :, :], True, True, [sk("Kt"), sk("AR")], [pgk])
                                mm(pl_[0:64, h * 64:(h + 1) * 64], S["AR"][hs, c, 0, :], S["Bt"][hs, csl], True, True, [sk("AR"), sk("Bt")], [plk])
                            tt("dve", Cq["G1"][:], pg_[0:64, :].rearrange("p (h f) -> p h f", h=2), mask4[:], ALU.mult, [pgk, "mask4"], [ck("G1")])
                            tt("dve", Cq["Lm"][:], pl_[0:64, 0:128].rearrange("p (h f) -> p h f", h=2), maskL[:], ALU.mult, [plk, "maskL"], [ck("Lm")])

                        def d0():
                            pd, pdk = nps()
                            for h in range(2):
                                mm(pd[0:64, h * 192:h * 192 + 64], Cq["Lm"][:, h, :], Cq["G1"][:, h, 0:64], True, True, [ck("Lm"), ck("G1")], [pdk])
                                mm(pd[0:64, h * 192 + 128:h * 192 + 192], Cq["G1"][:, h, 0:64], Cq["Lm"][:, h, :], True, True, [ck("Lm"), ck("G1")], [pdk])
                            pdv = pd[0:64, 0:384].rearrange("p (h f) -> p h f", h=2)
                            cp("dve", NPc[1][:, :, 0:64], pdv[:, :, 0:64], [pdk], [ck("NP1")])
                            cp("dve", LLc[1][:], pdv[:, :, 128:192], [pdk], [ck("LL1")])
                            for h in range(2):
                                tt("dve", NPc[1][:, h, 64:128], Cq["G1"][:, h, 0:64], ident[0:64, 0:64], ALU.add, [ck("G1"), "ident"], [ck("NP1")])

                        def dstage(i):
                            def f():
                                cur, nxt = i % 2, (i + 1) % 2
                                pd, pdk = nps()
                                for h in range(2):
                                    mm(pd[0:64, h * 192:h * 192 + 128], LLc[cur][:, h, :], NPc[cur][:, h, :], True, True,
                                       [ck("LL%d" % cur), ck("NP%d" % cur)], [pdk])
                                    if i < 5:
                                        mm(pd[0:64, h * 192 + 128:h * 192 + 192], NPc[cur][:, h, 0:64], LLc[cur][:, h, :], True, True,
                                           [ck("LL%d" % cur), ck("NP%d" % cur)], [pdk])
                                pdv = pd[0:64, 0:384].rearrange("p (h f) -> p h f", h=2)
                                if i < 5:
                                    cp("dve", NPc[nxt][:, :, 0:64], pdv[:, :, 0:64], [pdk], [ck("NP%d" % nxt)])
                                    cp("dve", LLc[nxt][:], pdv[:, :, 128:192], [pdk], [ck("LL%d" % nxt)])
                                    tt("dve", NPc[nxt][:, :, 64:128], pdv[:, :, 64:128], NPc[cur][:, :, 64:128], ALU.add,
                                       [pdk, ck("NP%d" % cur)], [ck("NP%d" % nxt)])
                                else:
                                    tt("dve", Cq["TT"][:], pdv[:, :, 64:128], NPc[cur][:, :, 64:128], ALU.add, [pdk, ck("NP%d" % cur)], [ck("TT")])
                            return f

                        def sx():
                            px, pxk = nps()
                            mm(px[0:64, 0:128], S["AR"][:, c, 0, :], STbd[:], True, False, [sk("AR"), "STbd"], [pxk])
                            for h in range(2):
                                mm(px[0:64, h * 64:(h + 1) * 64], Cq["G1"][:, h, 128:192], S["Vtok"][:, c, h * 64:(h + 1) * 64], False, h == 1,
                                   [ck("G1"), sk("Vtok")], [pxk])
                            cp("dve", Cq["Xs"][:], px[0:64, 0:128], [pxk], [ck("Xs")])

                        def su():
                            pu, puk = nps()
                            for h in range(2):
                                mm(pu[0:64, h * 64:(h + 1) * 64], Cq["TT"][:, h, :], Cq["Xs"][:, h * 64:(h + 1) * 64], True, True, [ck("TT"), ck("Xs")], [puk])
                            cp("dve", Cq["Us"][:], pu[0:64, 0:128], [puk], [ck("Us")])

                        def sy():
                            if not own:
                                return
                            for h in range(2):
                                cp("dve", Cq["Upad"][:, h, h * 64:(h + 1) * 64], Cq["Us"][:, h * 64:(h + 1) * 64], [ck("Us")], [ck("Upad")])
                            py, pyk = nps()
                            mm(py[:, 0:64], STbd[:], S["AR"][:, c, 1, :], True, False, ["STbd", sk("AR")], [pyk])
                            for h in range(2):
                                mm(py[:, 0:64], Cq["Upad"][:, h, :], Cq["G1"][:, h, 64:128], False, False, [ck("Upad"), ck("G1")], [pyk])
                                mm(py[:, 0:64], S["Vpad"][:, c, h, :], Cq["G1"][:, h, 192:256], False, h == 1, [sk("Vpad"), ck("G1")], [pyk])
                            cp("dve", S["yT"][:, csl], py[:, 0:64], [pyk], [sk("yT")])

                        def ss():
                            pss, psk = nps()
                            mm(pss[:, 0:128], S["Btok"][:, c, :], Cq["Us"][:], True, False, [sk("Btok"), ck("Us")], [psk])
                            mm(pss[:, 0:128], S["Ktok"][:, c, :], S["Vtok"][:, c, :], False, False, [sk("Ktok"), sk("Vtok")], [psk])
                            mm(pss[:, 0:128], ident[:], STbd[:], False, True, ["ident", "STbd"], [psk])
                            stt(STbd[:], pss[:, 0:128], S["gC"][:, c:c + 1], bdm[:], ALU.mult, ALU.mult, [psk, sk("gC"), "bdm"], ["STbd"])
                        return [gram, d0] + [dstage(i) for i in range(1, 6)], [sx, su, sy, ss]

                    chunks_ = [make_chunk(c) for c in range(8)]
                    for f in chunks_[0][0]:
                        f()
                    for c in range(8):
                        Dn = chunks_[c + 1][0] if c + 1 < 8 else []
                        Sc = chunks_[c][1]
                        order = []
                        for k_ in range(max(len(Dn), len(Sc))):
                            if k_ < len(Dn):
                                order.append(Dn[k_])
                            if k_ < len(Sc):
                                order.append(Sc[k_])
                        for f in order:
                            f()
                    if own:
                        pm, pmk = nps()
                        mm(pm[:], bdm[:], S["yT"][:], True, True, ["bdm", sk("yT")], [pmk])
                        stt(post["yc"][:], pm[:], -1.0 / 64, S["yT"][:], ALU.mult, ALU.add, [pmk, sk("yT")], ["po_yc"])
                        tt("dve", post["sq"][:], post["yc"][:], post["yc"][:], ALU.mult, ["po_yc"], ["po_sq"])
                        pm, pmk = nps()
                        mm(pm[:], bdm[:], post["sq"][:], True, True, ["bdm", "po_sq"], [pmk])
                        act(post["rs"][:], pm[:], AF.Ln, [pmk], ["po_rs"], bias=64e-5, scale=1.0 / 64)
                        act(post["rs"][:], post["rs"][:], AF.Exp, ["po_rs"], ["po_rs"], scale=-0.5)
                        tt("dve", post["yn"][:], post["yc"][:], post["rs"][:], ALU.mult, ["po_yc", "po_rs"], ["po_yn"])
                        ts("dve", post["yn"][:], post["yn"][:], pvc(35 + j), pvc(39 + j), ALU.mult, ALU.add, ["po_yn", "pv"], ["po_yn"])
                        stt(post["rk"][:], S["r_s"][:], pvc(31 + j), S["k2"][:], ALU.mult, ALU.mult, [sk("r_s"), sk("k2"), "pv"], ["po_rk"])
                        pm, pmk = nps()
                        mm(pm[:], bdm[:], post["rk"][:], True, True, ["bdm", "po_rk"], [pmk])
                        tt("dve", post["bon"][:], pm[:], S["v_s"][:], ALU.mult, [pmk, sk("v_s")], ["po_bon"])
                        tt("dve", post["bon"][:], post["bon"][:], post["yn"][:], ALU.add, ["po_bon", "po_yn"], ["po_bon"])
                        tt("dve", outb[:], post["bon"][:], S["g"][:], ALU.mult, ["po_bon", sk("g")], ["outb"])
                        bo = b - NBP
                        dma("sp", "outb", YT[j * 128:(j + 1) * 128, bo * 512:(bo + 1) * 512], outb[:], ["outb"], [])
                    if _os3.environ.get("BLKBAR") == "1":
                        P.barrier()
                    if stop is not None and stop.startswith("Bm") and [int(v) for v in stop[2:].split("_")] == [j, b]:
                        P.barrier()
                        P.emit()
                        return nc
                    if stop is not None and stop.startswith("Bn") and int(stop[2:]) == b:
                        P.barrier()
                        P.emit()
                        return nc
                if stop is not None and stop.startswith("Bj") and int(stop[2:]) == j:
                    P.barrier()
                    P.emit()
                    return nc
            P.barrier()

        if stop == 'B':
            P.barrier()
            P.emit()
            return nc
        with contextlib.ExitStack() as st:
            nmk = sbt(st, "nmk", [128, 4, 512])
            sel127 = sbt(st, "sel127", [128, 128])
            kbs = sbt(st, "kbs", [128, NTL])
            dma("sp", "g_C", nmk[:], nmaskd, (), ["nmk"], group="C")
            dma("sp", "g_C", sel127[:], sel127d, (), ["sel127"], group="C")
            dma("sp", "g_C", kbs[:], kbd, (), ["kbs"], group="C")
            fT = sbt(st, "fT", [8, TT])
            cnT = sbt(st, "cnT", [8, TT])
            cntok = sbt(st, "cntok", [128, NTL, 8])
            cref = sbt(st, "cref", [128, 8])
            biasq = sbt(st, "biasq", [128, NTL, 8])
            qahi = sbt(st, "qahi", [8, TO], BF16)
            qalo = sbt(st, "qalo", [8, TO], BF16)
            KTa = sbt(st, "KTa", [66, TT], BF16)
            QTa = sbt(st, "QTa", [66, TO], BF16)
            Vx = sbt(st, "Vx", [128, NTL, 65], BF16)
            ld = [sbt(st, "ld%d" % i, [64, 512]) for i in range(2)]
            sq2 = sbt(st, "sq2", [64, 512])
            rn2 = sbt(st, "rn2", [64, 512])
            Pt = [sbt(st, "Pt%d" % i, [128, 512], BF16) for i in range(3)]
            mt = [sbt(st, "mt%d" % i, [128, 512]) for i in range(2)]
            osb = sbt(st, "osb", [65, 512])
            ones65 = sbt(st, "ones65", [65, 64])
            yfx = sbt(st, "yfx", [64, 512], BF16)
            memset("dve", ones65[:], 1.0, ["ones65"])
            memset("dve", Vx[:], 1.0, ["Vx"])
            memset("dve", KTa[64:66, :], 1.0, ["KTa"])
            dma("sp", "g_C", fT[:], PT[3328:3336, 1:TT + 1], (), ["fT"], group="C")
            P.group_end("C", "g_C")
            nbf = sbt(st, "nbf", [8, 1])
            ts("dve", nbf[:], pv[0:8, 45:46], -1.0, None, ALU.mult, None, ["pv"], ["nbf"])
            act(fT[:], fT[:], AF.Exp, ["fT", "nbf"], ["fT"], bias=nbf[:, 0:1], scale=-1.0)
            act(fT[:], fT[:], AF.Ln, ["fT"], ["fT"], bias=1.0)
            P.op("dve", lambda e: e.tensor_tensor_scan(out=cnT[:], data0=ones65[0:8, 0:1].to_broadcast([8, TT]), data1=fT[:], initial=0.0, op0=ALU.mult, op1=ALU.add),
                 ["fT", "ones65"], ["cnT"])
            for g0 in range(0, NTL, 64):
                gn = min(64, NTL - g0)
                pt, pk = nps()
                for tl in range(gn):
                    tr(pt[:, tl * 8:(tl + 1) * 8], cnT[0:8, (g0 + tl) * 128:(g0 + tl + 1) * 128], ident[0:8, 0:8], ["cnT", "ident"], [pk], last=(tl == gn - 1))
                cp("act", cntok[:, g0:g0 + gn, :], pt[:, 0:gn * 8].rearrange("p (t h) -> p t h", h=8), [pk], ["cntok"])
            for h8 in range(8):
                tt("dve", cntok[:, :, h8], cntok[:, :, h8], kbs[:], ALU.add, ["cntok", "kbs"], ["cntok"])
            qa32 = fT[:, 0:TO]
            for jb in range(NBO):
                e_ = TP + (jb + 1) * 512 - 1
                ts("dve", fT[:, jb * 512:(jb + 1) * 512], cnT[0:8, TP + jb * 512:TP + (jb + 1) * 512], cnT[0:8, e_:e_ + 1], -8.0,
                   ALU.subtract, ALU.mult, ["cnT", "fT"], ["qa32", "fT"])
            cp("dve", qahi[:], qa32, ["qa32"], ["qahi"])
            tt("dve", qalo[:], qa32, qahi[:], ALU.subtract, ["qa32", "qahi"], ["qalo"])
            pti = [0]
            mti = [0]
            for hf in range(8):
                dma("sp", "qa_hi", QTa[64:65, :], qahi[hf:hf + 1, :], ["qahi"], ["QTa"])
                dma("sp", "qa_lo", QTa[65:66, :], qalo[hf:hf + 1, :], ["qalo"], ["QTa"])
                for kind, row0, dst, dk, nblk, toff, wcol in (("k", 2304 + hf * 64, KTa, "KTa", NB, 0, 44),
                                                              ("q", 1792 + hf * 64, QTa, "QTa", NBO, TP, 43)):
                    for b in range(nblk):
                        q = b % 2
                        lk = "ld%d" % q
                        dma("sp", lk, ld[q][:], PT[row0:row0 + 64, 1 + toff + b * 512:1 + toff + (b + 1) * 512], (), [lk])
                        tt("dve", sq2[:], ld[q][:], ld[q][:], ALU.mult, [lk], ["sq2"])
                        pt, pk = nps()
                        mm(pt[0:64, :], bdm[0:64, 0:64], sq2[:], True, True, ["bdm", "sq2"], [pk])
                        act(rn2[:], pt[0:64, :], AF.Ln, [pk], ["rn2"], bias=1e-6, scale=1.0 / 64)
                        act(rn2[:], rn2[:], AF.Exp, ["rn2"], ["rn2"], scale=-0.5)
                        stt(dst[0:64, b * 512:(b + 1) * 512], ld[q][:], pvc(wcol, 64), rn2[:], ALU.mult, ALU.mult, [lk, "pv", "rn2"], [dk])
                for b in range(NB):
                    q = b % 2
                    lk = "ld%d" % q
                    dma("sp", lk, ld[q][:], PT[2816 + hf * 64:2816 + (hf + 1) * 64, 1 + b * 512:1 + (b + 1) * 512], (), [lk])
                    pt, pk = nps()
                    for s in range(4):
                        tr(pt[:, s * 64:(s + 1) * 64], ld[q][:, s * 128:(s + 1) * 128], ident[0:64, 0:64], [lk, "ident"], [pk], last=(s == 3))
                    cp("act", Vx[:, b * 4:(b + 1) * 4, 0:64], pt[:, 0:256].rearrange("p (s d) -> p s d", s=4), [pk], ["Vx"])
                for jb in range(NBO):
                    nkt = (TP + (jb + 1) * 512) // 128
                    pt, pk = nps()
                    mm(pt[:, 0:8], sel127[:], cntok[:, nkt - 1, :], True, True, ["sel127", "cntok"], [pk])
                    cp("act", cref[:], pt[:, 0:8], [pk], ["cref"])
                    ts("dve", biasq[:, 0:nkt, hf], cntok[:, 0:nkt, hf], cref[:, hf:hf + 1], None, ALU.subtract, None, ["cntok", "cref"], ["biasq"])
                    po, pok = nps()

                    def score(kt):
                        psc, psk_ = nps()
                        if psk_ == pok:
                            psc, psk_ = nps()
                        mm(psc[:], KTa[0:66, kt * 128:(kt + 1) * 128], QTa[0:66, jb * 512:(jb + 1) * 512], True, True, ["KTa", "QTa"], [psk_])
                        return psc, psk_

                    def finish(kt, psc, psk_):
                        pi = pti[0] % 3
                        pti[0] += 1
                        pkk = "Pt%d" % pi
                        o = kt - (nkt - 4)
                        if o >= 0:
                            mi = mti[0] % 2
                            mti[0] += 1
                            tt("dve", mt[mi][:], psc[:], nmk[:, o, :], ALU.add, [psk_, "nmk"], ["mt%d" % mi])
                            act(Pt[pi][:], mt[mi][:], AF.Exp, ["mt%d" % mi, "biasq"], [pkk], bias=biasq[:, kt, hf:hf + 1], scale=0.125)
                        else:
                            act(Pt[pi][:], psc[:], AF.Exp, [psk_, "biasq"], [pkk], bias=biasq[:, kt, hf:hf + 1], scale=0.125)
                        mm(po[0:65, :], Vx[:, kt, :], Pt[pi][:], kt == 0, kt == nkt - 1, ["Vx", pkk], [pok])

                    q_ = []
                    for kt in range(nkt):
                        q_.append((kt,) + score(kt))
                        if len(q_) > 2:
                            finish(*q_.pop(0))
                    while q_:
                        finish(*q_.pop(0))
                    cp("act", osb[:], po[0:65, :], [pok], ["osb"])
                    P.op("dve", lambda e: e.reciprocal(out=osb[64:65, :], in_=osb[64:65, :]), ["osb"], ["osb"])
                    pt, pk = nps()
                    mm(pt[0:64, :], ones65[64:65, :], osb[64:65, :], True, True, ["ones65", "osb"], [pk])
                    tt("dve", yfx[:], osb[0:64, :], pt[0:64, :], ALU.mult, ["osb", pk], ["yfx"])
                    dma("sp", "yfx", YT[512 + hf * 64:512 + (hf + 1) * 64, jb * 512:(jb + 1) * 512], yfx[:], ["yfx"], [])
            P.barrier()
        if stop == 'C':
            P.barrier()
            P.emit()
            return nc
        TH = min(2048, TO)
        NH = TO // TH
        NTH = TH // 128
        with contextlib.ExitStack() as st:
            wo = sbt(st, "wo", [128, 8, D], BF16)
            for c in range(8):
                dma("pool", "g_D", wo[:, c, :], w_out[c * 128:(c + 1) * 128, :], (), ["wo%d" % c], group="D")
            rw32 = sbt(st, "rw32", [128, 8, 72])
            dma("pool", "g_D", rw32[:, :, 0:8], rgw.rearrange("(kt p) g -> p kt g", p=128), (), ["rw32a"], group="D")
            dma("pool", "g_D", rw32[:, :, 8:72], rew.rearrange("(kt p) g -> p kt g", p=128), (), ["rw32b"], group="D")
            rbs = sbt(st, "rbs", [1, 72])
            dma("pool", "g_D", rbs[:], rbd, (), ["rbs"], group="D")
            P.group_end("D", "g_D")
            ones1 = sbt(st, "ones1", [1, 128])
            memset("dve", ones1[:], 1.0, ["ones1"])
            acc = sbt(st, "acc", [128, NTH, D])
            h2T = sbt(st, "h2T", [128, 8, TH], BF16)
            gates = sbt(st, "gates", [128, NTH, 64])
            ym = [sbt(st, "ym%d" % i, [128, 8, 128], BF16) for i in range(2)]
            xt = [sbt(st, "xt%d" % i, [128, D]) for i in range(2)]
            xs2 = sbt(st, "xs2", [128, D])
            junk2 = sbt(st, "junk2", [128, D], BF16)
            ss2 = sbt(st, "ss2", [128, 1])
            h32 = sbt(st, "h32", [128, 8, 128])
            lg = sbt(st, "lg", [128, 72])
            sm = {nm: sbt(st, "sm_" + nm, [128, 8]) for nm in ("gm", "goh", "ge", "sel", "m1", "k1", "s2", "k2", "gl")}
            sc1 = {nm: sbt(st, "sc_" + nm, [128, 1]) for nm in ("gmax", "gsum", "m1", "m2", "w1", "w2")}
            e3 = sbt(st, "e3", [128, 8, 8])
            wg = [sbt(st, "wg%d" % i, [128, 8, 512], BF16) for i in range(2)]
            wu = [sbt(st, "wu%d" % i, [128, 8, 512], BF16) for i in range(2)]
            wdn = [sbt(st, "wdn%d" % i, [128, 4, D], BF16) for i in range(2)]
            sgl = [sbt(st, "sgl%d" % i, [128, 512]) for i in range(2)]
            hid = [sbt(st, "hid%d" % i, [128, 4, 512], BF16) for i in range(2)]
            for hh in range(NH):
                for i in range(NTH):
                    q = i % 2
                    tok0 = hh * TH + i * 128
                    dma("sp", "ym%d" % q, ym[q][:], YT[:, tok0:tok0 + 128].rearrange("(c p) t -> p c t", p=128), (), ["ym%d" % q])
                    dma("sp", "xt%d" % q, xt[q][:], xo[tok0:tok0 + 128, :], (), ["xt%d" % q])
                    for nh in range(2):
                        pt, pk = nps()
                        ns = slice(nh * 512, (nh + 1) * 512)
                        for c in range(8):
                            mm(pt[:], ym[q][:, c, :], wo[:, c, ns], c == 0, c == 7, ["ym%d" % q, "wo%d" % c], [pk])
                        tt("dve", acc[:, i, ns], pt[:], xt[q][:, ns], ALU.add, [pk, "xt%d" % q], ["acc%d" % i])
                    ak = "acc%d" % i
                    act(junk2[:], acc[:, i, :], AF.Square, [ak], ["junk2", "ss2"], accum_out=ss2[:, 0:1])
                    act(ss2[:], ss2[:], AF.Ln, ["ss2"], ["ss2"], bias=1e-6, scale=1.0 / D)
                    act(ss2[:], ss2[:], AF.Exp, ["ss2"], ["ss2"], scale=-0.5)
                    ts("dve", xs2[:], acc[:, i, :], ss2[:, 0:1], None, ALU.mult, None, [ak, "ss2"], ["xs2"])
                    for half in range(2):
                        pt, pk = nps()
                        for k4 in range(4):
                            kt = half * 4 + k4
                            tr(pt[:, k4 * 128:(k4 + 1) * 128], xs2[:, kt * 128:(kt + 1) * 128], ident[:], ["xs2", "ident"], [pk], last=(k4 == 3))
                        for k4 in range(4):
                            kt = half * 4 + k4
                            if k4 % 2 == 0:
                                P.op("act", lambda e, o=h32[:, kt, :], i_=pt[:, k4 * 128:(k4 + 1) * 128], sc=pvc(54 + kt): e.activation(out=o, in_=i_, func=AF.Copy, scale=sc),
                                     [pk, "pv"], ["h32"])
                            else:
                                ts("dve", h32[:, kt, :], pt[:, k4 * 128:(k4 + 1) * 128], pvc(54 + kt), None, ALU.mult, None, [pk, "pv"], ["h32"])
                    cp("dve", h2T[:, :, i * 128:(i + 1) * 128], h32[:], ["h32"], ["h2T"])
                    pt, pk = nps()
                    for kt in range(8):
                        mm(pt[:, 0:72], h32[:, kt, :], rw32[:, kt, :], kt == 0, False, ["h32", "rw32a", "rw32b"], [pk])
                    mm(pt[:, 0:72], ones1[0:1, :], rbs[0:1, :], False, True, ["ones1", "rbs"], [pk])
                    cp("act", lg[:], pt[:, 0:72], [pk], ["lg"])
                    R = ["lg"]
                    W = ["rt"]
                    P.op("dve", lambda e: e.reduce_max(out=sc1["gmax"][:], in_=lg[:, 0:8], axis=mybir.AxisListType.X), R, W)
                    ts("dve", sm["goh"][:], lg[:, 0:8], sc1["gmax"][:, 0:1], None, ALU.is_equal, None, R + W, W)
                    ts("dve", sm["gm"][:], lg[:, 0:8], sc1["gmax"][:, 0:1], None, ALU.subtract, None, R + W, W)
                    act(sm["ge"][:], sm["gm"][:], AF.Exp, W, W, accum_out=sc1["gsum"][:, 0:1])
                    P.op("dve", lambda e: e.reciprocal(out=sc1["gsum"][:], in_=sc1["gsum"][:]), ["rt"], ["rt"])
                    tt("dve", e3[:], lg[:, 8:72].rearrange("p (g e) -> p g e", e=8), sm["goh"][:].unsqueeze(2).to_broadcast([128, 8, 8]), ALU.mult, R + W, W)
                    P.op("dve", lambda e: e.reduce_sum(out=sm["sel"][:], in_=e3[:].rearrange("p g e -> p e g"), axis=mybir.AxisListType.X), W, W)
                    P.op("dve", lambda e: e.reduce_max(out=sc1["m1"][:], in_=sm["sel"][:], axis=mybir.AxisListType.X), W, W)
                    ts("dve", sm["k1"][:], sm["sel"][:], sc1["m1"][:, 0:1], None, ALU.is_equal, None, W, W)
                    stt(sm["s2"][:], sm["k1"][:], -1e30, sm["sel"][:], ALU.mult, ALU.add, W, W)
                    P.op("dve", lambda e: e.reduce_max(out=sc1["m2"][:], in_=sm["s2"][:], axis=mybir.AxisListType.X), W, W)
                    ts("dve", sm["k2"][:], sm["s2"][:], sc1["m2"][:, 0:1], None, ALU.is_equal, None, W, W)
                    tt("dve", sc1["w1"][:], sc1["m1"][:], sc1["m2"][:], ALU.subtract, W, W)
                    act(sc1["w1"][:], sc1["w1"][:], AF.Sigmoid, W, ["rt"])
                    ts("dve", sc1["w2"][:], sc1["w1"][:], -1.0, 1.0, ALU.mult, ALU.add, ["rt"], W)
                    tt("dve", sc1["w1"][:], sc1["w1"][:], sc1["gsum"][:], ALU.mult, W, W)
                    tt("dve", sc1["w2"][:], sc1["w2"][:], sc1["gsum"][:], ALU.mult, W, W)
                    ts("dve", sm["gl"][:], sm["k1"][:], sc1["w1"][:, 0:1], None, ALU.mult, None, W, W)
                    stt(sm["gl"][:], sm["k2"][:], sc1["w2"][:, 0:1], sm["gl"][:], ALU.mult, ALU.add, W, W)
                    tt("dve", gates[:, i, :].rearrange("p (g e) -> p g e", e=8), sm["goh"][:].unsqueeze(2).to_broadcast([128, 8, 8]),
                       sm["gl"][:].unsqueeze(1).to_broadcast([128, 8, 8]), ALU.mult, W, ["gates"])
                for ex in range(n_exp):
                    q = ex % 2
                    dma("pool", "wg%d" % q, wg[q][:], wgd[ex].rearrange("(kt p) f -> p kt f", p=128), (), ["wg%d" % q])
                    dma("pool", "wu%d" % q, wu[q][:], wud[ex].rearrange("(kt p) f -> p kt f", p=128), (), ["wu%d" % q])
                    dma("pool", "wdn%d" % q, wdn[q][:], wdd[ex].rearrange("(fc p) n -> p fc n", p=128), (), ["wdn%d" % q])
                    for sb_ in range(TH // 512):
                        hq = sb_ % 2
                        tsl = slice(sb_ * 512, (sb_ + 1) * 512)
                        for fc in range(4):
                            pg_, pgk = nps()
                            pu_, puk = nps()
                            for kt in range(8):
                                mm(pg_[:], wg[q][:, kt, fc * 128:(fc + 1) * 128], h2T[:, kt, tsl], kt == 0, kt == 7, ["wg%d" % q, "h2T"], [pgk])
                            for kt in range(8):
                                mm(pu_[:], wu[q][:, kt, fc * 128:(fc + 1) * 128], h2T[:, kt, tsl], kt == 0, kt == 7, ["wu%d" % q, "h2T"], [puk])
                            sq_ = fc % 2
                            act(sgl[sq_][:], pg_[:], AF.Silu, [pgk], ["sgl%d" % sq_])
                            tt("dve", hid[hq][:, fc, :], sgl[sq_][:], pu_[:], ALU.mult, ["sgl%d" % sq_, puk], ["hid%d_%d" % (hq, fc)])
                        for t4 in range(4):
                            ti = sb_ * 4 + t4
                            for nh in range(2):
                                po, pok = nps()
                                for fc in range(4):
                                    mm(po[:], hid[hq][:, fc, t4 * 128:(t4 + 1) * 128], wdn[q][:, fc, nh * 512:(nh + 1) * 512], fc == 0, fc == 3,
                                       ["hid%d_%d" % (hq, fc), "wdn%d" % q], [pok])
                                stt(acc[:, ti, nh * 512:(nh + 1) * 512], po[:], gates[:, ti, ex:ex + 1], acc[:, ti, nh * 512:(nh + 1) * 512],
                                    ALU.mult, ALU.add, [pok, "gates", "acc%d" % ti], ["acc%d" % ti])
                for i in range(NTH):
                    tok0 = hh * TH + i * 128
                    dma("sp", "ost%d" % (i % 4), out[tok0:tok0 + 128, :], acc[:, i, :], ["acc%d" % i], ["out%d" % i])
                P.barrier()
        P.barrier()
        P.emit()
    return nc


def make_core_inputs(inputs, c, T, consts, pvt):
    b, th = c // 2, c % 2
    TP = TO = T // 2
    x = inputs["x"]
    d = {}
    d["xp"] = np.ascontiguousarray(x[b, 0:TP]) if th == 1 else np.zeros((TP, D), np.float32)
    d["xo"] = np.ascontiguousarray(x[b, th * TO:(th + 1) * TO])
    d["w_in"] = inputs["w_in"][0]
    d["pv"] = pvt
    d["rw_w2"] = inputs["rw_w2"][0]
    d["rw_a2"] = inputs["rw_a2"][0]
    d["rw_g2"] = inputs["rw_g2"][0]
    d["w_out"] = inputs["w_out"][0]
    d["rgw"] = inputs["router_group_w"][0]
    d["rew"] = inputs["router_expert_w"][0]
    d["rb"] = np.concatenate([inputs["router_group_b"][0], inputs["router_expert_b"][0]])[None, :]
    d["wg"] = inputs["exp_w_gate"][0]
    d["wu"] = inputs["exp_w_up"][0]
    d["wd"] = inputs["exp_w_down"][0]
    kb = np.zeros((128, T // 128), np.float32)
    if th == 0:
        kb[:, :TP // 128] = -30000.0
    d["kb"] = kb
    d.update(consts)
    return d


def kernel(**inputs):
    inputs = {k: np.asarray(v) for k, v in inputs.items()}
    x = inputs["x"]
    B, T, _ = x.shape
    TP = TO = T // 2
    consts = host_consts()
    pvt = host_pv(inputs)
    nc = build(TP, TO)
    ncores = 2 * B
    in_maps = [make_core_inputs(inputs, c, T, consts, pvt) for c in range(ncores)]
    res = run_bass_kernel_spmd(nc, in_maps, core_ids=list(range(ncores)))
    out = np.empty((B, T, D), np.float32)
    for c in range(ncores):
        b, th = c // 2, c % 2
        out[b, th * TO:(th + 1) * TO] = res.results[c]["out"]
    return out
```

```python
import contextlib
import numpy as np
import ml_dtypes
import concourse.bass as bass
import concourse.mybir as mybir
from concourse.bass_utils import run_bass_kernel_spmd

F32 = mybir.dt.float32
BF16 = mybir.dt.bfloat16
AF = mybir.ActivationFunctionType
ALU = mybir.AluOpType

ENGS = ("pe", "act", "dve", "pool", "sp")
EPOCH = 1400
D = 1024
NCOL = 3336
NE = 64
DECAY_C = 0.6065306597126334


class Prog:
    def __init__(self, nc):
        self.nc = nc
        self.ops = {e: [] for e in ENGS}
        self.cnt = {}
        self.seen = {e: {} for e in ENGS}
        self.lastw = {}
        self.readers = {}
        self.eng_n = {e: 0 for e in ENGS}
        self.semkeys = []
        self.dma_n = {}
        self.groups = {}

    def _semkey(self, k):
        if k not in self.cnt:
            self.cnt[k] = 0
            self.semkeys.append(k)
        return k

    def _deps(self, eng, reads, writes):
        deps = {}

        def add(tok):
            if tok is None:
                return
            k, v = tok
            if deps.get(k, 0) < v:
                deps[k] = v
        for b in reads:
            add(self.lastw.get(b))
        for b in writes:
            add(self.lastw.get(b))
            for t in self.readers.get(b, ()):
                add(t)
        waits = []
        for k, v in deps.items():
            if eng == "pe" and k[0] == "pe":
                continue
            if self.seen[eng].get(k, 0) >= v:
                continue
            self.seen[eng][k] = v
            waits.append((k, v))
        return waits

    def _commit(self, tok, reads, writes):
        for b in writes:
            self.lastw[b] = tok
            self.readers[b] = []
        for b in reads:
            self.readers.setdefault(b, []).append(tok)

    def op(self, eng, fn, reads=(), writes=(), serial=False, inc=True):
        waits = self._deps(eng, reads, writes)
        k = self._semkey((eng, self.eng_n[eng] // EPOCH))
        if serial and self.cnt[k] > 0 and self.seen[eng].get(k, 0) < self.cnt[k]:
            self.seen[eng][k] = self.cnt[k]
            waits.append((k, self.cnt[k]))
        if inc:
            self.cnt[k] += 1
            tok = (k, self.cnt[k])
            self.ops[eng].append((waits, fn, (k, 1)))
            if self.cnt[k] >= EPOCH:
                self.eng_n[eng] += EPOCH
        else:
            tok = (k, self.cnt[k] + 1)
            self.ops[eng].append((waits, fn, None))
        self._commit(tok, reads, writes)
        return tok

    def dma(self, eng, slot, fn, reads=(), writes=(), group=None):
        waits = self._deps(eng, reads, writes)
        n = self.dma_n.get(slot, 0)
        self.dma_n[slot] = n + 1
        k = self._semkey(("dma", slot, n // 96))
        self.cnt[k] += 16
        tok = (k, self.cnt[k])
        self.ops[eng].append((waits, fn, (k, 16)))
        self._commit(tok, reads, writes)
        if group is not None:
            self.groups.setdefault(group, []).extend(writes)
        return tok

    def group_end(self, group, slot):
        n = self.dma_n[slot]
        k = ("dma", slot, (n - 1) // 96)
        assert (n - 1) // 96 == 0
        for b in self.groups.pop(group, []):
            self.lastw[b] = (k, self.cnt[k])

    def barrier(self, engs=ENGS):
        for e in engs:
            waits = []
            for k in self.semkeys:
                v = self.cnt[k]
                if v == 0 or self.seen[e].get(k, 0) >= v:
                    continue
                if e == "pe" and k[0] == "pe":
                    continue
                self.seen[e][k] = v
                waits.append((k, v))
            if waits:
                self.ops[e].append((waits, None, None))

    def emit(self):
        nc = self.nc
        with contextlib.ExitStack() as st:
            st.enter_context(nc.allow_non_contiguous_dma(reason="small strided setup / layout DMAs"))
            sems = {}
            print("n_sems", len(self.semkeys), {e: len(v) for e, v in self.ops.items()})
            for i, k in enumerate(self.semkeys):
                sems[k] = st.enter_context(nc.semaphore("s%d" % i))
            block = st.enter_context(nc.Block())
            engmap = {"pe": block.tensor, "act": block.scalar, "dve": block.vector,
                      "pool": block.gpsimd, "sp": block.sync}

            def mk(e):
                def body(engine):
                    for waits, fn, inc in self.ops[e]:
                        for (k, v) in waits:
                            engine.wait_ge(sems[k], v)
                        if fn is not None:
                            ins = fn(engine)
                            if inc is not None:
                                ins.then_inc(sems[inc[0]], inc[1])
                return body
            for e in ENGS:
                if self.ops[e]:
                    engmap[e](mk(e))


def host_consts():
    c = {}
    s = np.arange(64)[:, None]
    t = np.arange(64)[None, :]
    strict = (s < t).astype(np.float32)
    incl = (s <= t).astype(np.float32)
    m4 = np.concatenate([strict, incl, strict, incl], axis=1)
    c["mask4"] = np.ascontiguousarray(np.stack([m4, m4], axis=1))
    ml = (t < s).astype(np.float32)
    c["maskL"] = np.ascontiguousarray(np.stack([ml, ml], axis=1))
    bd = np.zeros((128, 128), np.float32)
    bd[:64, :64] = 1
    bd[64:, 64:] = 1
    c["bdmask"] = bd
    c["ident"] = np.eye(128, dtype=np.float32)
    rm = np.ones((128, 512), np.float32)
    rm[:, ::64] = 0
    c["rmask"] = rm
    sel = np.zeros((128, 128), np.float32)
    sel[127, :] = 1
    c["sel127"] = sel
    ss = np.arange(128)[:, None]
    qq = np.arange(512)[None, :]
    c["nmask"] = np.ascontiguousarray(np.stack([np.where((128 * o + ss) <= qq, 0.0, -240000.0) for o in range(4)], axis=1).astype(np.float32))
    return c


PV_N = 62


def host_pv(inp):
    pv = np.zeros((128, PV_N), np.float32)
    mu = inp["mu_shift"][0]

    def put(col, vec):
        pv[:len(vec), col] = vec
    for j in range(4):
        put(j, mu[j * 128:(j + 1) * 128])
        put(5 + j, mu[576 + j * 128:576 + (j + 1) * 128])
        put(9 + j, mu[1088 + j * 128:1088 + (j + 1) * 128])
        put(15 + j, inp["rw_w0"][0][j * 128:(j + 1) * 128])
        put(19 + j, inp["rw_a0"][0][j * 128:(j + 1) * 128])
        put(23 + j, inp["rw_k_k"][0][j * 128:(j + 1) * 128])
        put(27 + j, inp["rw_k_a"][0][j * 128:(j + 1) * 128])
        put(31 + j, inp["rw_r_k"][0].reshape(-1)[j * 128:(j + 1) * 128])
        put(35 + j, inp["rw_ln_w"][0][j * 128:(j + 1) * 128])
        put(39 + j, inp["rw_ln_b"][0][j * 128:(j + 1) * 128])
    put(4, mu[512:576])
    put(13, mu[1600:1664])
    put(14, mu[1664:1792])
    put(43, np.tile(inp["fox_q_norm_w"][0], 2))
    put(44, np.tile(inp["fox_k_norm_w"][0], 2))
    put(45, inp["fox_b_f"][0])
    for kt in range(8):
        put(46 + kt, inp["norm_mix_w"][0][kt * 128:(kt + 1) * 128])
        put(54 + kt, inp["norm_ffn_w"][0][kt * 128:(kt + 1) * 128])
    return pv


def build(TP, TO, n_exp=NE, dbg=False, stop=None):
    TT = TP + TO
    NB = TT // 512
    NBP = TP // 512
    NBO = TO // 512
    NTL = TT // 128
    nc = bass.Bass("TRN2", target_bir_lowering=False)

    def din(name, shape, dt=F32):
        return nc.dram_tensor(name, shape, dt, kind="ExternalInput").ap()
    xp = din("xp", [TP, D])
    xo = din("xo", [TO, D])
    w_in = din("w_in", [D, NCOL])
    pvd = din("pv", [128, PV_N])
    w2d = din("rw_w2", [64, 512])
    a2d = din("rw_a2", [64, 512])
    g2d = din("rw_g2", [128, 512])
    w_out = din("w_out", [D, D])
    rgw = din("rgw", [D, 8])
    rew = din("rew", [D, 64])
    rbd = din("rb", [1, 72])
    if stop is None:
        wgd = din("wg", [NE, D, 512])
        wud = din("wu", [NE, D, 512])
        wdd = din("wd", [NE, 512, D])
    kbd = din("kb", [128, NTL])
    mask4d = din("mask4", [64, 2, 256])
    maskLd = din("maskL", [64, 2, 64])
    bdd = din("bdmask", [128, 128])
    identd = din("ident", [128, 128])
    rmaskd = din("rmask", [128, 512])
    sel127d = din("sel127", [128, 128])
    nmaskd = din("nmask", [128, 4, 512])
    out = nc.dram_tensor("out", [TO, D], F32, kind="ExternalOutput").ap()
    PT = nc.dram_tensor("PTo", [NCOL, TT + 1], F32, kind="ExternalOutput" if dbg else "Internal").ap()
    YT = nc.dram_tensor("YTo", [D, TO], BF16, kind="ExternalOutput" if dbg else "Internal").ap()

    P = Prog(nc)

    pe_hist = {}
    pe_idx = [0]

    def pe_serial(bank, base):
        pe_idx[0] += 1
        import os as _os2
        if _os2.environ.get("PESER") == "1":
            return True
        ser = False
        h = pe_hist.get(bank)
        if h is not None and h[0] != base and pe_idx[0] - h[1] <= 80:
            ser = True
        pe_hist[bank] = (base, pe_idx[0])
        return ser

    def mm(o, lhsT, rhs, start, stop, reads, writes):
        ser = pe_serial(writes[0], lhsT.base_partition())
        return P.op("pe", lambda e: e.matmul(o, lhsT=lhsT, rhs=rhs, start=start, stop=stop), reads, writes, serial=ser, inc=bool(stop))

    def tr(o, i, idn, reads, writes, last=True):
        ser = pe_serial(writes[0], i.base_partition())
        return P.op("pe", lambda e: e.transpose(o, i, idn), reads, writes, serial=ser, inc=last)

    def act(o, i, func, reads, writes, bias=None, scale=None, accum_out=None):
        kw = {}
        if bias is not None:
            kw["bias"] = bias
        if scale is not None:
            kw["scale"] = scale
        if accum_out is not None:
            kw["accum_out"] = accum_out
        return P.op("act", lambda e: e.activation(out=o, in_=i, func=func, **kw), reads, writes)

    def ts(eng, o, i, s1, s2, op0, op1, reads, writes):
        if s2 is None:
            return P.op(eng, lambda e: e.tensor_scalar(out=o, in0=i, scalar1=s1, scalar2=None, op0=op0), reads, writes)
        return P.op(eng, lambda e: e.tensor_scalar(out=o, in0=i, scalar1=s1, scalar2=s2, op0=op0, op1=op1), reads, writes)

    def tt(eng, o, a, b, op, reads, writes):
        return P.op(eng, lambda e: e.tensor_tensor(out=o, in0=a, in1=b, op=op), reads, writes)

    def stt(o, a, s, b, op0, op1, reads, writes):
        return P.op("dve", lambda e: e.scalar_tensor_tensor(out=o, in0=a, scalar=s, in1=b, op0=op0, op1=op1), reads, writes)

    def cp(eng, o, i, reads, writes):
        if eng == "act":
            return P.op("act", lambda e: e.activation(out=o, in_=i, func=AF.Copy), reads, writes)
        return P.op(eng, lambda e: e.tensor_copy(out=o, in_=i), reads, writes)

    def dma(eng, slot, o, i, reads, writes, group=None):
        return P.dma(eng, slot, lambda e: e.dma_start(out=o, in_=i), reads, writes, group=group)

    def memset(eng, o, val, writes):
        return P.op(eng, lambda e: e.memset(o, val), (), writes)

    with contextlib.ExitStack() as top:
        def sbt(st, name, shape, dt=F32):
            return st.enter_context(nc.sbuf_tensor("s_" + name, shape, dt))
        psb = [top.enter_context(nc.psum_tensor("ps%d" % i, [128, 512], F32)) for i in range(8)]
        ps_i = [0]

        def nps():
            i = ps_i[0] % 8
            ps_i[0] += 1
            return psb[i], "ps%d" % i

        pv = sbt(top, "pv", [128, PV_N])
        pv1 = sbt(top, "pv1", [128, PV_N])
        ident = sbt(top, "ident", [128, 128])
        bdm = sbt(top, "bdm", [128, 128])
        dma("sp", "g_top", pv[:], pvd, (), ["pv"], group="top")
        dma("sp", "g_top", ident[:], identd, (), ["ident"], group="top")
        dma("sp", "g_top", bdm[:], bdd, (), ["bdm"], group="top")
        P.group_end("top", "g_top")
        ts("dve", pv1[:], pv[:], -1.0, 1.0, ALU.mult, ALU.add, ["pv"], ["pv1"])
        import os as _os4
        for _ in range(int(_os4.environ.get("PENOPS", "0"))):
            pt_, pk_ = nps()
            mm(pt_[:, 0:128], ident[:], bdm[:], True, True, ["ident", "bdm"], [pk_])
        for _ in range(int(_os4.environ.get("NOPS", "0"))):
            ts("dve", pv1[:], pv[:], -1.0, 1.0, ALU.mult, ALU.add, ["pv"], ["pv1"])
            P.op("act", lambda e: e.activation(out=pv1[:], in_=pv[:], func=AF.Copy), ["pv"], ["pv1"])
            ts("dve", pv1[:], pv[:], -1.0, 1.0, ALU.mult, ALU.add, ["pv"], ["pv1"])
            ts("dve", pv1[:], pv[:], -1.0, 1.0, ALU.mult, ALU.add, ["pv"], ["pv1"])

        def pvc(col, n=128):
            return pv[0:n, col:col + 1]

        def pv1c(col, n=128):
            return pv1[0:n, col:col + 1]

        chunks = []
        c0 = 0
        while c0 < NCOL:
            n = min(128, NCOL - c0)
            for bnd in (512, 576, 1600, 1664):
                if c0 < bnd < c0 + n:
                    n = bnd - c0
            chunks.append((c0, n))
            c0 += n
        with contextlib.ExitStack() as st:
            win = sbt(st, "win", [128, 8, NCOL], BF16)
            for kt in range(8):
                for pc in range(3):
                    dma("pool", "g_win", win[:, kt, pc * 1112:(pc + 1) * 1112], w_in[kt * 128:(kt + 1) * 128, pc * 1112:(pc + 1) * 1112], (), ["win%d_%d" % (kt, pc)], group="win")
            P.group_end("win", "g_win")
            for kt in range(8):
                P.lastw["win%d" % kt] = P.lastw["win0_0"]
            xtm = [sbt(st, "xtm%d" % i, [128, 4, D]) for i in range(2)]
            xs = sbt(st, "xs", [128, 4, D])
            junk = sbt(st, "junk", [128, D], BF16)
            ssq = sbt(st, "ssq", [128, 4])
            hT = [sbt(st, "hT%d" % i, [128, 8, 512], BF16) for i in range(2)]
            stg = [sbt(st, "stg%d" % i, [128, 512]) for i in range(4)]
            zc = sbt(st, "zc", [128, 1])
            memset("dve", zc[:], 0.0, ["zc"])
            for r0 in range(0, NCOL, 128):
                n = min(128, NCOL - r0)
                dma("sp", "zc", PT[r0:r0 + n, 0:1], zc[0:n, :], ["zc"], [])
            sgi = 0
            for b in range(NB):
                q = b % 2
                src = xp[b * 512:(b + 1) * 512, :] if b < NBP else xo[(b - NBP) * 512:(b - NBP + 1) * 512, :]
                dma("sp", "xtm%d" % q, xtm[q][:], src.rearrange("(s p) d -> p s d", p=128), (), ["xtm%d" % q])
                for s in range(4):
                    act(junk[:], xtm[q][:, s, :], AF.Square, ["xtm%d" % q], ["junk", "ssq"], accum_out=ssq[:, s:s + 1])
                act(ssq[:], ssq[:], AF.Ln, ["ssq"], ["ssq"], bias=1e-6, scale=1.0 / D)
                act(ssq[:], ssq[:], AF.Exp, ["ssq"], ["ssq"], scale=-0.5)
                for s in range(4):
                    ts("dve", xs[:, s, :], xtm[q][:, s, :], ssq[:, s:s + 1], None, ALU.mult, None, ["xtm%d" % q, "ssq"], ["xs"])
                for kt in range(8):
                    pt, pk = nps()
                    for s in range(4):
                        tr(pt[:, s * 128:(s + 1) * 128], xs[:, s, kt * 128:(kt + 1) * 128], ident[:], ["xs", "ident"], [pk], last=(s == 3))
                    if kt % 2 == 0:
                        P.op("act", lambda e, o=hT[q][:, kt, :], i=pt[:], sc=pvc(46 + kt): e.activation(out=o, in_=i, func=AF.Copy, scale=sc),
                             [pk, "pv"], ["hT%d" % q])
                    else:
                        ts("dve", hT[q][:, kt, :], pt[:], pvc(46 + kt), None, ALU.mult, None, [pk, "pv"], ["hT%d" % q])
                for (c0, n) in chunks:
                    if b < NBP and 1792 <= c0 < 2304:
                        continue
                    pt, pk = nps()
                    for kt in range(8):
                        mm(pt[0:n, :], win[:, kt, c0:c0 + n], hT[q][:, kt, :], kt == 0, kt == 7, ["win%d" % kt, "hT%d" % q], [pk])
                    si = sgi % 4
                    sgi += 1
                    cp("act" if si % 2 == 0 else "dve", stg[si][0:n, :], pt[0:n, :], [pk], ["stg%d" % si])
                    dma("sp", "stg%d" % si, PT[c0:c0 + n, 1 + b * 512:1 + (b + 1) * 512], stg[si][0:n, :], ["stg%d" % si], [])
            P.barrier()
        if dbg:
            pass
        if stop == 'A':
            P.barrier()
            P.emit()
            return nc

        with contextlib.ExitStack() as st:
            w2s = sbt(st, "w2s", [64, 512])
            a2s = sbt(st, "a2s", [64, 512])
            g2s = sbt(st, "g2s", [128, 512])
            mask4 = sbt(st, "mask4", [64, 2, 256])
            maskL = sbt(st, "maskL", [64, 2, 64])
            rmask = sbt(st, "rmask", [128, 512])
            for nm, t_, d_ in (("w2s", w2s, w2d), ("a2s", a2s, a2d), ("g2s", g2s, g2d), ("mask4", mask4, mask4d),
                               ("maskL", maskL, maskLd), ("rmask", rmask, rmaskd)):
                dma("sp", "g_B", t_[:], d_, (), [nm], group="B")
            P.group_end("B", "g_B")
            raw = [sbt(st, "raw%d" % i, [128, 513]) for i in range(3)]
            tmp = [sbt(st, "tmp%d" % i, [128, 512]) for i in range(2)]
            names = ["pw_s", "pa_s", "pg_s", "th", "sg", "k_s", "kk", "sq", "rn", "kkn", "a_t", "ka", "sig", "cums", "dcs",
                     "E1", "E2", "E3", "t2"]
            T = {nm: sbt(st, nm, [128, 512]) for nm in names}
            SET = []
            for q in range(2):
                S = {}
                S["AR"] = sbt(st, "AR%d" % q, [128, 8, 2, 64])
                for nm in ("Bt", "Kt", "r_s", "k2", "v_s", "g", "yT"):
                    S[nm] = sbt(st, "%s%d" % (nm, q), [128, 512])
                for nm in ("Btok", "Ktok", "Vtok"):
                    S[nm] = sbt(st, "%s%d" % (nm, q), [64, 8, 128])
                S["Vpad"] = sbt(st, "Vpad%d" % q, [64, 8, 2, 128])
                S["gC"] = sbt(st, "gC%d" % q, [128, 8])
                memset("dve", S["Vpad"][:], 0.0, ["Vpad%d" % q])
                SET.append(S)
            CH = []
            for cq in range(4):
                Cq = {}
                Cq["G1"] = sbt(st, "G1%d" % cq, [64, 2, 256])
                Cq["Lm"] = sbt(st, "Lm%d" % cq, [64, 2, 64])
                Cq["NP"] = [sbt(st, "NP%d_%d" % (cq, i), [64, 2, 128]) for i in range(2)]
                Cq["LL"] = [sbt(st, "LL%d_%d" % (cq, i), [64, 2, 64]) for i in range(2)]
                Cq["TT"] = sbt(st, "TTm%d" % cq, [64, 2, 64])
                Cq["Xs"] = sbt(st, "Xs%d" % cq, [64, 128])
                Cq["Us"] = sbt(st, "Us%d" % cq, [64, 128])
                Cq["Upad"] = sbt(st, "Upad%d" % cq, [64, 2, 128])
                memset("dve", Cq["Upad"][:], 0.0, ["Upad%d" % cq])
                CH.append(Cq)
            STbd = sbt(st, "STbd", [128, 128])
            post = {nm: sbt(st, "po_" + nm, [128, 512]) for nm in ("yc", "sq", "rs", "yn", "rk", "bon")}
            outb = sbt(st, "outb", [128, 512], BF16)
            rawi = [0]

            def shift_tile(row0, n, mucol, b, dst, dkey):
                ri = rawi[0] % 3
                rawi[0] += 1
                rk_ = "raw%d" % ri
                dma("sp", rk_, raw[ri][0:n, :], PT[row0:row0 + n, b * 512:b * 512 + 513], (), [rk_])
                tq = ri % 2
                P.op("act", lambda e, o=tmp[tq][0:n, :], i=raw[ri][0:n, 0:512], sc=pvc(mucol, n): e.activation(out=o, in_=i, func=AF.Copy, scale=sc),
                     [rk_, "pv"], ["tmp%d" % tq])
                stt(dst[0:n, :], raw[ri][0:n, 1:513], pv1c(mucol, n), tmp[tq][0:n, :], ALU.mult, ALU.add,
                    [rk_, "pv1", "tmp%d" % tq], [dkey])

            import os as _os3
            for _ in range(int(_os3.environ.get("PSSHIFT", "0"))):
                nps()
            for j in range(int(_os3.environ.get("JSTART", "0")), 4):
                memset("dve", STbd[:], 0.0, ["STbd"])
                for b in range(NB):
                    q = b % 2
                    S = SET[q]
                    sk = lambda nm: "%s%d" % (nm, q)
                    own = b >= NBP
                    shift_tile(512, 64, 4, b, T["pw_s"], "pw_s")
                    shift_tile(1600, 64, 13, b, T["pa_s"], "pa_s")
                    shift_tile(1664, 128, 14, b, T["pg_s"], "pg_s")
                    shift_tile(j * 128, 128, j, b, S["r_s"], sk("r_s"))
                    shift_tile(576 + j * 128, 128, 5 + j, b, T["k_s"], "k_s")
                    shift_tile(1088 + j * 128, 128, 9 + j, b, S["v_s"], sk("v_s"))
                    act(T["th"][0:64, :], T["pw_s"][0:64, :], AF.Tanh, ["pw_s"], ["th"])
                    pt, pk = nps()
                    mm(pt[:], w2s[0:64, j * 128:(j + 1) * 128], T["th"][0:64, :], True, True, ["w2s", "th"], [pk])
                    act(T["sig"][:], pt[:], AF.Sigmoid, [pk, "pv"], ["sig"], bias=pvc(15 + j))
                    pt, pk = nps()
                    mm(pt[:], a2s[0:64, j * 128:(j + 1) * 128], T["pa_s"][0:64, :], True, True, ["a2s", "pa_s"], [pk])
                    act(T["a_t"][:], pt[:], AF.Sigmoid, [pk, "pv"], ["a_t"], bias=pvc(19 + j))
                    act(T["sg"][:], T["pg_s"][:], AF.Sigmoid, ["pg_s"], ["sg"])
                    pt, pk = nps()
                    mm(pt[:], g2s[:, j * 128:(j + 1) * 128], T["sg"][:], True, True, ["g2s", "sg"], [pk])
                    cp("dve", S["g"][:], pt[:], [pk], [sk("g")])
                    ts("dve", T["kk"][:], T["k_s"][:], pvc(23 + j), None, ALU.mult, None, ["k_s", "pv"], ["kk"])
                    tt("dve", T["sq"][:], T["kk"][:], T["kk"][:], ALU.mult, ["kk"], ["sq"])
                    pt, pk = nps()
                    mm(pt[:], bdm[:], T["sq"][:], True, True, ["bdm", "sq"], [pk])
                    act(T["rn"][:], pt[:], AF.Ln, [pk], ["rn"], bias=1e-16)
                    act(T["rn"][:], T["rn"][:], AF.Exp, ["rn"], ["rn"], scale=-0.5)
                    tt("dve", T["kkn"][:], T["kk"][:], T["rn"][:], ALU.mult, ["kk", "rn"], ["kkn"])
                    ts("dve", T["ka"][:], T["a_t"][:], pvc(27 + j), pv1c(27 + j), ALU.mult, ALU.add, ["a_t", "pv", "pv1"], ["ka"])
                    tt("dve", S["k2"][:], T["k_s"][:], T["ka"][:], ALU.mult, ["k_s", "ka"], [sk("k2")])
                    P.op("dve", lambda e, o=T["cums"][:], m=rmask[:], d1=T["sig"][:]: e.tensor_tensor_scan(out=o, data0=m, data1=d1, initial=0.0, op0=ALU.mult, op1=ALU.add),
                         ["rmask", "sig"], ["cums"])
                    tt("dve", T["dcs"][:], T["cums"][:], T["sig"][:], ALU.subtract, ["cums", "sig"], ["dcs"])
                    act(T["E1"][:], T["cums"][:], AF.Exp, ["cums"], ["E1"], scale=DECAY_C)
                    act(T["E2"][:], T["cums"][:], AF.Exp, ["cums"], ["E2"], scale=-DECAY_C)
                    act(T["E3"][:], T["dcs"][:], AF.Exp, ["dcs"], ["E3"], scale=-DECAY_C)
                    stt(S["AR"][:, :, 0, :], T["kkn"][:].rearrange("p (c t) -> p c t", t=64), -1.0,
                        T["E3"][:].rearrange("p (c t) -> p c t", t=64), ALU.mult, ALU.mult, ["kkn", "E3"], [sk("AR")])
                    tt("dve", S["AR"][:, :, 1, :], S["r_s"][:].rearrange("p (c t) -> p c t", t=64),
                       T["E2"][:].rearrange("p (c t) -> p c t", t=64), ALU.mult, [sk("r_s"), "E2"], [sk("AR")])
                    tt("dve", T["t2"][:], T["kkn"][:], T["a_t"][:], ALU.mult, ["kkn", "a_t"], ["t2"])
                    tt("dve", S["Bt"][:], T["t2"][:], T["E1"][:], ALU.mult, ["t2", "E1"], [sk("Bt")])
                    tt("dve", S["Kt"][:], S["k2"][:], T["E1"][:], ALU.mult, [sk("k2"), "E1"], [sk("Kt")])
                    cp("dve", S["gC"][:], T["E2"][:].rearrange("p (c t) -> p c t", t=64)[:, :, 63], ["E2"], [sk("gC")])
                    for nm_src, nm_dst in (("Bt", "Btok"), ("Kt", "Ktok"), ("v_s", "Vtok")):
                        for half in range(2):
                            pt, pk = nps()
                            for c4 in range(4):
                                c = half * 4 + c4
                                tr(pt[0:64, c4 * 128:(c4 + 1) * 128], S[nm_src][:, c * 64:(c + 1) * 64], ident[:], [sk(nm_src), "ident"], [pk], last=(c4 == 3))
                            cp("dve" if half == 0 else "dve", S[nm_dst][:, half * 4:(half + 1) * 4, :],
                               pt[0:64, :].rearrange("p (c f) -> p c f", f=128), [pk], [sk(nm_dst)])
                    for h in range(2):
                        cp("dve", S["Vpad"][:, :, h, h * 64:(h + 1) * 64], S["Vtok"][:, :, h * 64:(h + 1) * 64], [sk("Vtok")], [sk("Vpad")])
                    if stop is not None and stop.startswith("Bk") and [int(v) for v in stop[2:].split("_")] == [j, b, -1]:
                        P.barrier()
                        P.emit()
                        return nc
                    if stop == 'B1':
                        P.barrier()
                        P.emit()
                        return nc
                    def make_chunk(c, S=S, sk=sk, own=own):
                        cq = c % 4
                        Cq = CH[cq]
                        ck = lambda nm, cq=cq: "%s%d" % (nm, cq)
                        csl = slice(c * 64, (c + 1) * 64)
                        NPc, LLc = Cq["NP"], Cq["LL"]

                        def gram():
                            pg_, pgk = nps()
                            pl_, plk = nps()
                            for h in range(2):
                                hs = slice(h * 64, (h + 1) * 64)
                                mm(pg_[0:64, h * 256:h * 256 + 128], S["Bt"][hs, csl], S["AR"][hs, c, :, :], True, True, [sk("Bt"), sk("AR")], [pgk])
                                mm(pg_[0:64, h * 256 + 128:h * 256 + 256], S["Kt"][hs, csl], S["AR"][hs, c, :, :], True, True, [sk("Kt"), sk("AR")], [pgk])
                                mm(pl_[0:64, h * 64:(h + 1) * 64], S["AR"][hs, c, 0, :], S["Bt"][hs, csl], True, True, [sk("AR"), sk("Bt")], [plk])
                            tt("dve", Cq["G1"][:], pg_[0:64, :].rearrange("p (h f) -> p h f", h=2), mask4[:], ALU.mult, [pgk, "mask4"], [ck("G1")])
                            tt("dve", Cq["Lm"][:], pl_[0:64, 0:128].rearrange("p (h f) -> p h f", h=2), maskL[:], ALU.mult, [plk, "maskL"], [ck("Lm")])

                        def d0():
                            pd, pdk = nps()
                            for h in range(2):
                                mm(pd[0:64, h * 192:h * 192 + 64], Cq["Lm"][:, h, :], Cq["G1"][:, h, 0:64], True, True, [ck("Lm"), ck("G1")], [pdk])
                                mm(pd[0:64, h * 192 + 128:h * 192 + 192], Cq["G1"][:, h, 0:64], Cq["Lm"][:, h, :], True, True, [ck("Lm"), ck("G1")], [pdk])
                            pdv = pd[0:64, 0:384].rearrange("p (h f) -> p h f", h=2)
                            cp("dve", NPc[1][:, :, 0:64], pdv[:, :, 0:64], [pdk], [ck("NP1")])
                            cp("dve", LLc[1][:], pdv[:, :, 128:192], [pdk], [ck("LL1")])
                            for h in range(2):
                                tt("dve", NPc[1][:, h, 64:128], Cq["G1"][:, h, 0:64], ident[0:64, 0:64], ALU.add, [ck("G1"), "ident"], [ck("NP1")])

                        def dstage(i):
                            def f():
                                cur, nxt = i % 2, (i + 1) % 2
                                pd, pdk = nps()
                                for h in range(2):
                                    mm(pd[0:64, h * 192:h * 192 + 128], LLc[cur][:, h, :], NPc[cur][:, h, :], True, True,
                                       [ck("LL%d" % cur), ck("NP%d" % cur)], [pdk])
                                    if i < 5:
                                        mm(pd[0:64, h * 192 + 128:h * 192 + 192], NPc[cur][:, h, 0:64], LLc[cur][:, h, :], True, True,
                                           [ck("LL%d" % cur), ck("NP%d" % cur)], [pdk])
                                pdv = pd[0:64, 0:384].rearrange("p (h f) -> p h f", h=2)
                                if i < 5:
                                    cp("dve", NPc[nxt][:, :, 0:64], pdv[:, :, 0:64], [pdk], [ck("NP%d" % nxt)])
                                    cp("dve", LLc[nxt][:], pdv[:, :, 128:192], [pdk], [ck("LL%d" % nxt)])
                                    tt("dve", NPc[nxt][:, :, 64:128], pdv[:, :, 64:128], NPc[cur][:, :, 64:128], ALU.add,
                                       [pdk, ck("NP%d" % cur)], [ck("NP%d" % nxt)])
                                else:
                                    tt("dve", Cq["TT"][:], pdv[:, :, 64:128], NPc[cur][:, :, 64:128], ALU.add, [pdk, ck("NP%d" % cur)], [ck("TT")])
                            return f

                        def sx():
                            px, pxk = nps()
                            mm(px[0:64, 0:128], S["AR"][:, c, 0, :], STbd[:], True, False, [sk("AR"), "STbd"], [pxk])
                            for h in range(2):
                                mm(px[0:64, h * 64:(h + 1) * 64], Cq["G1"][:, h, 128:192], S["Vtok"][:, c, h * 64:(h + 1) * 64], False, h == 1,
                                   [ck("G1"), sk("Vtok")], [pxk])
                            cp("dve", Cq["Xs"][:], px[0:64, 0:128], [pxk], [ck("Xs")])

                        def su():
                            pu, puk = nps()
                            for h in range(2):
                                mm(pu[0:64, h * 64:(h + 1) * 64], Cq["TT"][:, h, :], Cq["Xs"][:, h * 64:(h + 1) * 64], True, True, [ck("TT"), ck("Xs")], [puk])
                            cp("dve", Cq["Us"][:], pu[0:64, 0:128], [puk], [ck("Us")])

                        def sy():
                            if not own:
                                return
                            for h in range(2):
                                cp("dve", Cq["Upad"][:, h, h * 64:(h + 1) * 64], Cq["Us"][:, h * 64:(h + 1) * 64], [ck("Us")], [ck("Upad")])
                            py, pyk = nps()
                            mm(py[:, 0:64], STbd[:], S["AR"][:, c, 1, :], True, False, ["STbd", sk("AR")], [pyk])
                            for h in range(2):
                                mm(py[:, 0:64], Cq["Upad"][:, h, :], Cq["G1"][:, h, 64:128], False, False, [ck("Upad"), ck("G1")], [pyk])
                                mm(py[:, 0:64], S["Vpad"][:, c, h, :], Cq["G1"][:, h, 192:256], False, h == 1, [sk("Vpad"), ck("G1")], [pyk])
                            cp("dve", S["yT"][:, csl], py[:, 0:64], [pyk], [sk("yT")])

                        def ss():
                            pss, psk = nps()
                            mm(pss[:, 0:128], S["Btok"][:, c, :], Cq["Us"][:], True, False, [sk("Btok"), ck("Us")], [psk])
                            mm(pss[:, 0:128], S["Ktok"][:, c, :], S["Vtok"][:, c, :], False, False, [sk("Ktok"), sk("Vtok")], [psk])
                            mm(pss[:, 0:128], ident[:], STbd[:], False, True, ["ident", "STbd"], [psk])
                            stt(STbd[:], pss[:, 0:128], S["gC"][:, c:c + 1], bdm[:], ALU.mult, ALU.mult, [psk, sk("gC"), "bdm"], ["STbd"])
                        return [gram, d0] + [dstage(i) for i in range(1, 6)], [sx, su, sy, ss]

                    chunks_ = [make_chunk(c) for c in range(8)]
                    dptr = [0] * 8
                    sptr = 0
                    cur = 0
                    while cur < 8:
                        progressed = False
                        if dptr[cur] == len(chunks_[cur][0]):
                            chunks_[cur][1][sptr]()
                            sptr += 1
                            progressed = True
                            if sptr == len(chunks_[cur][1]):
                                sptr = 0
                                cur += 1
                                if cur >= 8:
                                    break
                        for c2 in range(cur, min(8, cur + 4)):
                            if dptr[c2] < len(chunks_[c2][0]):
                                chunks_[c2][0][dptr[c2]]()
                                dptr[c2] += 1
                                progressed = True
                        assert progressed
                    if own:
                        pm, pmk = nps()
                        mm(pm[:], bdm[:], S["yT"][:], True, True, ["bdm", sk("yT")], [pmk])
                        stt(post["yc"][:], pm[:], -1.0 / 64, S["yT"][:], ALU.mult, ALU.add, [pmk, sk("yT")], ["po_yc"])
                        tt("dve", post["sq"][:], post["yc"][:], post["yc"][:], ALU.mult, ["po_yc"], ["po_sq"])
                        pm, pmk = nps()
                        mm(pm[:], bdm[:], post["sq"][:], True, True, ["bdm", "po_sq"], [pmk])
                        act(post["rs"][:], pm[:], AF.Ln, [pmk], ["po_rs"], bias=64e-5, scale=1.0 / 64)
                        act(post["rs"][:], post["rs"][:], AF.Exp, ["po_rs"], ["po_rs"], scale=-0.5)
                        tt("dve", post["yn"][:], post["yc"][:], post["rs"][:], ALU.mult, ["po_yc", "po_rs"], ["po_yn"])
                        ts("dve", post["yn"][:], post["yn"][:], pvc(35 + j), pvc(39 + j), ALU.mult, ALU.add, ["po_yn", "pv"], ["po_yn"])
                        stt(post["rk"][:], S["r_s"][:], pvc(31 + j), S["k2"][:], ALU.mult, ALU.mult, [sk("r_s"), sk("k2"), "pv"], ["po_rk"])
                        pm, pmk = nps()
                        mm(pm[:], bdm[:], post["rk"][:], True, True, ["bdm", "po_rk"], [pmk])
                        tt("dve", post["bon"][:], pm[:], S["v_s"][:], ALU.mult, [pmk, sk("v_s")], ["po_bon"])
                        tt("dve", post["bon"][:], post["bon"][:], post["yn"][:], ALU.add, ["po_bon", "po_yn"], ["po_bon"])
                        tt("dve", outb[:], post["bon"][:], S["g"][:], ALU.mult, ["po_bon", sk("g")], ["outb"])
                        bo = b - NBP
                        dma("sp", "outb", YT[j * 128:(j + 1) * 128, bo * 512:(bo + 1) * 512], outb[:], ["outb"], [])
                    if _os3.environ.get("BLKBAR") == "1":
                        P.barrier()
                    if stop is not None and stop.startswith("Bm") and [int(v) for v in stop[2:].split("_")] == [j, b]:
                        P.barrier()
                        P.emit()
                        return nc
                    if stop is not None and stop.startswith("Bn") and int(stop[2:]) == b:
                        P.barrier()
                        P.emit()
                        return nc
                if stop is not None and stop.startswith("Bj") and int(stop[2:]) == j:
                    P.barrier()
                    P.emit()
                    return nc
            P.barrier()

        if stop == 'B':
            P.barrier()
            P.emit()
            return nc
        with contextlib.ExitStack() as st:
            nmk = sbt(st, "nmk", [128, 4, 512])
            sel127 = sbt(st, "sel127", [128, 128])
            kbs = sbt(st, "kbs", [128, NTL])
            dma("sp", "g_C", nmk[:], nmaskd, (), ["nmk"], group="C")
            dma("sp", "g_C", sel127[:], sel127d, (), ["sel127"], group="C")
            dma("sp", "g_C", kbs[:], kbd, (), ["kbs"], group="C")
            fT = sbt(st, "fT", [8, TT])
            cnT = sbt(st, "cnT", [8, TT])
            cntok = sbt(st, "cntok", [128, NTL, 8])
            cref = sbt(st, "cref", [128, 8])
            biasq = sbt(st, "biasq", [128, NTL, 8])
            qahi = sbt(st, "qahi", [8, TO], BF16)
            qalo = sbt(st, "qalo", [8, TO], BF16)
            KTa = sbt(st, "KTa", [66, TT], BF16)
            QTa = sbt(st, "QTa", [66, TO], BF16)
            Vx = sbt(st, "Vx", [128, NTL, 65], BF16)
            ld = [sbt(st, "ld%d" % i, [64, 512]) for i in range(2)]
            sq2 = sbt(st, "sq2", [64, 512])
            rn2 = sbt(st, "rn2", [64, 512])
            Pt = [sbt(st, "Pt%d" % i, [128, 512], BF16) for i in range(3)]
            mt = [sbt(st, "mt%d" % i, [128, 512]) for i in range(2)]
            osb = sbt(st, "osb", [65, 512])
            ones65 = sbt(st, "ones65", [65, 64])
            yfx = sbt(st, "yfx", [64, 512], BF16)
            memset("dve", ones65[:], 1.0, ["ones65"])
            memset("dve", Vx[:], 1.0, ["Vx"])
            memset("dve", KTa[64:66, :], 1.0, ["KTa"])
            dma("sp", "g_C", fT[:], PT[3328:3336, 1:TT + 1], (), ["fT"], group="C")
            P.group_end("C", "g_C")
            nbf = sbt(st, "nbf", [8, 1])
            ts("dve", nbf[:], pv[0:8, 45:46], -1.0, None, ALU.mult, None, ["pv"], ["nbf"])
            act(fT[:], fT[:], AF.Exp, ["fT", "nbf"], ["fT"], bias=nbf[:, 0:1], scale=-1.0)
            act(fT[:], fT[:], AF.Ln, ["fT"], ["fT"], bias=1.0)
            P.op("dve", lambda e: e.tensor_tensor_scan(out=cnT[:], data0=ones65[0:8, 0:1].to_broadcast([8, TT]), data1=fT[:], initial=0.0, op0=ALU.mult, op1=ALU.add),
                 ["fT", "ones65"], ["cnT"])
            for g0 in range(0, NTL, 64):
                gn = min(64, NTL - g0)
                pt, pk = nps()
                for tl in range(gn):
                    tr(pt[:, tl * 8:(tl + 1) * 8], cnT[0:8, (g0 + tl) * 128:(g0 + tl + 1) * 128], ident[0:8, 0:8], ["cnT", "ident"], [pk], last=(tl == gn - 1))
                cp("act", cntok[:, g0:g0 + gn, :], pt[:, 0:gn * 8].rearrange("p (t h) -> p t h", h=8), [pk], ["cntok"])
            for h8 in range(8):
                tt("dve", cntok[:, :, h8], cntok[:, :, h8], kbs[:], ALU.add, ["cntok", "kbs"], ["cntok"])
            qa32 = fT[:, 0:TO]
            for jb in range(NBO):
                e_ = TP + (jb + 1) * 512 - 1
                ts("dve", fT[:, jb * 512:(jb + 1) * 512], cnT[0:8, TP + jb * 512:TP + (jb + 1) * 512], cnT[0:8, e_:e_ + 1], -8.0,
                   ALU.subtract, ALU.mult, ["cnT", "fT"], ["qa32", "fT"])
            cp("dve", qahi[:], qa32, ["qa32"], ["qahi"])
            tt("dve", qalo[:], qa32, qahi[:], ALU.subtract, ["qa32", "qahi"], ["qalo"])
            pti = [0]
            mti = [0]
            for hf in range(8):
                dma("sp", "qa_hi", QTa[64:65, :], qahi[hf:hf + 1, :], ["qahi"], ["QTa"])
                dma("sp", "qa_lo", QTa[65:66, :], qalo[hf:hf + 1, :], ["qalo"], ["QTa"])
                for kind, row0, dst, dk, nblk, toff, wcol in (("k", 2304 + hf * 64, KTa, "KTa", NB, 0, 44),
                                                              ("q", 1792 + hf * 64, QTa, "QTa", NBO, TP, 43)):
                    for b in range(nblk):
                        q = b % 2
                        lk = "ld%d" % q
                        dma("sp", lk, ld[q][:], PT[row0:row0 + 64, 1 + toff + b * 512:1 + toff + (b + 1) * 512], (), [lk])
                        tt("dve", sq2[:], ld[q][:], ld[q][:], ALU.mult, [lk], ["sq2"])
                        pt, pk = nps()
                        mm(pt[0:64, :], bdm[0:64, 0:64], sq2[:], True, True, ["bdm", "sq2"], [pk])
                        act(rn2[:], pt[0:64, :], AF.Ln, [pk], ["rn2"], bias=1e-6, scale=1.0 / 64)
                        act(rn2[:], rn2[:], AF.Exp, ["rn2"], ["rn2"], scale=-0.5)
                        stt(dst[0:64, b * 512:(b + 1) * 512], ld[q][:], pvc(wcol, 64), rn2[:], ALU.mult, ALU.mult, [lk, "pv", "rn2"], [dk])
                for b in range(NB):
                    q = b % 2
                    lk = "ld%d" % q
                    dma("sp", lk, ld[q][:], PT[2816 + hf * 64:2816 + (hf + 1) * 64, 1 + b * 512:1 + (b + 1) * 512], (), [lk])
                    pt, pk = nps()
                    for s in range(4):
                        tr(pt[:, s * 64:(s + 1) * 64], ld[q][:, s * 128:(s + 1) * 128], ident[0:64, 0:64], [lk, "ident"], [pk], last=(s == 3))
                    cp("act", Vx[:, b * 4:(b + 1) * 4, 0:64], pt[:, 0:256].rearrange("p (s d) -> p s d", s=4), [pk], ["Vx"])
                for jb in range(NBO):
                    nkt = (TP + (jb + 1) * 512) // 128
                    pt, pk = nps()
                    mm(pt[:, 0:8], sel127[:], cntok[:, nkt - 1, :], True, True, ["sel127", "cntok"], [pk])
                    cp("act", cref[:], pt[:, 0:8], [pk], ["cref"])
                    ts("dve", biasq[:, 0:nkt, hf], cntok[:, 0:nkt, hf], cref[:, hf:hf + 1], None, ALU.subtract, None, ["cntok", "cref"], ["biasq"])
                    po, pok = nps()

                    def score(kt):
                        psc, psk_ = nps()
                        if psk_ == pok:
                            psc, psk_ = nps()
                        mm(psc[:], KTa[0:66, kt * 128:(kt + 1) * 128], QTa[0:66, jb * 512:(jb + 1) * 512], True, True, ["KTa", "QTa"], [psk_])
                        return psc, psk_

                    def finish(kt, psc, psk_):
                        pi = pti[0] % 3
                        pti[0] += 1
                        pkk = "Pt%d" % pi
                        o = kt - (nkt - 4)
                        if o >= 0:
                            mi = mti[0] % 2
                            mti[0] += 1
                            tt("dve", mt[mi][:], psc[:], nmk[:, o, :], ALU.add, [psk_, "nmk"], ["mt%d" % mi])
                            act(Pt[pi][:], mt[mi][:], AF.Exp, ["mt%d" % mi, "biasq"], [pkk], bias=biasq[:, kt, hf:hf + 1], scale=0.125)
                        else:
                            act(Pt[pi][:], psc[:], AF.Exp, [psk_, "biasq"], [pkk], bias=biasq[:, kt, hf:hf + 1], scale=0.125)
                        mm(po[0:65, :], Vx[:, kt, :], Pt[pi][:], kt == 0, kt == nkt - 1, ["Vx", pkk], [pok])

                    q_ = []
                    for kt in range(nkt):
                        q_.append((kt,) + score(kt))
                        if len(q_) > 2:
                            finish(*q_.pop(0))
                    while q_:
                        finish(*q_.pop(0))
                    cp("act", osb[:], po[0:65, :], [pok], ["osb"])
                    P.op("dve", lambda e: e.reciprocal(out=osb[64:65, :], in_=osb[64:65, :]), ["osb"], ["osb"])
                    pt, pk = nps()
                    mm(pt[0:64, :], ones65[64:65, :], osb[64:65, :], True, True, ["ones65", "osb"], [pk])
                    tt("dve", yfx[:], osb[0:64, :], pt[0:64, :], ALU.mult, ["osb", pk], ["yfx"])
                    dma("sp", "yfx", YT[512 + hf * 64:512 + (hf + 1) * 64, jb * 512:(jb + 1) * 512], yfx[:], ["yfx"], [])
            P.barrier()
        if stop == 'C':
            P.barrier()
            P.emit()
            return nc
        TH = min(2048, TO)
        NH = TO // TH
        NTH = TH // 128
        with contextlib.ExitStack() as st:
            wo = sbt(st, "wo", [128, 8, D], BF16)
            for c in range(8):
                dma("pool", "g_D", wo[:, c, :], w_out[c * 128:(c + 1) * 128, :], (), ["wo%d" % c], group="D")
            rw32 = sbt(st, "rw32", [128, 8, 72])
            dma("pool", "g_D", rw32[:, :, 0:8], rgw.rearrange("(kt p) g -> p kt g", p=128), (), ["rw32a"], group="D")
            dma("pool", "g_D", rw32[:, :, 8:72], rew.rearrange("(kt p) g -> p kt g", p=128), (), ["rw32b"], group="D")
            rbs = sbt(st, "rbs", [1, 72])
            dma("pool", "g_D", rbs[:], rbd, (), ["rbs"], group="D")
            P.group_end("D", "g_D")
            ones1 = sbt(st, "ones1", [1, 128])
            memset("dve", ones1[:], 1.0, ["ones1"])
            acc = sbt(st, "acc", [128, NTH, D])
            h2T = sbt(st, "h2T", [128, 8, TH], BF16)
            gates = sbt(st, "gates", [128, NTH, 64])
            ym = [sbt(st, "ym%d" % i, [128, 8, 128], BF16) for i in range(2)]
            xt = [sbt(st, "xt%d" % i, [128, D]) for i in range(2)]
            xs2 = sbt(st, "xs2", [128, D])
            junk2 = sbt(st, "junk2", [128, D], BF16)
            ss2 = sbt(st, "ss2", [128, 1])
            h32 = sbt(st, "h32", [128, 8, 128])
            lg = sbt(st, "lg", [128, 72])
            sm = {nm: sbt(st, "sm_" + nm, [128, 8]) for nm in ("gm", "goh", "ge", "sel", "m1", "k1", "s2", "k2", "gl")}
            sc1 = {nm: sbt(st, "sc_" + nm, [128, 1]) for nm in ("gmax", "gsum", "m1", "m2", "w1", "w2")}
            e3 = sbt(st, "e3", [128, 8, 8])
            wg = [sbt(st, "wg%d" % i, [128, 8, 512], BF16) for i in range(2)]
            wu = [sbt(st, "wu%d" % i, [128, 8, 512], BF16) for i in range(2)]
            wdn = [sbt(st, "wdn%d" % i, [128, 4, D], BF16) for i in range(2)]
            sgl = [sbt(st, "sgl%d" % i, [128, 512]) for i in range(2)]
            hid = [sbt(st, "hid%d" % i, [128, 4, 512], BF16) for i in range(2)]
            for hh in range(NH):
                for i in range(NTH):
                    q = i % 2
                    tok0 = hh * TH + i * 128
                    dma("sp", "ym%d" % q, ym[q][:], YT[:, tok0:tok0 + 128].rearrange("(c p) t -> p c t", p=128), (), ["ym%d" % q])
                    dma("sp", "xt%d" % q, xt[q][:], xo[tok0:tok0 + 128, :], (), ["xt%d" % q])
                    for nh in range(2):
                        pt, pk = nps()
                        ns = slice(nh * 512, (nh + 1) * 512)
                        for c in range(8):
                            mm(pt[:], ym[q][:, c, :], wo[:, c, ns], c == 0, c == 7, ["ym%d" % q, "wo%d" % c], [pk])
                        tt("dve", acc[:, i, ns], pt[:], xt[q][:, ns], ALU.add, [pk, "xt%d" % q], ["acc%d" % i])
                    ak = "acc%d" % i
                    act(junk2[:], acc[:, i, :], AF.Square, [ak], ["junk2", "ss2"], accum_out=ss2[:, 0:1])
                    act(ss2[:], ss2[:], AF.Ln, ["ss2"], ["ss2"], bias=1e-6, scale=1.0 / D)
                    act(ss2[:], ss2[:], AF.Exp, ["ss2"], ["ss2"], scale=-0.5)
                    ts("dve", xs2[:], acc[:, i, :], ss2[:, 0:1], None, ALU.mult, None, [ak, "ss2"], ["xs2"])
                    for half in range(2):
                        pt, pk = nps()
                        for k4 in range(4):
                            kt = half * 4 + k4
                            tr(pt[:, k4 * 128:(k4 + 1) * 128], xs2[:, kt * 128:(kt + 1) * 128], ident[:], ["xs2", "ident"], [pk], last=(k4 == 3))
                        for k4 in range(4):
                            kt = half * 4 + k4
                            if k4 % 2 == 0:
                                P.op("act", lambda e, o=h32[:, kt, :], i_=pt[:, k4 * 128:(k4 + 1) * 128], sc=pvc(54 + kt): e.activation(out=o, in_=i_, func=AF.Copy, scale=sc),
                                     [pk, "pv"], ["h32"])
                            else:
                                ts("dve", h32[:, kt, :], pt[:, k4 * 128:(k4 + 1) * 128], pvc(54 + kt), None, ALU.mult, None, [pk, "pv"], ["h32"])
                    cp("dve", h2T[:, :, i * 128:(i + 1) * 128], h32[:], ["h32"], ["h2T"])
                    pt, pk = nps()
                    for kt in range(8):
                        mm(pt[:, 0:72], h32[:, kt, :], rw32[:, kt, :], kt == 0, False, ["h32", "rw32a", "rw32b"], [pk])
                    mm(pt[:, 0:72], ones1[0:1, :], rbs[0:1, :], False, True, ["ones1", "rbs"], [pk])
                    cp("act", lg[:], pt[:, 0:72], [pk], ["lg"])
                    R = ["lg"]
                    W = ["rt"]
                    P.op("dve", lambda e: e.reduce_max(out=sc1["gmax"][:], in_=lg[:, 0:8], axis=mybir.AxisListType.X), R, W)
                    ts("dve", sm["goh"][:], lg[:, 0:8], sc1["gmax"][:, 0:1], None, ALU.is_equal, None, R + W, W)
                    ts("dve", sm["gm"][:], lg[:, 0:8], sc1["gmax"][:, 0:1], None, ALU.subtract, None, R + W, W)
                    act(sm["ge"][:], sm["gm"][:], AF.Exp, W, W, accum_out=sc1["gsum"][:, 0:1])
                    P.op("dve", lambda e: e.reciprocal(out=sc1["gsum"][:], in_=sc1["gsum"][:]), ["rt"], ["rt"])
                    tt("dve", e3[:], lg[:, 8:72].rearrange("p (g e) -> p g e", e=8), sm["goh"][:].unsqueeze(2).to_broadcast([128, 8, 8]), ALU.mult, R + W, W)
                    P.op("dve", lambda e: e.reduce_sum(out=sm["sel"][:], in_=e3[:].rearrange("p g e -> p e g"), axis=mybir.AxisListType.X), W, W)
                    P.op("dve", lambda e: e.reduce_max(out=sc1["m1"][:], in_=sm["sel"][:], axis=mybir.AxisListType.X), W, W)
                    ts("dve", sm["k1"][:], sm["sel"][:], sc1["m1"][:, 0:1], None, ALU.is_equal, None, W, W)
                    stt(sm["s2"][:], sm["k1"][:], -1e30, sm["sel"][:], ALU.mult, ALU.add, W, W)
                    P.op("dve", lambda e: e.reduce_max(out=sc1["m2"][:], in_=sm["s2"][:], axis=mybir.AxisListType.X), W, W)
                    ts("dve", sm["k2"][:], sm["s2"][:], sc1["m2"][:, 0:1], None, ALU.is_equal, None, W, W)
                    tt("dve", sc1["w1"][:], sc1["m1"][:], sc1["m2"][:], ALU.subtract, W, W)
                    act(sc1["w1"][:], sc1["w1"][:], AF.Sigmoid, W, ["rt"])
                    ts("dve", sc1["w2"][:], sc1["w1"][:], -1.0, 1.0, ALU.mult, ALU.add, ["rt"], W)
                    tt("dve", sc1["w1"][:], sc1["w1"][:], sc1["gsum"][:], ALU.mult, W, W)
                    tt("dve", sc1["w2"][:], sc1["w2"][:], sc1["gsum"][:], ALU.mult, W, W)
                    ts("dve", sm["gl"][:], sm["k1"][:], sc1["w1"][:, 0:1], None, ALU.mult, None, W, W)
                    stt(sm["gl"][:], sm["k2"][:], sc1["w2"][:, 0:1], sm["gl"][:], ALU.mult, ALU.add, W, W)
                    tt("dve", gates[:, i, :].rearrange("p (g e) -> p g e", e=8), sm["goh"][:].unsqueeze(2).to_broadcast([128, 8, 8]),
                       sm["gl"][:].unsqueeze(1).to_broadcast([128, 8, 8]), ALU.mult, W, ["gates"])
                for ex in range(n_exp):
                    q = ex % 2
                    dma("pool", "wg%d" % q, wg[q][:], wgd[ex].rearrange("(kt p) f -> p kt f", p=128), (), ["wg%d" % q])
                    dma("pool", "wu%d" % q, wu[q][:], wud[ex].rearrange("(kt p) f -> p kt f", p=128), (), ["wu%d" % q])
                    dma("pool", "wdn%d" % q, wdn[q][:], wdd[ex].rearrange("(fc p) n -> p fc n", p=128), (), ["wdn%d" % q])
                    for sb_ in range(TH // 512):
                        hq = sb_ % 2
                        tsl = slice(sb_ * 512, (sb_ + 1) * 512)
                        for fc in range(4):
                            pg_, pgk = nps()
                            pu_, puk = nps()
                            for kt in range(8):
                                mm(pg_[:], wg[q][:, kt, fc * 128:(fc + 1) * 128], h2T[:, kt, tsl], kt == 0, kt == 7, ["wg%d" % q, "h2T"], [pgk])
                            for kt in range(8):
                                mm(pu_[:], wu[q][:, kt, fc * 128:(fc + 1) * 128], h2T[:, kt, tsl], kt == 0, kt == 7, ["wu%d" % q, "h2T"], [puk])
                            sq_ = fc % 2
                            act(sgl[sq_][:], pg_[:], AF.Silu, [pgk], ["sgl%d" % sq_])
                            tt("dve", hid[hq][:, fc, :], sgl[sq_][:], pu_[:], ALU.mult, ["sgl%d" % sq_, puk], ["hid%d_%d" % (hq, fc)])
                        for t4 in range(4):
                            ti = sb_ * 4 + t4
                            for nh in range(2):
                                po, pok = nps()
                                for fc in range(4):
                                    mm(po[:], hid[hq][:, fc, t4 * 128:(t4 + 1) * 128], wdn[q][:, fc, nh * 512:(nh + 1) * 512], fc == 0, fc == 3,
                                       ["hid%d_%d" % (hq, fc), "wdn%d" % q], [pok])
                                stt(acc[:, ti, nh * 512:(nh + 1) * 512], po[:], gates[:, ti, ex:ex + 1], acc[:, ti, nh * 512:(nh + 1) * 512],
                                    ALU.mult, ALU.add, [pok, "gates", "acc%d" % ti], ["acc%d" % ti])
                for i in range(NTH):
                    tok0 = hh * TH + i * 128
                    dma("sp", "ost%d" % (i % 4), out[tok0:tok0 + 128, :], acc[:, i, :], ["acc%d" % i], ["out%d" % i])
                P.barrier()
        P.barrier()
        P.emit()
    return nc


def make_core_inputs(inputs, c, T, consts, pvt):
    b, th = c // 2, c % 2
    TP = TO = T // 2
    x = inputs["x"]
    d = {}
    d["xp"] = np.ascontiguousarray(x[b, 0:TP]) if th == 1 else np.zeros((TP, D), np.float32)
    d["xo"] = np.ascontiguousarray(x[b, th * TO:(th + 1) * TO])
    d["w_in"] = inputs["w_in"][0]
    d["pv"] = pvt
    d["rw_w2"] = inputs["rw_w2"][0]
    d["rw_a2"] = inputs["rw_a2"][0]
    d["rw_g2"] = inputs["rw_g2"][0]
    d["w_out"] = inputs["w_out"][0]
    d["rgw"] = inputs["router_group_w"][0]
    d["rew"] = inputs["router_expert_w"][0]
    d["rb"] = np.concatenate([inputs["router_group_b"][0], inputs["router_expert_b"][0]])[None, :]
    d["wg"] = inputs["exp_w_gate"][0]
    d["wu"] = inputs["exp_w_up"][0]
    d["wd"] = inputs["exp_w_down"][0]
    kb = np.zeros((128, T // 128), np.float32)
    if th == 0:
        kb[:, :TP // 128] = -30000.0
    d["kb"] = kb
    d.update(consts)
    return d


def kernel(**inputs):
    inputs = {k: np.asarray(v) for k, v in inputs.items()}
    x = inputs["x"]
    B, T, _ = x.shape
    TP = TO = T // 2
    consts = host_consts()
    pvt = host_pv(inputs)
    nc = build(TP, TO)
    ncores = 2 * B
    in_maps = [make_core_inputs(inputs, c, T, consts, pvt) for c in range(ncores)]
    res = run_bass_kernel_spmd(nc, in_maps, core_ids=list(range(ncores)))
    out = np.empty((B, T, D), np.float32)
    for c in range(ncores):
        b, th = c // 2, c % 2
        out[b, th * TO:(th + 1) * TO] = res.results[c]["out"]
    return out
```
